# Optimizing a Trainium2 kernel written in Bass

```python
import jax
import jax.numpy as jnp
from jax import lax
import numpy as np

D_MODEL = 1024
BATCH = 4
SEQ = 4096
DEPTH = 1
DEC_BATCH = 128
DEC_SEQ = 1
PAST_LEN = 8192
PAGE_SIZE = 128

N_HEADS = 8
HEAD_DIM = 64
KV_HEADS = 2
GQA_GROUP = N_HEADS // KV_HEADS
D_ATTN = N_HEADS * HEAD_DIM
D_CONV = D_MODEL - D_ATTN
D_KV = KV_HEADS * HEAD_DIM
CMP_BLOCK = 32
CMP_STRIDE = 16
CMP_RATIO = CMP_BLOCK // CMP_STRIDE
CMP_HIDDEN = HEAD_DIM
SLC_BLOCK = 64
SLC_TOPN = 16
N_LOCAL_BLOCKS = 2
WINDOW = 512
FORCED_SCORE = 1e4
CONV_WIDTH = 31
N_EXPERT_GROUPS = 4
EXPERTS_PER_GROUP = 8
N_EXPERTS = N_EXPERT_GROUPS * EXPERTS_PER_GROUP
D_EXPERT = 256
MOE_TOPK = 2
Q_BLOCK = 128
EPS = 1e-6
NEG_INF = -1e30
D_IN = D_ATTN + 6 * D_KV + 3 * N_HEADS + 2 * D_CONV

kernel_name = 'hymba_nsa_conformer_hmoe_step'


def rmsnorm(x, g):
    x32 = x.astype(jnp.float32)
    y = x32 * lax.rsqrt(jnp.mean(x32 * x32, axis=-1, keepdims=True) + EPS)
    return (y * g.astype(jnp.float32)).astype(x.dtype)


def layernorm(x, g, b):
    x32 = x.astype(jnp.float32)
    xc = x32 - jnp.mean(x32, axis=-1, keepdims=True)
    y = xc * lax.rsqrt(jnp.mean(xc * xc, axis=-1, keepdims=True) + EPS)
    return (y * g.astype(jnp.float32) + b.astype(jnp.float32)).astype(x.dtype)


def masked_softmax(s, mask):
    p = jax.nn.softmax(jnp.where(mask, s, NEG_INF), axis=-1)
    return jnp.where(mask, p, 0.0)


def adaln(c, w_ada, b_ada):
    m = (jax.nn.silu(c) @ w_ada + b_ada).reshape(c.shape[0], 6, D_MODEL)
    return [m[:, i, None, :] for i in range(6)]


def split_proj(p):
    b, s = p.shape[:2]
    q = p[..., :D_ATTN].reshape(b, s, N_HEADS, HEAD_DIM)
    o = D_ATTN
    kv = p[..., o:o + 6 * D_KV].reshape(b, s, 3, 2, KV_HEADS, HEAD_DIM)
    o += 6 * D_KV
    gl = p[..., o:o + 3 * N_HEADS].reshape(b, s, N_HEADS, 3)
    o += 3 * N_HEADS
    u = p[..., o:o + D_CONV] * jax.nn.sigmoid(p[..., o + D_CONV:])
    return q, kv[:, :, 0], kv[:, :, 1], kv[:, :, 2], gl, u


def mixer_input(x, mods, norm_g, w_in):
    h = rmsnorm(x, norm_g) * (1.0 + mods[1]) + mods[0]
    return split_proj(h @ w_in)


def chunk_project(rows, w_cmp1):
    b, t = rows.shape[:2]
    n = t // CMP_STRIDE
    chunks = rows[:, :n * CMP_STRIDE].reshape(b, n, CMP_STRIDE, 2, KV_HEADS, HEAD_DIM)
    w1 = w_cmp1.reshape(2, CMP_RATIO, CMP_STRIDE, HEAD_DIM, CMP_HIDDEN)
    return jnp.einsum('bnsckd,crsdf->bnrckf', chunks, w1)


def compress_chunks(part, w_cmp1, pos_cmp, w_cmp2):
    nb = part.shape[1] - CMP_RATIO + 1
    pre = part[:, 0:nb, 0]
    for r in range(1, CMP_RATIO):
        pre = pre + part[:, r:r + nb, r]
    pre = pre + jnp.einsum('cld,cldf->cf', pos_cmp, w_cmp1)[:, None, :]
    out = jnp.einsum('bnckf,cfd->bnckd', jax.nn.silu(pre), w_cmp2)
    return out[:, :, 0], out[:, :, 1]


def overlap_matrix(nb_cmp, nb_slc):
    c_start = jnp.arange(nb_cmp) * CMP_STRIDE
    s_start = jnp.arange(nb_slc) * SLC_BLOCK
    hit = (c_start[:, None] <= s_start[None, :] + SLC_BLOCK - 1) & (c_start[:, None] + CMP_BLOCK - 1 >= s_start[None, :])
    return hit.astype(jnp.float32)


def nsa_attend(q, q_pos, kc, vc, gather_sel, n_slc, kw, vw, kw_pos, gate_logits):
    f32 = jnp.float32
    b, nq = q.shape[:2]
    scale = HEAD_DIM ** -0.5
    qg = q.reshape(b, nq, KV_HEADS, GQA_GROUP, HEAD_DIM).transpose(0, 2, 3, 1, 4)
    nb = kc.shape[1]
    c_mask = (jnp.arange(nb) * CMP_STRIDE + CMP_BLOCK - 1)[None, :] <= q_pos[:, None]
    s_c = jnp.einsum('bkgqd,bnkd->bkgqn', qg, kc).astype(f32) * scale
    p_c = masked_softmax(s_c, c_mask)
    o_c = jnp.einsum('bkgqn,bnkd->bkgqd', p_c.astype(vc.dtype), vc)
    imp = jnp.einsum('bkgqn,nj->bkqj', p_c, overlap_matrix(nb, n_slc))
    blk = jnp.arange(n_slc)[None, :]
    cur = (q_pos // SLC_BLOCK)[:, None]
    valid = blk <= cur
    forced = (blk == 0) | ((cur - blk >= 0) & (cur - blk < N_LOCAL_BLOCKS))
    imp = jnp.where(valid & forced, FORCED_SCORE, jnp.where(valid, imp, -1.0))
    top_val, top_idx = lax.top_k(imp, min(SLC_TOPN, n_slc))
    kv_sel = gather_sel(top_idx)
    nsel = top_idx.shape[-1]
    key_pos = top_idx[..., None] * SLC_BLOCK + jnp.arange(SLC_BLOCK)
    sel_mask = (top_val >= 0.0)[..., None] & (key_pos <= q_pos[None, None, :, None, None])
    s_s = jnp.einsum('bkgqd,bkqnld->bkgqnl', qg, kv_sel[..., 0, :]).astype(f32) * scale
    p_s = masked_softmax(s_s.reshape(b, KV_HEADS, GQA_GROUP, nq, nsel * SLC_BLOCK),
                         sel_mask.reshape(b, KV_HEADS, 1, nq, nsel * SLC_BLOCK))
    p_s = p_s.reshape(b, KV_HEADS, GQA_GROUP, nq, nsel, SLC_BLOCK)
    o_s = jnp.einsum('bkgqnl,bkqnld->bkgqd', p_s.astype(kv_sel.dtype), kv_sel[..., 1, :])
    w_mask = (kw_pos[None, :] <= q_pos[:, None]) & (kw_pos[None, :] >= q_pos[:, None] - WINDOW) & (kw_pos[None, :] >= 0)
    s_w = jnp.einsum('bkgqd,btkd->bkgqt', qg, kw).astype(f32) * scale
    p_w = masked_softmax(s_w, w_mask)
    o_w = jnp.einsum('bkgqt,btkd->bkgqd', p_w.astype(vw.dtype), vw)
    g = jax.nn.sigmoid(gate_logits.astype(f32)).reshape(b, nq, KV_HEADS, GQA_GROUP, 3).transpose(0, 2, 3, 1, 4)
    o = g[..., 0:1] * o_c + g[..., 1:2] * o_s + g[..., 2:3] * o_w
    return o.transpose(0, 3, 1, 2, 4).reshape(b, nq, D_ATTN).astype(q.dtype)


def conformer_conv(u_ext, w_dw, b_dw, ln_g, ln_b):
    y = lax.conv_general_dilated(u_ext, w_dw[:, None, :], window_strides=(1,), padding='VALID',
                                 dimension_numbers=('NWC', 'WIO', 'NWC'), feature_group_count=D_CONV)
    return jax.nn.silu(layernorm(y + b_dw, ln_g, ln_b))


def hier_moe(h, w_group, b_group, w_router, b_router, w_gate, w_up, w_down):
    f32 = jnp.float32
    shp = h.shape
    hf = h.reshape(-1, D_MODEL)
    p_group = jax.nn.softmax((hf @ w_group).astype(f32) + b_group, axis=-1)
    p_top, g_idx = lax.top_k(p_group, 1)
    e_logits = ((hf @ w_router).astype(f32) + b_router).reshape(-1, N_EXPERT_GROUPS, EXPERTS_PER_GROUP)
    e_in = jnp.take_along_axis(e_logits, g_idx[:, :, None], axis=1)[:, 0]
    w_top, e_idx = lax.top_k(jax.nn.softmax(e_in, axis=-1), MOE_TOPK)
    w_top = w_top / jnp.sum(w_top, axis=-1, keepdims=True) * p_top
    eid = g_idx * EXPERTS_PER_GROUP + e_idx
    combine = jnp.sum(jax.nn.one_hot(eid, N_EXPERTS, dtype=f32) * w_top[..., None], axis=1)
    hid = jax.nn.silu(jnp.einsum('nd,edf->nef', hf, w_gate)) * jnp.einsum('nd,edf->nef', hf, w_up)
    y = jnp.einsum('nef,ne,efd->nd', hid, combine.astype(hid.dtype), w_down)
    return y.reshape(shp)


def finish_layer(x, mods, o_attn, o_conv, g_attn_out, g_conv_out, w_out, norm2_g,
                 w_group, b_group, w_router, b_router, w_gate, w_up, w_down):
    mix = jnp.concatenate([rmsnorm(o_attn, g_attn_out), rmsnorm(o_conv, g_conv_out)], axis=-1) @ w_out
    x = x + mods[2] * mix
    h = rmsnorm(x, norm2_g) * (1.0 + mods[4]) + mods[3]
    return x + mods[5] * hier_moe(h, w_group, b_group, w_router, b_router, w_gate, w_up, w_down)


def setup_inputs(seed: int = 0) -> dict:
    key = jax.random.key(seed)
    ks = iter(jax.random.split(key, 48))
    f32 = jnp.float32

    def nrm(shape, scale=1.0):
        return jax.random.normal(next(ks), shape, f32) * scale

    n_pages = PAST_LEN // PAGE_SIZE
    n_used = DEC_BATCH * n_pages
    n_phys = n_used + n_used // 4
    win_buf = min(WINDOW, PAST_LEN)
    d_in = D_MODEL ** -0.5
    return {
        'x_prompt': nrm((BATCH, SEQ, D_MODEL)),
        'x_sample': nrm((DEC_BATCH, DEC_SEQ, D_MODEL)),
        'cache_cmp_kv': nrm((DEPTH, n_phys, PAGE_SIZE, 2, KV_HEADS, HEAD_DIM)),
        'cache_slc_kv': nrm((DEPTH, n_phys, PAGE_SIZE, 2, KV_HEADS, HEAD_DIM)),
        'state_win_kv': nrm((DEPTH, DEC_BATCH, win_buf, 2, KV_HEADS, HEAD_DIM)),
        'state_conv': nrm((DEPTH, DEC_BATCH, CONV_WIDTH - 1, D_CONV), 0.5),
        'page_table': jax.random.permutation(next(ks), n_phys)[:n_used].reshape(DEC_BATCH, n_pages).astype(jnp.int32),
        'c_prompt': nrm((BATCH, D_MODEL)),
        'c_sample': nrm((DEC_BATCH, D_MODEL)),
        'norm1_g': 1.0 + nrm((DEPTH, D_MODEL), 0.02),
        'w_ada': nrm((DEPTH, D_MODEL, 6 * D_MODEL), 0.5 * d_in),
        'b_ada': nrm((DEPTH, 6 * D_MODEL), 0.02),
        'w_in': nrm((DEPTH, D_MODEL, D_IN), d_in),
        'w_cmp1': nrm((DEPTH, 2, CMP_BLOCK, HEAD_DIM, CMP_HIDDEN), (CMP_BLOCK * HEAD_DIM) ** -0.5),
        'pos_cmp': nrm((DEPTH, 2, CMP_BLOCK, HEAD_DIM), 0.1),
        'w_cmp2': nrm((DEPTH, 2, CMP_HIDDEN, HEAD_DIM), CMP_HIDDEN ** -0.5),
        'w_dw': nrm((DEPTH, CONV_WIDTH, D_CONV), CONV_WIDTH ** -0.5),
        'b_dw': nrm((DEPTH, D_CONV), 0.02),
        'conv_ln_g': 1.0 + nrm((DEPTH, D_CONV), 0.02),
        'conv_ln_b': nrm((DEPTH, D_CONV), 0.02),
        'g_attn_out': 1.0 + nrm((DEPTH, D_ATTN), 0.02),
        'g_conv_out': 1.0 + nrm((DEPTH, D_CONV), 0.02),
        'w_out': nrm((DEPTH, D_MODEL, D_MODEL), d_in),
        'norm2_g': 1.0 + nrm((DEPTH, D_MODEL), 0.02),
        'w_group': nrm((DEPTH, D_MODEL, N_EXPERT_GROUPS), d_in),
        'b_group': nrm((DEPTH, N_EXPERT_GROUPS), 0.01),
        'w_router': nrm((DEPTH, D_MODEL, N_EXPERTS), d_in),
        'b_router': nrm((DEPTH, N_EXPERTS), 0.01),
        'w_gate': nrm((DEPTH, N_EXPERTS, D_MODEL, D_EXPERT), d_in),
        'w_up': nrm((DEPTH, N_EXPERTS, D_MODEL, D_EXPERT), d_in),
        'w_down': nrm((DEPTH, N_EXPERTS, D_EXPERT, D_MODEL), D_EXPERT ** -0.5),
        'final_g': 1.0 + nrm((D_MODEL,), 0.02),
    }


def reference(x_prompt, x_sample, cache_cmp_kv, cache_slc_kv, state_win_kv, state_conv, page_table,
              c_prompt, c_sample, norm1_g, w_ada, b_ada, w_in, w_cmp1, pos_cmp, w_cmp2, w_dw, b_dw,
              conv_ln_g, conv_ln_b, g_attn_out, g_conv_out, w_out, norm2_g, w_group, b_group,
              w_router, b_router, w_gate, w_up, w_down, final_g):
    n_past_slc = PAST_LEN // SLC_BLOCK
    sub = PAGE_SIZE // SLC_BLOCK
    n_new_slc = -(-DEC_SEQ // SLC_BLOCK)
    win_buf = state_win_kv.shape[2]
    win_keep_p = min(WINDOW, SEQ)
    bi_p = jnp.arange(BATCH)[:, None, None, None]
    bi_s = jnp.arange(DEC_BATCH)[:, None, None, None]
    ki = jnp.arange(KV_HEADS)[None, :, None, None]
    q_pos_s = PAST_LEN + jnp.arange(DEC_SEQ)
    kw_pos_s = PAST_LEN - win_buf + jnp.arange(win_buf + DEC_SEQ)

    xp, xs = x_prompt, x_sample
    cmp_p, slc_p, win_p, conv_p = [], [], [], []
    cmp_s, slc_s, win_s, conv_s = [], [], [], []
    for l in range(DEPTH):
        mods_p = adaln(c_prompt, w_ada[l], b_ada[l])
        mods_s = adaln(c_sample, w_ada[l], b_ada[l])

        q, kv_c, kv_s, kv_w, gl, u = mixer_input(xp, mods_p, norm1_g[l], w_in[l])
        kc, vc = compress_chunks(chunk_project(kv_c, w_cmp1[l]), w_cmp1[l], pos_cmp[l], w_cmp2[l])
        slc_blocks = kv_s.reshape(BATCH, SEQ // SLC_BLOCK, SLC_BLOCK, 2, KV_HEADS, HEAD_DIM)
        win_pad = jnp.pad(kv_w, ((0, 0), (WINDOW, 0), (0, 0), (0, 0), (0, 0)))

        def gather_p(idx):
            return slc_blocks[bi_p, idx, :, :, ki]

        def block_fn(i):
            start = i * Q_BLOCK
            q_b = lax.dynamic_slice_in_dim(q, start, Q_BLOCK, axis=1)
            gl_b = lax.dynamic_slice_in_dim(gl, start, Q_BLOCK, axis=1)
            w_b = lax.dynamic_slice_in_dim(win_pad, start, WINDOW + Q_BLOCK, axis=1)
            q_pos = start + jnp.arange(Q_BLOCK)
            kw_pos = start - WINDOW + jnp.arange(WINDOW + Q_BLOCK)
            return nsa_attend(q_b, q_pos, kc, vc, gather_p, SEQ // SLC_BLOCK,
                              w_b[:, :, 0], w_b[:, :, 1], kw_pos, gl_b)

        o_blocks = lax.map(block_fn, jnp.arange(SEQ // Q_BLOCK))
        o_attn = jnp.transpose(o_blocks, (1, 0, 2, 3)).reshape(BATCH, SEQ, D_ATTN)
        u_ext = jnp.pad(u, ((0, 0), (CONV_WIDTH - 1, 0), (0, 0)))
        o_conv = conformer_conv(u_ext, w_dw[l], b_dw[l], conv_ln_g[l], conv_ln_b[l])
        cmp_p.append(kv_c)
        slc_p.append(kv_s)
        win_p.append(kv_w[:, SEQ - win_keep_p:])
        conv_p.append(u_ext[:, -(CONV_WIDTH - 1):])
        xp = finish_layer(xp, mods_p, o_attn, o_conv, g_attn_out[l], g_conv_out[l], w_out[l], norm2_g[l],
                          w_group[l], b_group[l], w_router[l], b_router[l], w_gate[l], w_up[l], w_down[l])

        q, kv_c, kv_s, kv_w, gl, u = mixer_input(xs, mods_s, norm1_g[l], w_in[l])
        past_c = cache_cmp_kv[l, page_table].reshape(DEC_BATCH, PAST_LEN, 2, KV_HEADS, HEAD_DIM)
        part = chunk_project(past_c, w_cmp1[l])
        if DEC_SEQ >= CMP_STRIDE:
            part = jnp.concatenate([part, chunk_project(kv_c, w_cmp1[l])], axis=1)
        kc, vc = compress_chunks(part, w_cmp1[l], pos_cmp[l], w_cmp2[l])
        new_blocks = jnp.pad(kv_s, ((0, 0), (0, n_new_slc * SLC_BLOCK - DEC_SEQ), (0, 0), (0, 0), (0, 0)))
        new_blocks = new_blocks.reshape(DEC_BATCH, n_new_slc, SLC_BLOCK, 2, KV_HEADS, HEAD_DIM)

        def gather_s(idx):
            jp = jnp.minimum(idx, n_past_slc - 1)
            pg = page_table[bi_s, jp // sub]
            rw = (jp % sub)[..., None] * SLC_BLOCK + jnp.arange(SLC_BLOCK)
            from_pool = cache_slc_kv[l, pg[..., None], rw, :, ki[..., None]]
            jn = jnp.clip(idx - n_past_slc, 0, n_new_slc - 1)
            from_new = new_blocks[bi_s, jn, :, :, ki]
            return jnp.where((idx < n_past_slc)[..., None, None, None], from_pool, from_new)

        kw_all = jnp.concatenate([state_win_kv[l], kv_w], axis=1)
        o_attn = nsa_attend(q, q_pos_s, kc, vc, gather_s, n_past_slc + n_new_slc,
                            kw_all[:, :, 0], kw_all[:, :, 1], kw_pos_s, gl)
        u_ext = jnp.concatenate([state_conv[l], u], axis=1)
        o_conv = conformer_conv(u_ext, w_dw[l], b_dw[l], conv_ln_g[l], conv_ln_b[l])
        cmp_s.append(kv_c)
        slc_s.append(kv_s)
        win_s.append(kw_all[:, -win_buf:])
        conv_s.append(u_ext[:, -(CONV_WIDTH - 1):])
        xs = finish_layer(xs, mods_s, o_attn, o_conv, g_attn_out[l], g_conv_out[l], w_out[l], norm2_g[l],
                          w_group[l], b_group[l], w_router[l], b_router[l], w_gate[l], w_up[l], w_down[l])

    y_prompt = rmsnorm(xp, final_g)
    y_sample = rmsnorm(xs, final_g)
    new_cmp_prompt = jnp.stack(cmp_p, axis=0)
    new_slc_prompt = jnp.stack(slc_p, axis=0)
    new_win_prompt = jnp.stack(win_p, axis=0)
    new_conv_prompt = jnp.stack(conv_p, axis=0)
    new_cmp_sample = jnp.stack(cmp_s, axis=0)
    new_slc_sample = jnp.stack(slc_s, axis=0)
    new_win_sample = jnp.stack(win_s, axis=0)
    new_conv_sample = jnp.stack(conv_s, axis=0)
    return (y_prompt, y_sample, new_cmp_prompt, new_slc_prompt, new_win_prompt, new_conv_prompt,
            new_cmp_sample, new_slc_sample, new_win_sample, new_conv_sample)
```

```python
from contextlib import ExitStack
import numpy as np
import concourse.bass as bass
import concourse.mybir as mybir
from concourse.bass_utils import run_bass_kernel_spmd

F32 = mybir.dt.float32
BF16 = mybir.dt.bfloat16
I32 = mybir.dt.int32
AF = mybir.ActivationFunctionType
ALU = mybir.AluOpType
AX = mybir.AxisListType

ENGS = ("pe", "act", "dve", "pool", "sp")
NDMA_SEMS = {"sp": 24, "pool": 12, "act": 8}

D = 1024
SEQ = 4096
NOWN = 2048
NS = 16
D_IN = 2328
EPS = 1e-6
STAGE = 4


class Buf:
    __slots__ = ("name", "last_w", "readers")

    def __init__(self, name=""):
        self.name = name
        self.last_w = None
        self.readers = []


class Prog:
    def __init__(self, nc):
        self.nc = nc
        self.q = {e: [] for e in ENGS}
        self.cnt = {e: 0 for e in ENGS}
        self.waited = {e: {} for e in ENGS}
        self.dma_i = {e: 0 for e in NDMA_SEMS}
        self.sems = {}
        self.final_tokens = []
        self.barrier_toks = []
        self.dma_last = {}

    def barrier(self):
        toks = [(("eng", e), self.cnt[e]) for e in ENGS if self.cnt[e] > 0]
        toks += list(self.dma_last.items())
        self.barrier_toks = toks

    def _deps(self, eng, reads, writes, strict=False):
        deps = {}

        def add(tok):
            if tok is None:
                return
            k, v = tok
            if k == ("eng", eng) and not strict and eng == "pe":
                return
            if deps.get(k, 0) < v:
                deps[k] = v
        for b in reads:
            add(b.last_w)
        for b in writes:
            add(b.last_w)
            for r in b.readers:
                add(r)
        for t in self.barrier_toks:
            if t[0] != ("eng", eng):
                add(t)
        out = []
        for k, v in deps.items():
            if self.waited[eng].get(k, 0) < v:
                self.waited[eng][k] = v
                out.append((k, v))
        return out

    def _commit(self, tok, reads, writes):
        for b in reads:
            b.readers.append(tok)
            if len(b.readers) > 48:
                mx = {}
                for k, v in b.readers:
                    if mx.get(k, 0) < v:
                        mx[k] = v
                b.readers = list(mx.items())
        for b in writes:
            b.last_w = tok
            b.readers = []

    def op(self, eng, emit, reads=(), writes=()):
        waits = self._deps(eng, reads, writes)
        self.cnt[eng] += 1
        tok = (("eng", eng), self.cnt[eng])
        self.q[eng].append((waits, emit, (("eng", eng), 1)))
        self._commit(tok, reads, writes)
        return tok

    def dma(self, eng, emit, reads=(), writes=(), final=False):
        n = NDMA_SEMS[eng]
        i = self.dma_i[eng]
        self.dma_i[eng] += 1
        k = ("dma", eng, i % n)
        val = 16 * (i // n + 1)
        waits = self._deps(eng, reads, writes, strict=True)
        if i >= n and self.waited[eng].get(k, 0) < val - 16:
            self.waited[eng][k] = val - 16
            waits.append((k, val - 16))
        self.q[eng].append((waits, emit, (k, 16)))
        tok = (k, val)
        self.dma_last[k] = val
        self._commit(tok, reads, writes)
        if final:
            self.final_tokens.append(tok)
        return tok

    def build(self, stack):
        nc = self.nc
        keys = [("eng", e) for e in ENGS]
        for e, n in NDMA_SEMS.items():
            keys += [("dma", e, i) for i in range(n)]
        for k in keys:
            self.sems[k] = stack.enter_context(nc.semaphore("s_" + "_".join(map(str, k))))
        fin = {}
        for k, v in self.final_tokens:
            fin[k] = max(fin.get(k, 0), v)
        block = stack.enter_context(nc.Block())
        sems, q, cnt = self.sems, self.q, self.cnt

        def replay(e, name):
            for waits, emit, inc in q[name]:
                for k, v in waits:
                    e.wait_ge(sems[k], v)
                emit(e).then_inc(sems[inc[0]], inc[1])
            if name == "sp":
                for k, v in fin.items():
                    e.wait_ge(sems[k], v)
                for en in ENGS:
                    if en != "sp" and cnt[en] > 0:
                        e.wait_ge(sems[("eng", en)], cnt[en])

        @block.sync
        def _(e):
            replay(e, "sp")

        @block.tensor
        def _(e):
            replay(e, "pe")

        @block.scalar
        def _(e):
            replay(e, "act")

        @block.vector
        def _(e):
            replay(e, "dve")

        @block.gpsimd
        def _(e):
            replay(e, "pool")


OFFS = {}


class Ctx:
    def __init__(self, nc, st, arena_bytes=0):
        self.nc, self.st = nc, st
        self.bufs = {}
        self.off = 0
        self.peak = 0
        self.arena_bytes = arena_bytes
        if arena_bytes:
            self.arena = st.enter_context(nc.sbuf_tensor("arena", [128, arena_bytes // 4], F32))

    def al(self, name, shape, dt=F32):
        esz = 4 if dt in (F32, I32) else 2
        n = 1
        for d in shape[1:]:
            n *= d
        nb = (n * esz + 31) // 32 * 32
        assert self.off + nb <= self.arena_bytes, (name, self.off, nb)
        v = self.arena[0:shape[0], self.off // 4:(self.off + nb) // 4]
        if dt != F32:
            v = v.bitcast(dt)
        v = v[:, 0:n]
        if len(shape) > 2:
            names = " ".join("a%d" % i for i in range(len(shape) - 1))
            kw = {"a%d" % i: shape[i + 1] for i in range(len(shape) - 1)}
            v = v.rearrange("p (%s) -> p %s" % (names, names), **kw)
        OFFS[name] = (self.off, tuple(shape), "f32" if dt == F32 else ("i32" if dt == I32 else "bf16"))
        self.off += nb
        self.peak = max(self.peak, self.off)
        b = Buf(name)
        self.bufs[name] = b
        return v, b

    def mark(self):
        return self.off

    def reset(self, m):
        self.off = m

    def sb(self, name, shape, dt=F32):
        t = self.st.enter_context(self.nc.sbuf_tensor(name, list(shape), dt))
        self.bufs[name] = Buf(name)
        return t, self.bufs[name]

    def ps(self, name, shape, dt=F32):
        t = self.st.enter_context(self.nc.psum_tensor(name, list(shape), dt))
        self.bufs[name] = Buf(name)
        return t, self.bufs[name]

    def din(self, name, shape, dt=F32):
        return self.nc.dram_tensor(name, list(shape), dt, kind="ExternalInput").ap()

    def dout(self, name, shape, dt=F32):
        return self.nc.dram_tensor(name, list(shape), dt, kind="ExternalOutput").ap()


import os
SKIP = set(os.environ.get('KSKIP', '').split(','))
NEG = -30000.0
SCALE = 0.125
ARENA = 196 * 1024


def build_program(stage=STAGE, upto='Z'):
    nc = bass.Bass("TRN2", target_bir_lowering=False)
    st = ExitStack()
    with st:
        C = Ctx(nc, st, arena_bytes=ARENA)
        P = Prog(nc)
        xc = C.din("xc", [SEQ, D])
        xs = C.din("xs", [NS, D])
        cin = C.din("cin", [1 + NS, D])
        norm1_g = C.din("norm1_g", [1, D])
        w_ada = C.din("w_ada", [D, 6 * D])
        b_ada = C.din("b_ada", [1, 6 * D])
        w_in = C.din("w_in", [D, D_IN])
        identf = C.din("identf", [128, 128])
        hfv = C.din("hfv", [128, 1])
        state_win = C.din("state_win", [NS, 512, 256])
        state_conv = C.din("state_conv", [NS, 30, 512])
        etab = C.din("etab", [128, SEQ])
        tridiag = C.din("tridiag", [128, 128])
        trilo = C.din("trilo", [128, 128])
        pe_slot = C.din("pe_slot", [128, 2])
        qrow = C.din("qrow", [1, NOWN])
        ovt = C.din("ovt", [128, 2, 64])
        blknat = C.din("blknat", [1, 64])
        isz = C.din("isz", [1, 64])
        curt = C.din("curt", [128, 16])
        nvrow = C.din("nvrow", [1, 512])
        w1bd = C.din("w1bd", [128, 2, 32, 128])
        posT2 = C.din("posT2", [128, 2, 32])
        w2bd = C.din("w2bd", [128, 2, 128])
        wdwT = C.din("wdwT", [128, 4, 31])
        bdwT = C.din("bdwT", [128, 4])
        conv_ln_g = C.din("conv_ln_g", [1, 512])
        conv_ln_b = C.din("conv_ln_b", [1, 512])
        g_conv_out = C.din("g_conv_out", [1, 512])
        g_attn_out = C.din("g_attn_out", [1, 512])

        if stage >= 4:
            cache_cmp = C.din("cache_cmp", [81920, 4096])
            cache_slc = C.din("cache_slc", [81920, 4096])
        ptrep = C.din("ptrep", [128, NS * 4], I32)
        pm8rep = C.din("pm8rep", [128, NS * 4])
        egtab = C.din("egtab", [128, 4, 128])
        ovstab = C.din("ovstab", [128, 4, 128])
        forctab = C.din("forctab", [128, 1])
        fmaskAtab = C.din("fmaskAtab", [2, 128])
        b_dw_row = C.din("b_dw_row", [1, 512])
        w_dw_rows = C.din("w_dw_rows", [1, 31 * 512])
        w_out = C.din("w_out", [D, D])
        norm2_g = C.din("norm2_g", [1, D])
        w_gr = C.din("w_gr", [D, 36])
        b_gr = C.din("b_gr", [1, 36])
        w_gate = C.din("w_gate", [32, D, 256])
        w_up = C.din("w_up", [32, D, 256])
        w_down = C.din("w_down", [32, 256, D])
        final_g = C.din("final_g", [1, D])
        y_p = C.dout("y_p", [NOWN, D])
        y_s = C.dout("y_s", [NS, D])
        o_kv_p = C.dout("o_kv_p", [SEQ, 768])
        o_kv_s = C.dout("o_kv_s", [NS, 768])
        o_conv_p = C.dout("o_conv_p", [30, 512])
        o_win_s = C.dout("o_win_s", [NS, 512, 256])
        o_conv_s = C.dout("o_conv_s", [NS, 30, 512])
        DBG = "D" in SKIP
        if stage == 2:
            dbg_attn = C.dout("dbg_attn", [NOWN, 512])
            dbg_conv = C.dout("dbg_conv", [NOWN, 512])
            dbg_br = C.dout("dbg_br", [3, NOWN, 512])
        if DBG:
            dbg_os = C.dout("dbg_os", [NS, 512])
            dbg_ycv = C.dout("dbg_ycv", [NS, 512])

        def scratch(name, shape, dt=F32):
            return nc.dram_tensor(name, list(shape), dt, kind="Internal").ap(), Buf(name)
        mods_d, b_mods_d = scratch("mods_d", [1 + NS, 6 * D])
        qT_d, b_qT_d = scratch("qT_d", [2, 16, 64, 512], BF16)
        u_d, b_u_d = scratch("u_d", [128, 4, 30 + NOWN], BF16)
        x2_d, b_x2_d = scratch("x2_d", [NOWN + NS, D])
        HAVE_SAMPLE = stage >= 4
        R_d, b_R_d = scratch("R_d", [NS, 8, 3, 129])

        ps_tr, b_ps_tr = C.ps("ps_tr", [128, 8, 128], BF16)
        ps_a, b_ps_a = C.ps("ps_a", [128, 512], F32)
        ps_b, b_ps_b = C.ps("ps_b", [128, 512], F32)
        ps_c, b_ps_c = C.ps("ps_c", [128, 512], F32)
        ps_d, b_ps_d = C.ps("ps_d", [128, 512], F32)
        ps_e, b_ps_e = C.ps("ps_e", [128, 512], F32)
        ps_f, b_ps_f = C.ps("ps_f", [128, 512], F32)
        ps_g, b_ps_g = C.ps("ps_g", [128, 512], F32)

        idf, b_idf = C.al("idf", [128, 128], F32)
        idb, b_idb = C.al("idb", [128, 128], BF16)
        epsT, b_eps = C.al("epsT", [128, 1], F32)
        hfvT, b_hfv = C.al("hfvT", [128, 1], F32)
        P.dma("sp", lambda e: e.dma_start(out=idf[:], in_=identf), writes=[b_idf])
        P.dma("sp", lambda e: e.dma_start(out=hfvT[:], in_=hfv), writes=[b_hfv])
        P.op("dve", lambda e: e.tensor_copy(out=idb[:], in_=idf[:]), reads=[b_idf], writes=[b_idb])
        P.op("dve", lambda e: e.memset(epsT[:], EPS), writes=[b_eps])

        m_const = C.mark()
        kvS, b_kvS = C.al("kvS", [NS, 768], F32)
        usS, b_usS = C.al("usS", [NS, 512], F32)
        qtmS, b_qtmS = C.al("qtmS", [NS, 512], F32)
        gatesS, b_gatesS = C.al("gatesS", [NS, 24], F32)
        QbdAll, b_QbdAll = C.al("QbdAll", [128, NS, 8], BF16)
        catS, b_catS = C.al("catS", [128, 8, NS], BF16)
        m_samp = C.mark()
        KE = [C.al("KE%d" % k, [128, SEQ], BF16) for k in range(2)]
        KwT, b_KwT = C.al("KwT", [128, NOWN + 512], BF16)
        XTc, b_XTc = C.al("XTc", [128, 2, SEQ + 32], BF16)
        Vs, b_Vs = C.al("Vs", [128, 32, 2, 68], BF16)
        Vw, b_Vw = C.al("Vw", [128, 20, 2, 68], BF16)
        gates, b_gates = C.al("gates", [128, 16, 24], F32)
        kcT, b_kcT = C.al("kcT", [128, 256], BF16)
        vcx, b_vcx = C.al("vcx", [128, 2, 2, 132], BF16)
        m_persist = C.mark()

        c_t, b_ct = C.al("c_t", [1 + NS, D], F32)
        c_b, b_cb = C.al("c_b", [1 + NS, D], F32)
        cT, b_cT = C.al("cT", [128, 8, 1 + NS], F32)
        badaT, b_badaT = C.al("badaT", [1 + NS, 6 * D], F32)
        wst = [C.al("wst%d" % i, [128, 8, 512], F32) for i in range(4)]
        wst_h = [[Buf("wsth%d_%d" % (i, j)) for j in range(2)] for i in range(4)]
        mo = [C.al("mo%d" % i, [1 + NS, 512], F32) for i in range(2)]
        P.dma("sp", lambda e: e.dma_start(out=c_t[:], in_=cin), writes=[b_ct])
        P.dma("sp", lambda e: e.dma_start(out=badaT[:], in_=b_ada.partition_broadcast(1 + NS)), writes=[b_badaT])
        P.op("act", lambda e: e.activation(out=c_b[:], in_=c_t[:], func=AF.Silu), reads=[b_ct], writes=[b_cb])
        for c in range(8):
            P.op("pe", lambda e, c=c: e.transpose(out=ps_c[:, c * (1 + NS):(c + 1) * (1 + NS)], in_=c_b[:, c * 128:(c + 1) * 128],
                                                  identity=idf[0:1 + NS, 0:1 + NS]),
                 reads=[b_cb, b_idf], writes=[b_ps_c])
        P.op("dve", lambda e: e.tensor_copy(out=cT[:], in_=ps_c[:, 0:8 * (1 + NS)].rearrange("p (a b) -> p a b", a=8)),
             reads=[b_ps_c], writes=[b_cT])
        w_ada_v = w_ada.rearrange("(c p) n -> p c n", p=128)
        for blk in range(12):
            (wf, b_wf), (m_o, b_mo) = wst[blk % 4], mo[blk % 2]
            psx, b_psx = (ps_a, b_ps_a) if blk % 2 == 0 else (ps_b, b_ps_b)
            sl = slice(blk * 512, (blk + 1) * 512)
            q_ = "sp" if blk % 2 == 0 else "act"
            hb2 = wst_h[blk % 4]
            for hh in range(2):
                P.dma(q_, lambda e, sl=sl, wf=wf, hh=hh: e.dma_start(out=wf[:, hh * 4:(hh + 1) * 4, :], in_=w_ada_v[:, hh * 4:(hh + 1) * 4, sl]),
                      writes=[hb2[hh]])
            for c in range(8):
                P.op("pe", lambda e, c=c, wf=wf, psx=psx: e.matmul(psx[0:1 + NS, :], lhsT=cT[:, c, :], rhs=wf[:, c, :],
                                                                    start=(c == 0), stop=(c == 7)),
                     reads=[b_cT, hb2[c // 4]], writes=[b_psx])
            P.op("dve", lambda e, sl=sl, psx=psx, m_o=m_o: e.tensor_tensor(out=m_o[:], in0=psx[0:1 + NS, :],
                                                                            in1=badaT[:, sl], op=ALU.add),
                 reads=[b_psx, b_badaT], writes=[b_mo])
            P.dma("sp", lambda e, sl=sl, m_o=m_o: e.dma_start(out=mods_d[:, sl], in_=m_o[:]), reads=[b_mo], writes=[b_mods_d])

        def load_mod(i, dstP, b_dstP, dstS, b_dstS):
            P.dma("sp", lambda e: e.dma_start(out=dstP[:], in_=mods_d[0:1, i * D:(i + 1) * D].partition_broadcast(128)),
                  reads=[b_mods_d], writes=[b_dstP])
            P.dma("sp", lambda e: e.dma_start(out=dstS[:], in_=mods_d[1:1 + NS, i * D:(i + 1) * D]),
                  reads=[b_mods_d], writes=[b_dstS])

        if upto == 'A':
            P.build(st)
            return nc
        P.barrier()
        C.reset(m_persist)

        winb, b_winb = C.al("winb", [128, 8, D_IN], BF16)
        wstB, b_wstB = C.al("wstB", [128, 8, 256], F32)
        sh1P, b_sh1P = C.al("sh1P", [128, D], F32)
        A1P, b_A1P = C.al("A1P", [128, D], F32)
        sh1S, b_sh1S = C.al("sh1S", [NS, D], F32)
        A1S, b_A1S = C.al("A1S", [NS, D], F32)
        g1P, b_g1P = C.al("g1P", [128, D], F32)
        load_mod(0, sh1P, b_sh1P, sh1S, b_sh1S)
        load_mod(1, A1P, b_A1P, A1S, b_A1S)
        P.dma("sp", lambda e: e.dma_start(out=g1P[:], in_=norm1_g.partition_broadcast(128)), writes=[b_g1P])
        P.op("dve", lambda e: e.scalar_tensor_tensor(out=A1P[:], in0=A1P[:], scalar=1.0, in1=g1P[:],
                                                     op0=ALU.add, op1=ALU.mult),
             reads=[b_A1P, b_g1P], writes=[b_A1P])
        P.op("dve", lambda e: e.scalar_tensor_tensor(out=A1S[:], in0=A1S[:], scalar=1.0, in1=g1P[0:NS, :],
                                                     op0=ALU.add, op1=ALU.mult),
             reads=[b_A1S, b_g1P], writes=[b_A1S])
        w_in_v = w_in.rearrange("(c p) n -> p c n", p=128)
        for c0 in range(0, D_IN, 256):
            c1 = min(D_IN, c0 + 256)
            P.dma("sp", lambda e, c0=c0, c1=c1: e.dma_start(out=wstB[:, :, 0:c1 - c0], in_=w_in_v[:, :, c0:c1]),
                  writes=[b_wstB])
            if c0 < 512:
                for hh in range(4):
                    h = c0 // 64 + hh
                    k, g = h // 4, h % 4
                    dc = g * 128 + k * 64
                    P.op("pool", lambda e, hh=hh, dc=dc: e.tensor_copy(out=winb[:, :, dc:dc + 64],
                                                                       in_=wstB[:, :, hh * 64:(hh + 1) * 64]),
                         reads=[b_wstB], writes=[b_winb])
            else:
                P.op("pool", lambda e, c0=c0, c1=c1: e.tensor_copy(out=winb[:, :, c0:c1], in_=wstB[:, :, 0:c1 - c0]),
                     reads=[b_wstB], writes=[b_winb])

        P.op("pool", lambda e: e.memset(Vs[:], 0.0), writes=[b_Vs])
        P.op("pool", lambda e: e.memset(Vw[:], 0.0), writes=[b_Vw])
        P.op("pool", lambda e: e.memset(Vs[:, :, :, 64:65], 1.0), writes=[b_Vs])
        P.op("pool", lambda e: e.memset(Vw[:, :, :, 64:65], 1.0), writes=[b_Vw])
        for k in range(2):
            r0 = 64 if k == 0 else 0
            for c0 in range(0, SEQ, 2048):
                P.dma("sp", lambda e, r0=r0, c0=c0: e.dma_start(out=wstB[r0:r0 + 64, :, :].rearrange("p a b -> p (a b)"),
                                                                 in_=etab[r0:r0 + 64, c0:c0 + 2048]), writes=[b_wstB])
                P.op("pool", lambda e, r0=r0, c0=c0, k=k: e.tensor_copy(
                    out=KE[k][0][r0:r0 + 64, c0:c0 + 2048], in_=wstB[r0:r0 + 64, :, :].rearrange("p a b -> p (a b)")),
                    reads=[b_wstB], writes=[KE[k][1]])

        if upto == 'B1':
            P.build(st)
            return nc
        xt = [C.al("xt%d" % i, [128, D], F32) for i in range(2)]
        sss = [C.al("ss%d" % i, [128, 1], F32) for i in range(2)]
        rstds = [C.al("rstd%d" % i, [128, 1], F32) for i in range(2)]
        h32s = [C.al("h32%d" % i, [128, D], F32) for i in range(2)]
        hbs = [C.al("hb%d" % i, [128, D], BF16) for i in range(2)]
        hTs = [C.al("hT%d" % i, [128, 8, 128], BF16) for i in range(2)]
        kvt = [C.al("kvt%d" % i, [128, 792], F32) for i in range(2)]
        sig, b_sig = C.al("sig", [128, 4, 128], F32)
        qtmp, b_qtmp = C.al("qtmp", [128, 4, 128], BF16)
        utmp, b_utmp = C.al("utmp", [128, 4, 128], BF16)
        u32, b_u32 = C.al("u32", [128, 4, 32], F32)
        ps_q, b_ps_q = ps_c[:].rearrange("p (a b) -> p a b", a=4), b_ps_c
        ps_kv, b_ps_kv = ps_d[:].rearrange("p (a b) -> p a b", a=4), b_ps_d
        ps_ga, b_ps_ga = ps_e[:].rearrange("p (a b) -> p a b", a=4), b_ps_e
        ps_gg, b_ps_gg = ps_f[:].rearrange("p (a b) -> p a b", a=4), b_ps_f

        def glu_part(i, kind):
            hT, b_hT = hTs[i % 2]
            for j in range(4):
                for c in range(8):
                    P.op("pe", lambda e, c=c, j=j: e.matmul(ps_ga[:, j, :], lhsT=winb[:, c, 1304 + j * 128:1304 + (j + 1) * 128],
                                                            rhs=hT[:, c, :], start=(c == 0), stop=(c == 7)),
                         reads=[b_hT, b_winb], writes=[b_ps_ga])
            for j in range(4):
                for c in range(8):
                    P.op("pe", lambda e, c=c, j=j: e.matmul(ps_gg[:, j, :], lhsT=winb[:, c, 1816 + j * 128:1816 + (j + 1) * 128],
                                                            rhs=hT[:, c, :], start=(c == 0), stop=(c == 7)),
                         reads=[b_hT, b_winb], writes=[b_ps_gg])
            P.op("act", lambda e: e.activation(out=sig[:], in_=ps_gg[:], func=AF.Sigmoid), reads=[b_ps_gg], writes=[b_sig])
            if kind == "own":
                P.op("dve", lambda e: e.tensor_tensor(out=utmp[:], in0=ps_ga[:], in1=sig[:], op=ALU.mult),
                     reads=[b_ps_ga, b_sig], writes=[b_utmp])
                P.dma("pool", lambda e: e.dma_start(out=u_d[:, :, 30 + i * 128:30 + (i + 1) * 128], in_=utmp[:]),
                      reads=[b_utmp], writes=[b_u_d])
                if i == 15:
                    P.op("dve", lambda e: e.tensor_tensor(out=u32[:, :, 0:30], in0=ps_ga[:, :, 98:128], in1=sig[:, :, 98:128],
                                                          op=ALU.mult),
                         reads=[b_ps_ga, b_sig], writes=[b_u32])
            else:
                P.op("dve", lambda e: e.scalar_tensor_tensor(out=utmp[:, :, 0:30], in0=ps_ga[:, :, 98:128],
                                                             scalar=hfvT[:, 0:1], in1=sig[:, :, 98:128],
                                                             op0=ALU.mult, op1=ALU.mult),
                     reads=[b_ps_ga, b_sig, b_hfv], writes=[b_utmp])
                P.dma("pool", lambda e: e.dma_start(out=u_d[:, :, 0:30], in_=utmp[:, :, 0:30]),
                      reads=[b_utmp], writes=[b_u_d])

        def token_tile(i, kind):
            n = NS if kind == "sample" else 128
            hT, b_hT = hTs[i % 2]
            hb, b_hb = hbs[i % 2]
            h32, b_h32 = h32s[i % 2]
            ss, b_ss = sss[i % 2]
            rstd, b_rstd = rstds[i % 2]
            (x_t, b_x) = xt[i % 2]
            (kv_t, b_kv) = kvt[i % 2]
            src = xs if kind == "sample" else xc[i * 128:(i + 1) * 128, :]
            A1, b_A1 = (A1S, b_A1S) if kind == "sample" else (A1P, b_A1P)
            sh1, b_sh1 = (sh1S, b_sh1S) if kind == "sample" else (sh1P, b_sh1P)
            P.dma("sp", lambda e: e.dma_start(out=x_t[0:n, :], in_=src), writes=[b_x])
            P.op("act", lambda e: e.activation(out=h32[0:n, :], in_=x_t[0:n, :], func=AF.Square, accum_out=ss[0:n, :]),
                 reads=[b_x], writes=[b_h32, b_ss])
            P.op("act", lambda e: e.activation(out=rstd[0:n, :], in_=ss[0:n, :], func=AF.Sqrt, scale=1.0 / D,
                                               bias=epsT[0:n, :]),
                 reads=[b_ss, b_eps], writes=[b_rstd])
            P.op("dve", lambda e: e.reciprocal(out=rstd[0:n, :], in_=rstd[0:n, :]), reads=[b_rstd], writes=[b_rstd])
            P.op("dve", lambda e: e.scalar_tensor_tensor(out=h32[0:n, :], in0=x_t[0:n, :], scalar=rstd[0:n, :],
                                                         in1=A1[0:n, :], op0=ALU.mult, op1=ALU.mult),
                 reads=[b_x, b_rstd, b_A1], writes=[b_h32])
            P.op("pool", lambda e: e.tensor_tensor(out=hb[0:n, :], in0=h32[0:n, :], in1=sh1[0:n, :], op=ALU.add),
                 reads=[b_h32, b_sh1], writes=[b_hb])
            for c in range(8):
                P.op("pe", lambda e, c=c: e.transpose(out=ps_tr[:, c, 0:n], in_=hb[0:n, c * 128:(c + 1) * 128],
                                                      identity=idb[0:n, 0:n]),
                     reads=[b_hb, b_idb], writes=[b_ps_tr])
            P.op("act", lambda e: e.copy(out=hT[:, :, 0:n], in_=ps_tr[:, :, 0:n]), reads=[b_ps_tr], writes=[b_hT])
            ncol_b = 256 if kind == "other" else 280
            for c in range(8):
                P.op("pe", lambda e, c=c: e.matmul(ps_a[0:n, :], lhsT=hT[:, c, 0:n], rhs=winb[:, c, 512:1024],
                                                   start=(c == 0), stop=(c == 7)),
                     reads=[b_hT, b_winb], writes=[b_ps_a])
            for c in range(8):
                P.op("pe", lambda e, c=c: e.matmul(ps_b[0:n, 0:ncol_b], lhsT=hT[:, c, 0:n],
                                                   rhs=winb[:, c, 1024:1024 + ncol_b],
                                                   start=(c == 0), stop=(c == 7)),
                     reads=[b_hT, b_winb], writes=[b_ps_b])
            P.op("dve", lambda e: e.tensor_copy(out=kv_t[0:n, 0:512], in_=ps_a[0:n, :]), reads=[b_ps_a], writes=[b_kv])
            P.op("act", lambda e: e.copy(out=kv_t[0:n, 512:512 + ncol_b], in_=ps_b[0:n, 0:ncol_b]),
                 reads=[b_ps_b], writes=[b_kv])
            dst = o_kv_s if kind == "sample" else o_kv_p[i * 128:(i + 1) * 128, :]
            P.dma("pool", lambda e: e.dma_start(out=dst, in_=kv_t[0:n, 0:768]), reads=[b_kv], final=True)
            if kind == "sample":
                P.op("dve", lambda e: e.tensor_copy(out=kvS[:], in_=kv_t[0:NS, 0:768]), reads=[b_kv], writes=[b_kvS])
                P.op("act", lambda e: e.activation(out=gatesS[:], in_=kv_t[0:NS, 768:792], func=AF.Sigmoid), reads=[b_kv], writes=[b_gatesS])
                for c in range(8):
                    P.op("pe", lambda e, c=c: e.matmul(ps_a[0:NS, :], lhsT=hT[:, c, 0:NS], rhs=winb[:, c, 0:512], start=(c == 0), stop=(c == 7)),
                         reads=[b_hT, b_winb], writes=[b_ps_a])
                P.op("dve", lambda e: e.tensor_copy(out=qtmS[:], in_=ps_a[0:NS, :]), reads=[b_ps_a], writes=[b_qtmS])
                for g in range(4):
                    for c in range(8):
                        P.op("pe", lambda e, c=c, g=g: e.matmul(ps_q[:, g, 0:NS], lhsT=winb[:, c, g * 128:(g + 1) * 128], rhs=hT[:, c, 0:NS],
                                                                start=(c == 0), stop=(c == 7)),
                             reads=[b_hT, b_winb], writes=[b_ps_q])
                P.op("dve", lambda e: e.memset(QbdAll[:], 0.0), writes=[b_QbdAll])
                P.op("dve", lambda e: e.tensor_copy(out=QbdAll[0:64, :, 0:4], in_=ps_q[0:64, :, 0:NS].rearrange("p g b -> p b g")),
                     reads=[b_ps_q], writes=[b_QbdAll])
                P.op("dve", lambda e: e.tensor_copy(out=QbdAll[64:128, :, 4:8], in_=ps_q[64:128, :, 0:NS].rearrange("p g b -> p b g")),
                     reads=[b_ps_q], writes=[b_QbdAll])
                return kv_t, b_kv
            if 'V' not in SKIP:
              P.op("pool", lambda e: e.tensor_copy(out=Vs[:, i, :, 0:64],
                                                 in_=kv_t[:, 384:512].rearrange("p (k d) -> p k d", k=2)),
                 reads=[b_kv], writes=[b_Vs])
            wslot = None
            if kind == "own":
                wslot = 4 + i
            elif i >= 28:
                wslot = i - 28
            if wslot is not None and 'V' not in SKIP:
                P.op("pool", lambda e: e.tensor_copy(out=Vw[:, wslot, :, 0:64],
                                                     in_=kv_t[:, 640:768].rearrange("p (k d) -> p k d", k=2)),
                     reads=[b_kv], writes=[b_Vw])
            if kind == "own":
                P.op("act", lambda e: e.activation(out=gates[:, i, :], in_=kv_t[:, 768:792], func=AF.Sigmoid),
                     reads=[b_kv], writes=[b_gates])
            if 'CH' in SKIP:
                return kv_t, b_kv
            for j, c0 in enumerate((512, 640, 768, 1024)):
                for c in range(8):
                    P.op("pe", lambda e, c=c, j=j, c0=c0: e.matmul(ps_kv[:, j, :], lhsT=winb[:, c, c0:c0 + 128],
                                                                   rhs=hT[:, c, :], start=(c == 0), stop=(c == 7)),
                         reads=[b_hT, b_winb], writes=[b_ps_kv])
            tsl = slice(i * 128, (i + 1) * 128)
            P.op("dve", lambda e: e.tensor_copy(out=XTc[:, :, tsl], in_=ps_kv[:, 0:2, :]), reads=[b_ps_kv], writes=[b_XTc])
            if i == 0:
                P.op("dve", lambda e: e.tensor_copy(out=XTc[:, :, SEQ:SEQ + 32], in_=ps_kv[:, 0:2, 0:32]),
                     reads=[b_ps_kv], writes=[b_XTc])
            if 'KE0' not in SKIP:
                P.op("dve", lambda e: e.tensor_copy(out=KE[0][0][0:64, tsl], in_=ps_kv[0:64, 2, :]), reads=[b_ps_kv], writes=[KE[0][1]])
            if 'KE1' not in SKIP:
                P.op("dve", lambda e: e.tensor_copy(out=KE[1][0][64:128, tsl], in_=ps_kv[64:128, 2, :]), reads=[b_ps_kv], writes=[KE[1][1]])
            if wslot is not None:
                wsl = slice(wslot * 128, (wslot + 1) * 128)
                P.op("dve", lambda e: e.tensor_copy(out=KwT[:, wsl], in_=ps_kv[:, 3, :]), reads=[b_ps_kv], writes=[b_KwT])
            if kind == "own":
                for g in range(4):
                    for c in range(8):
                        P.op("pe", lambda e, c=c, g=g: e.matmul(ps_q[:, g, :], lhsT=winb[:, c, g * 128:(g + 1) * 128],
                                                                rhs=hT[:, c, :], start=(c == 0), stop=(c == 7)),
                             reads=[b_hT, b_winb], writes=[b_ps_q])
                P.op("dve", lambda e: e.tensor_copy(out=qtmp[:], in_=ps_q[:]), reads=[b_ps_q], writes=[b_qtmp])
                for k in range(2):
                    P.dma("pool", lambda e, k=k: e.dma_start(
                        out=qT_d[k, i, :, :], in_=qtmp[k * 64:(k + 1) * 64, :, :].rearrange("p a b -> p (a b)")),
                        reads=[b_qtmp], writes=[b_qT_d])
            if (kind == "own" or i == 31) and 'GLU' not in SKIP:
                glu_part(i, kind)
            return kv_t, b_kv

        for i in range(16, 32):
            token_tile(i, "other")
        if upto == 'B2':
            P.build(st)
            return nc
        for i in range(16):
            token_tile(i, "own")
        if upto == 'B3':
            P.build(st)
            return nc

        uo, b_uo = C.al("uo", [32, 512], F32)
        for j in range(4):
            P.op("pe", lambda e, j=j: e.transpose(out=ps_g[0:30, j * 128:(j + 1) * 128], in_=u32[:, j, 0:30],
                                                  identity=idf[:]),
                 reads=[b_u32, b_idf], writes=[b_ps_g])
        P.op("dve", lambda e: e.tensor_copy(out=uo[0:30, :], in_=ps_g[0:30, :]), reads=[b_ps_g], writes=[b_uo])
        P.dma("pool", lambda e: e.dma_start(out=o_conv_p, in_=uo[0:30, :]), reads=[b_uo], final=True)

        kv_s_t, b_kv_s = token_tile(32, "sample")
        hT, b_hT = hTs[0]
        us, b_us = C.al("us", [NS, 512], F32)
        sgs, b_sgs = C.al("sgs", [NS, 512], F32)
        for c in range(8):
            P.op("pe", lambda e, c=c: e.matmul(ps_a[0:NS, :], lhsT=hT[:, c, 0:NS], rhs=winb[:, c, 1304:1816],
                                               start=(c == 0), stop=(c == 7)),
                 reads=[b_hT, b_winb], writes=[b_ps_a])
        for c in range(8):
            P.op("pe", lambda e, c=c: e.matmul(ps_b[0:NS, :], lhsT=hT[:, c, 0:NS], rhs=winb[:, c, 1816:2328],
                                               start=(c == 0), stop=(c == 7)),
                 reads=[b_hT, b_winb], writes=[b_ps_b])
        P.op("act", lambda e: e.activation(out=sgs[:], in_=ps_b[0:NS, :], func=AF.Sigmoid), reads=[b_ps_b], writes=[b_sgs])
        P.op("dve", lambda e: e.tensor_tensor(out=us[:], in0=ps_a[0:NS, :], in1=sgs[:], op=ALU.mult),
             reads=[b_ps_a, b_sgs], writes=[b_us])
        P.op("dve", lambda e: e.tensor_copy(out=usS[:], in_=us[:]), reads=[b_us], writes=[b_usS])
        P.dma("pool", lambda e: e.dma_start(out=o_win_s[:, 0:511, :], in_=state_win[:, 1:512, :]), final=True)
        P.dma("pool", lambda e: e.dma_start(out=o_win_s[:, 511, :], in_=kv_s_t[0:NS, 512:768]), reads=[b_kv_s], final=True)
        P.dma("pool", lambda e: e.dma_start(out=o_conv_s[:, 0:29, :], in_=state_conv[:, 1:30, :]), final=True)
        P.dma("pool", lambda e: e.dma_start(out=o_conv_s[:, 29, :], in_=us[:]), reads=[b_us], final=True)

        P.barrier()
        C.reset(m_persist)
        catT, b_catT = C.al("catT", [128, 8, NOWN], BF16)
        m_persist2 = C.mark()

        if upto == 'B':
            P.build(st)
            return nc
        uTb, b_uTb = C.al("uTb", [128, 4, 30 + NOWN], BF16)
        wdw, b_wdw = C.al("wdw", [128, 4, 31], F32)
        bdw, b_bdw = C.al("bdw", [128, 4], F32)
        diag, b_diag = C.al("diag", [128, 4, 31, 128], BF16)
        lng, b_lng = C.al("lng", [128, 512], F32)
        lnb, b_lnb = C.al("lnb", [128, 512], F32)
        gco, b_gco = C.al("gco", [128, 512], F32)
        yT, b_yT = C.al("yT", [128, 4, 512], F32)
        zt, b_zt = C.al("zt", [128, 512], F32)
        zj, b_zj = C.al("zj", [128, 512], F32)
        zb, b_zb = C.al("zb", [128, 512], BF16)
        st6, b_st6 = C.al("st6", [128, 6], F32)
        mv, b_mv = C.al("mv", [128, 2], F32)
        rs2, b_rs2 = C.al("rs2", [128, 1], F32)
        ss2, b_ss2 = C.al("ss2", [128, 1], F32)
        for j in range(4):
            P.dma("sp", lambda e, j=j: e.dma_start(out=uTb[:, j, :], in_=u_d[:, j, :]), reads=[b_u_d], writes=[b_uTb])
        P.dma("sp", lambda e: e.dma_start(out=wdw[:], in_=wdwT), writes=[b_wdw])
        P.dma("sp", lambda e: e.dma_start(out=bdw[:], in_=bdwT), writes=[b_bdw])
        P.dma("sp", lambda e: e.dma_start(out=lng[:], in_=conv_ln_g.partition_broadcast(128)), writes=[b_lng])
        P.dma("sp", lambda e: e.dma_start(out=lnb[:], in_=conv_ln_b.partition_broadcast(128)), writes=[b_lnb])
        P.dma("sp", lambda e: e.dma_start(out=gco[:], in_=g_conv_out.partition_broadcast(128)), writes=[b_gco])
        for j in range(4):
            P.op("dve", lambda e, j=j: e.tensor_tensor(out=diag[:, j, :, :],
                                                       in0=idb[:].unsqueeze(1).to_broadcast([128, 31, 128]),
                                                       in1=wdw[:, j, :].unsqueeze(2).to_broadcast([128, 31, 128]),
                                                       op=ALU.mult),
                 reads=[b_idb, b_wdw], writes=[b_diag])
        ps_tm, b_ps_tm = ps_g, b_ps_g
        for G in range(4):
            for j in range(4):
                psx, b_psx = [(ps_a, b_ps_a), (ps_b, b_ps_b)][j % 2]
                for k in range(31):
                    P.op("pe", lambda e, j=j, k=k, psx=psx, G=G: e.matmul(psx[:], lhsT=diag[:, j, k, :],
                                                                          rhs=uTb[:, j, G * 512 + k:G * 512 + k + 512],
                                                                          start=(k == 0), stop=(k == 30)),
                         reads=[b_diag, b_uTb], writes=[b_psx])
                P.op("act", lambda e, j=j, psx=psx: e.activation(out=yT[:, j, :], in_=psx[:], func=AF.Identity,
                                                                 bias=bdw[:, j:j + 1]),
                     reads=[b_psx, b_bdw], writes=[b_yT])
            for t in range(4):
                tile_i = G * 4 + t
                for j in range(4):
                    P.op("pe", lambda e, j=j, t=t: e.transpose(out=ps_tm[:, j * 128:(j + 1) * 128],
                                                               in_=yT[:, j, t * 128:(t + 1) * 128], identity=idf[:]),
                         reads=[b_yT, b_idf], writes=[b_ps_tm])
                P.op("dve", lambda e: e.bn_stats(out=st6[:], in_=ps_tm[:]), reads=[b_ps_tm], writes=[b_st6])
                P.op("dve", lambda e: e.bn_aggr(out=mv[:], in_=st6[:]), reads=[b_st6], writes=[b_mv])
                P.op("act", lambda e: e.activation(out=rs2[:], in_=mv[:, 1:2], func=AF.Sqrt, bias=epsT[:]),
                     reads=[b_mv, b_eps], writes=[b_rs2])
                P.op("dve", lambda e: e.reciprocal(out=rs2[:], in_=rs2[:]), reads=[b_rs2], writes=[b_rs2])
                P.op("dve", lambda e: e.tensor_scalar(out=zt[:], in0=ps_tm[:], scalar1=mv[:, 0:1], scalar2=rs2[:, 0:1],
                                                      op0=ALU.subtract, op1=ALU.mult),
                     reads=[b_ps_tm, b_mv, b_rs2], writes=[b_zt])
                P.op("pool", lambda e: e.tensor_tensor(out=zt[:], in0=zt[:], in1=lng[:], op=ALU.mult),
                     reads=[b_zt, b_lng], writes=[b_zt])
                P.op("pool", lambda e: e.tensor_tensor(out=zt[:], in0=zt[:], in1=lnb[:], op=ALU.add),
                     reads=[b_zt, b_lnb], writes=[b_zt])
                P.op("act", lambda e: e.activation(out=zt[:], in_=zt[:], func=AF.Silu), reads=[b_zt], writes=[b_zt])
                if stage == 2:
                    P.dma("pool", lambda e, tile_i=tile_i: e.dma_start(out=dbg_conv[tile_i * 128:(tile_i + 1) * 128, :], in_=zt[:]),
                          reads=[b_zt], final=True)
                P.op("act", lambda e: e.activation(out=zj[:], in_=zt[:], func=AF.Square, accum_out=ss2[:]),
                     reads=[b_zt], writes=[b_zj, b_ss2])
                P.op("act", lambda e: e.activation(out=ss2[:], in_=ss2[:], func=AF.Sqrt, scale=1.0 / 512, bias=epsT[:]),
                     reads=[b_ss2, b_eps], writes=[b_ss2])
                P.op("dve", lambda e: e.reciprocal(out=ss2[:], in_=ss2[:]), reads=[b_ss2], writes=[b_ss2])
                P.op("dve", lambda e: e.scalar_tensor_tensor(out=zb[:], in0=zt[:], scalar=ss2[:, 0:1], in1=gco[:],
                                                             op0=ALU.mult, op1=ALU.mult),
                     reads=[b_zt, b_ss2, b_gco], writes=[b_zb])
                for j in range(4):
                    P.op("pe", lambda e, j=j: e.transpose(out=ps_tr[:, j, :], in_=zb[:, j * 128:(j + 1) * 128], identity=idb[:]),
                         reads=[b_zb, b_idb], writes=[b_ps_tr])
                P.op("act", lambda e, tile_i=tile_i: e.copy(out=catT[:, 4:8, tile_i * 128:(tile_i + 1) * 128], in_=ps_tr[:, 0:4, :]),
                     reads=[b_ps_tr], writes=[b_catT])
        P.barrier()
        C.reset(m_persist2)

        if upto == 'C':
            P.build(st)
            return nc
        w1b, b_w1b = C.al("w1b", [128, 2, 32, 128], BF16)
        w1s, b_w1s = C.al("w1s", [128, 16, 128], F32)
        pos2, b_pos2 = C.al("pos2", [128, 2, 32], F32)
        pos2b, b_pos2b = C.al("pos2b", [128, 2, 32], BF16)
        posb, b_posb = C.al("posb", [128, 2], F32)
        w2s, b_w2s = C.al("w2s", [128, 2, 128], F32)
        w2b, b_w2b = C.al("w2b", [128, 2, 128], BF16)
        hidT, b_hidT = C.al("hidT", [128, 2, 256], BF16)
        ovs, b_ovs = C.al("ovs", [128, 2, 64], F32)
        for c in range(2):
            for hh in range(2):
                P.dma("sp", lambda e, c=c, hh=hh: e.dma_start(out=w1s[:], in_=w1bd[:, c, hh * 16:(hh + 1) * 16, :]), writes=[b_w1s])
                P.op("pool", lambda e, c=c, hh=hh: e.tensor_copy(out=w1b[:, c, hh * 16:(hh + 1) * 16, :], in_=w1s[:]),
                     reads=[b_w1s], writes=[b_w1b])
        P.dma("sp", lambda e: e.dma_start(out=pos2[:], in_=posT2), writes=[b_pos2])
        P.dma("sp", lambda e: e.dma_start(out=w2s[:], in_=w2bd), writes=[b_w2s])
        P.dma("sp", lambda e: e.dma_start(out=ovs[:], in_=ovt), writes=[b_ovs])
        P.op("dve", lambda e: e.tensor_copy(out=pos2b[:], in_=pos2[:]), reads=[b_pos2], writes=[b_pos2b])
        P.op("dve", lambda e: e.tensor_copy(out=w2b[:], in_=w2s[:]), reads=[b_w2s], writes=[b_w2b])
        for c in range(2):
            for s in range(32):
                P.op("pe", lambda e, c=c, s=s: e.matmul(ps_g[:, c:c + 1], lhsT=w1b[:, c, s, :], rhs=pos2b[:, c, s:s + 1],
                                                        start=(s == 0), stop=(s == 31)),
                     reads=[b_w1b, b_pos2b], writes=[b_ps_g])
        P.op("dve", lambda e: e.tensor_copy(out=posb[:], in_=ps_g[:, 0:2]), reads=[b_ps_g], writes=[b_posb])
        for c in range(2):
            psx, b_psx = [(ps_a, b_ps_a), (ps_b, b_ps_b)][c]
            for s in range(32):
                P.op("pe", lambda e, c=c, s=s, psx=psx: e.matmul(psx[:, 0:256], lhsT=w1b[:, c, s, :],
                                                                 rhs=XTc[:, c, s:s + 4096:16],
                                                                 start=(s == 0), stop=(s == 31)),
                     reads=[b_w1b, b_XTc], writes=[b_psx])
            P.op("act", lambda e, c=c, psx=psx: e.activation(out=hidT[:, c, :], in_=psx[:, 0:256], func=AF.Silu,
                                                             bias=posb[:, c:c + 1]),
                 reads=[b_psx, b_posb], writes=[b_hidT])
        P.op("pe", lambda e: e.matmul(ps_c[:, 0:256], lhsT=w2b[:, 0, :], rhs=hidT[:, 0, :], start=True, stop=True),
             reads=[b_w2b, b_hidT], writes=[b_ps_c])
        P.op("dve", lambda e: e.tensor_copy(out=kcT[:], in_=ps_c[:, 0:256]), reads=[b_ps_c], writes=[b_kcT])
        for ch in range(2):
            P.op("pe", lambda e, ch=ch: e.matmul(ps_d[:, ch * 128:(ch + 1) * 128], lhsT=hidT[:, 1, ch * 128:(ch + 1) * 128],
                                                 rhs=w2b[:, 1, :], start=True, stop=True),
                 reads=[b_w2b, b_hidT], writes=[b_ps_d])
        P.op("dve", lambda e: e.tensor_copy(out=vcx[:, :, :, 0:64],
                                            in_=ps_d[:, 0:256].rearrange("p (c k d) -> p c k d", c=2, k=2)),
             reads=[b_ps_d], writes=[b_vcx])
        P.op("dve", lambda e: e.memset(vcx[:, :, :, 64:65], 1.0), writes=[b_vcx])
        for k in range(2):
            P.op("dve", lambda e, k=k: e.tensor_copy(out=vcx[:, :, k, 65:129], in_=ovs[:]), reads=[b_ovs], writes=[b_vcx])
        P.barrier()
        C.reset(m_persist2)

        if upto == 'D':
            P.build(st)
            return nc
        qrow_bc, b_qrow = C.al("qrow_bc", [128, NOWN], F32)
        pes, b_pes = C.al("pes", [128, 2], F32)
        blkn, b_blkn = C.al("blkn", [128, 64], F32)
        iszb, b_iszb = C.al("iszb", [128, 64], F32)
        cur, b_cur = C.al("cur", [128, 16], F32)
        trs, b_trs = C.al("trs", [128, 2, 128], F32)
        tdb, b_tdb = C.al("tdb", [128, 4, 128], BF16)
        tlb, b_tlb = C.al("tlb", [128, 4, 128], BF16)
        nvs, b_nvs = C.al("nvs", [1, 512], F32)
        nvr, b_nvr = C.al("nvr", [1, 512], BF16)
        ones1, b_ones1 = C.al("ones1", [1, 128], BF16)
        gao, b_gao = C.al("gao", [128, 512], F32)
        P.dma("sp", lambda e: e.dma_start(out=qrow_bc[:], in_=qrow.partition_broadcast(128)), writes=[b_qrow])
        P.dma("sp", lambda e: e.dma_start(out=pes[:], in_=pe_slot), writes=[b_pes])
        P.dma("sp", lambda e: e.dma_start(out=blkn[:], in_=blknat.partition_broadcast(128)), writes=[b_blkn])
        P.dma("sp", lambda e: e.dma_start(out=iszb[:], in_=isz.partition_broadcast(128)), writes=[b_iszb])
        P.dma("sp", lambda e: e.dma_start(out=cur[:], in_=curt), writes=[b_cur])
        P.dma("sp", lambda e: e.dma_start(out=trs[:, 0, :], in_=tridiag), writes=[b_trs])
        P.dma("sp", lambda e: e.dma_start(out=trs[:, 1, :], in_=trilo), writes=[b_trs])
        P.dma("sp", lambda e: e.dma_start(out=nvs[:], in_=nvrow), writes=[b_nvs])
        P.dma("sp", lambda e: e.dma_start(out=gao[:], in_=g_attn_out.partition_broadcast(128)), writes=[b_gao])
        P.op("dve", lambda e: e.tensor_copy(out=tdb[:], in_=trs[:, 0, :].unsqueeze(1).to_broadcast([128, 4, 128])),
             reads=[b_trs], writes=[b_tdb])
        P.op("dve", lambda e: e.tensor_copy(out=tlb[:], in_=trs[:, 1, :].unsqueeze(1).to_broadcast([128, 4, 128])),
             reads=[b_trs], writes=[b_tlb])
        P.op("dve", lambda e: e.tensor_copy(out=nvr[:], in_=nvs[:]), reads=[b_nvs], writes=[b_nvr])
        P.op("dve", lambda e: e.memset(ones1[:], 1.0), writes=[b_ones1])
        tdb_f = tdb.rearrange("p g q -> p (g q)")
        tlb_f = tlb.rearrange("p g q -> p (g q)")

        QBt = [[C.al("QBt%d_%d" % (r, k), [128, 512], BF16) for k in range(2)] for r in range(2)]
        cmask, b_cmask = C.al("cmask", [128, 2, 128], BF16)
        vmask, b_vmask = C.al("vmask", [128, 64], F32)
        fmask, b_fmask = C.al("fmask", [128, 64], F32)
        ndt, b_ndt = C.al("ndt", [128, 64], F32)
        pc, b_pc = C.al("pc", [128, 2, 512], BF16)
        pTs = [C.al("pT%d" % i, [128, 512], BF16) for i in range(3)]
        rden, b_rden = C.al("rden", [128, 4], F32)
        coef, b_coef = C.al("coef", [128, 4], F32)
        imp, b_imp = C.al("imp", [128, 64], F32)
        impF, b_impF = C.al("impF", [128, 64], F32)
        wk, b_wk = C.al("wk", [128, 64], F32)
        m8, b_m8 = C.al("m8", [128, 16], F32)
        selv, b_selv = C.al("selv", [128, 64], F32)
        Bq, b_Bq = C.al("Bq", [128, 128], BF16)
        oats = [C.al("oat%d" % i, [128, 512], F32) for i in range(2)]
        rden2, b_rden2 = C.al("rden2", [128, 4], F32)
        coef2, b_coef2 = C.al("coef2", [128, 4], F32)
        otmp, b_otmp = C.al("otmp", [128, 4, 64], F32)
        oj, b_oj = C.al("oj", [128, 512], F32)
        ob, b_ob = C.al("ob", [128, 512], BF16)
        ss3, b_ss3 = C.al("ss3", [128, 1], F32)
        ps_S = [(ps_a, b_ps_a), (ps_b, b_ps_b)]
        ps_oc = [(ps_c[:, 0:258].rearrange("p (g n) -> p g n", g=2), b_ps_c),
                 (ps_d[:, 0:258].rearrange("p (g n) -> p g n", g=2), b_ps_d)]
        ps_os = (ps_e[:, 0:260].rearrange("p (g n) -> p g n", g=4), b_ps_e)
        ps_ow = (ps_f[:, 0:260].rearrange("p (g n) -> p g n", g=4), b_ps_f)
        cnt = {"s": 0, "p": 0}

        def next_S():
            r = ps_S[cnt["s"] % 2]
            cnt["s"] += 1
            return r

        def next_pT():
            r = pTs[cnt["p"] % 3]
            cnt["p"] += 1
            return r

        def combine(k, br, O, b_O, first):
            P.op("dve", lambda e: e.tensor_scalar(out=rden[:], in0=O[:, :, 64], scalar1=1e-30, scalar2=None, op0=ALU.add),
                 reads=[b_O], writes=[b_rden])
            P.op("dve", lambda e: e.reciprocal(out=rden[:], in_=rden[:]), reads=[b_rden], writes=[b_rden])
            return

        n_qt = 16 if stage >= 2 else 0
        def attn_pre(qt):
            (oat, b_oat) = oats[qt % 2]
            qsl = slice(qt * 128, (qt + 1) * 128)
            QBk = QBt[qt % 2]
            for k in range(2):
                kh = slice(k * 64, (k + 1) * 64)
                P.dma("sp", lambda e, k=k, kh=kh, QBk=QBk: e.dma_start(out=QBk[k][0][kh, :], in_=qT_d[k, qt, :, :]),
                      reads=[b_qT_d], writes=[QBk[k][1]])
            for ch in range(2):
                P.op("dve", lambda e, ch=ch: e.tensor_scalar(out=cmask[:, ch, :], in0=qrow_bc[:, qsl], scalar1=pes[:, ch:ch + 1],
                                                             scalar2=None, op0=ALU.is_ge),
                     reads=[b_qrow, b_pes], writes=[b_cmask])
            P.op("dve", lambda e: e.tensor_scalar(out=ndt[:], in0=blkn[:], scalar1=cur[:, qt:qt + 1], scalar2=None,
                                                  op0=ALU.subtract),
                 reads=[b_blkn, b_cur], writes=[b_ndt])
            P.op("dve", lambda e: e.tensor_scalar(out=vmask[:], in0=ndt[:], scalar1=0.0, scalar2=None, op0=ALU.is_le),
                 reads=[b_ndt], writes=[b_vmask])
            P.op("dve", lambda e: e.tensor_scalar(out=fmask[:], in0=ndt[:], scalar1=-1.0, scalar2=None, op0=ALU.is_ge),
                 reads=[b_ndt], writes=[b_fmask])
            P.op("dve", lambda e: e.tensor_tensor(out=fmask[:], in0=fmask[:], in1=iszb[:], op=ALU.max),
                 reads=[b_fmask, b_iszb], writes=[b_fmask])
            P.op("dve", lambda e: e.tensor_tensor(out=fmask[:], in0=fmask[:], in1=vmask[:], op=ALU.mult),
                 reads=[b_fmask, b_vmask], writes=[b_fmask])
            def cmp_k(k):
                kh = slice(k * 64, (k + 1) * 64)
                (QB, b_QB) = QBk[k]
                for ch in range(2):
                    (pS, b_pS) = next_S()
                    P.op("pe", lambda e, ch=ch, pS=pS, QB=QB, kh=kh: e.matmul(pS[:], lhsT=kcT[kh, ch * 128:(ch + 1) * 128],
                                                                              rhs=QB[kh, :], start=True, stop=True),
                         reads=[b_kcT, b_QB], writes=[b_pS])
                    P.op("act", lambda e, ch=ch, pS=pS: e.activation(out=pc[:, ch, :], in_=pS[:], func=AF.Exp, scale=SCALE),
                         reads=[b_pS], writes=[b_pc])
                    P.op("dve", lambda e, ch=ch: e.tensor_tensor(
                        out=pc[:, ch, :].rearrange("p (g q) -> p g q", g=4),
                        in0=pc[:, ch, :].rearrange("p (g q) -> p g q", g=4),
                        in1=cmask[:, ch, :].unsqueeze(1).to_broadcast([128, 4, 128]), op=ALU.mult),
                        reads=[b_pc, b_cmask], writes=[b_pc])
                for g in range(4):
                    (O2, b_O2) = ps_oc[g // 2]
                    for ch in range(2):
                        P.op("pe", lambda e, g=g, ch=ch, O2=O2: e.matmul(O2[:, g % 2, :], lhsT=pc[:, ch, g * 128:(g + 1) * 128],
                                                                         rhs=vcx[:, ch, k, 0:129], start=(ch == 0), stop=(ch == 1)),
                             reads=[b_pc, b_vcx], writes=[b_O2])
                for hh in range(2):
                    (O2, b_O2) = ps_oc[hh]
                    P.op("dve", lambda e, hh=hh, O2=O2: e.tensor_scalar(out=rden[:, 2 * hh:2 * hh + 2], in0=O2[:, :, 64],
                                                                        scalar1=1e-30, scalar2=None, op0=ALU.add),
                         reads=[b_O2], writes=[b_rden])
                P.op("dve", lambda e: e.reciprocal(out=rden[:], in_=rden[:]), reads=[b_rden], writes=[b_rden])
                for g in range(4):
                    (O2, b_O2) = ps_oc[g // 2]
                    if g == 0:
                        P.op("dve", lambda e, O2=O2: e.tensor_scalar(out=imp[:], in0=O2[:, 0, 65:129], scalar1=rden[:, 0:1],
                                                                     scalar2=None, op0=ALU.mult),
                             reads=[b_O2, b_rden], writes=[b_imp])
                    else:
                        P.op("dve", lambda e, g=g, O2=O2: e.scalar_tensor_tensor(out=imp[:], in0=O2[:, g % 2, 65:129],
                                                                                 scalar=rden[:, g:g + 1], in1=imp[:],
                                                                                 op0=ALU.mult, op1=ALU.add),
                             reads=[b_O2, b_rden, b_imp], writes=[b_imp])
                P.op("dve", lambda e, k=k: e.tensor_tensor(
                    out=coef[:], in0=rden[:],
                    in1=gates[:, qt, k * 12:(k + 1) * 12].rearrange("p (g b) -> p g b", b=3)[:, :, 0], op=ALU.mult),
                    reads=[b_rden, b_gates], writes=[b_coef])
                for hh in range(2):
                    (O2, b_O2) = ps_oc[hh]
                    h0 = k * 4 + hh * 2
                    P.op("dve", lambda e, hh=hh, O2=O2, h0=h0: e.tensor_tensor(
                        out=oat[:, h0 * 64:(h0 + 2) * 64].rearrange("p (g d) -> p g d", g=2), in0=O2[:, :, 0:64],
                        in1=coef[:, 2 * hh:2 * hh + 2].unsqueeze(2).to_broadcast([128, 2, 64]), op=ALU.mult),
                        reads=[b_O2, b_coef], writes=[b_oat])
                P.op("dve", lambda e: e.tensor_tensor(out=impF[:], in0=imp[:], in1=vmask[:], op=ALU.mult),
                     reads=[b_imp, b_vmask], writes=[b_impF])
                P.op("dve", lambda e: e.scalar_tensor_tensor(out=impF[:], in0=impF[:], scalar=-1.0, in1=vmask[:],
                                                             op0=ALU.add, op1=ALU.add),
                     reads=[b_impF, b_vmask], writes=[b_impF])
                P.op("dve", lambda e: e.scalar_tensor_tensor(out=impF[:], in0=fmask[:], scalar=1e4, in1=impF[:],
                                                             op0=ALU.mult, op1=ALU.add),
                     reads=[b_impF, b_fmask], writes=[b_impF])
                P.op("dve", lambda e: e.max(out=m8[:, 0:8], in_=impF[:]), reads=[b_impF], writes=[b_m8])
                P.op("dve", lambda e: e.match_replace(out=wk[:], in_to_replace=m8[:, 0:8], in_values=impF[:], imm_value=-2.0),
                     reads=[b_impF, b_m8], writes=[b_wk])
                P.op("dve", lambda e: e.max(out=m8[:, 8:16], in_=wk[:]), reads=[b_wk], writes=[b_m8])
                P.op("dve", lambda e: e.tensor_scalar(out=selv[:], in0=impF[:], scalar1=m8[:, 15:16], scalar2=None, op0=ALU.is_ge),
                     reads=[b_impF, b_m8], writes=[b_selv])
                P.op("dve", lambda e: e.scalar_tensor_tensor(out=selv[:], in0=impF[:], scalar=0.0, in1=selv[:],
                                                             op0=ALU.is_ge, op1=ALU.mult),
                     reads=[b_impF, b_selv], writes=[b_selv])
                bc = slice(64, 128) if k == 0 else slice(0, 64)
                P.op("dve", lambda e, bc=bc: e.tensor_scalar(out=Bq[:, bc], in0=selv[:], scalar1=-NEG, scalar2=NEG,
                                                             op0=ALU.mult, op1=ALU.add),
                     reads=[b_selv], writes=[b_Bq])
            cmp_k(0)
            cmp_k(1)
            if stage == 2:
                P.dma("pool", lambda e: e.dma_start(out=dbg_br[0, qsl, :], in_=oat[:]), reads=[b_oat], final=True)
            P.op("pe", lambda e: e.transpose(out=ps_tr[:, 0, :], in_=Bq[:], identity=idb[:]), reads=[b_Bq, b_idb], writes=[b_ps_tr])
            P.op("dve", lambda e, QBk=QBk: e.tensor_copy(out=QBk[0][0][64:128, :].rearrange("p (g q) -> p g q", g=4),
                                                 in_=ps_tr[64:128, 0, :].unsqueeze(1).to_broadcast([64, 4, 128])),
                 reads=[b_ps_tr], writes=[QBk[0][1]])
            P.op("dve", lambda e, QBk=QBk: e.tensor_copy(out=QBk[1][0][0:64, :].rearrange("p (g q) -> p g q", g=4),
                                                 in_=ps_tr[0:64, 0, :].unsqueeze(1).to_broadcast([64, 4, 128])),
                 reads=[b_ps_tr], writes=[QBk[1][1]])
        def attn_main(qt):
            qsl = slice(qt * 128, (qt + 1) * 128)
            QBk = QBt[qt % 2]
            (oat, b_oat) = oats[qt % 2]
            tiles = []
            for k in range(2):
                kts = list(range(qt + 1)) + list(range(16, 32))
                for idx, kt in enumerate(kts):
                    tiles.append(("s", k, idx, kt, len(kts)))
                for w in range(5):
                    tiles.append(("w", k, w, None, 5))

            def emit_qk(t, pS, b_pS):
                kind, k, idx, kt, n = t
                kh = slice(k * 64, (k + 1) * 64)
                (QB, b_QB) = QBk[k]
                if kind == "s":
                    (KEk, b_KEk) = KE[k]
                    P.op("pe", lambda e: e.matmul(pS[:], lhsT=KEk[:, kt * 128:(kt + 1) * 128], rhs=QB[:, :], start=True, stop=(kt != qt)),
                         reads=[b_KEk, b_QB], writes=[b_pS])
                    if kt == qt:
                        P.op("pe", lambda e: e.matmul(pS[:], lhsT=idb[:], rhs=tdb_f, start=False, stop=True),
                             reads=[b_idb, b_tdb], writes=[b_pS])
                else:
                    w = idx
                    col0 = (qt + w) * 128
                    extra = []
                    if w == 0:
                        extra.append("lo")
                    if w == 4:
                        extra.append("di")
                    if qt + w < 4:
                        extra.append("nv")
                    P.op("pe", lambda e: e.matmul(pS[:], lhsT=KwT[kh, col0:col0 + 128], rhs=QB[kh, :], start=True, stop=(len(extra) == 0)),
                         reads=[b_KwT, b_QB], writes=[b_pS])
                    for xi, x in enumerate(extra):
                        last = (xi == len(extra) - 1)
                        if x == "lo":
                            P.op("pe", lambda e, last=last: e.matmul(pS[:], lhsT=idb[:], rhs=tlb_f, start=False, stop=last),
                                 reads=[b_idb, b_tlb], writes=[b_pS])
                        elif x == "di":
                            P.op("pe", lambda e, last=last: e.matmul(pS[:], lhsT=idb[:], rhs=tdb_f, start=False, stop=last),
                                 reads=[b_idb, b_tdb], writes=[b_pS])
                        else:
                            P.op("pe", lambda e, last=last: e.matmul(pS[:], lhsT=ones1[0:1, :], rhs=nvr[0:1, :], start=False, stop=last),
                                 reads=[b_ones1, b_nvr], writes=[b_pS])

            def emit_pv(t, pS, b_pS):
                kind, k, idx, kt, n = t
                (pT, b_pT) = next_pT()
                P.op("act", lambda e: e.activation(out=pT[:], in_=pS[:], func=AF.Exp, scale=SCALE), reads=[b_pS], writes=[b_pT])
                if kind == "s":
                    (Os, b_Os) = ps_os
                    for g in range(4):
                        P.op("pe", lambda e, g=g: e.matmul(Os[:, g, :], lhsT=pT[:, g * 128:(g + 1) * 128], rhs=Vs[:, kt, k, 0:65],
                                                           start=(idx == 0 and g == 0), stop=(idx == n - 1 and g == 3)),
                             reads=[b_pT, b_Vs], writes=[b_Os])
                else:
                    (Ow, b_Ow) = ps_ow
                    w = idx
                    for g in range(4):
                        P.op("pe", lambda e, g=g: e.matmul(Ow[:, g, :], lhsT=pT[:, g * 128:(g + 1) * 128], rhs=Vw[:, qt + w, k, 0:65],
                                                           start=(w == 0 and g == 0), stop=(w == 4 and g == 3)),
                             reads=[b_pT, b_Vw], writes=[b_Ow])
                    if w == 4:
                        combine_k(k)

            def combine_k(k):
                for br, (O, b_O) in ((1, ps_os), (2, ps_ow)):
                    P.op("dve", lambda e, O=O: e.tensor_scalar(out=rden2[:], in0=O[:, :, 64], scalar1=1e-30, scalar2=None, op0=ALU.add),
                         reads=[b_O], writes=[b_rden2])
                    P.op("dve", lambda e: e.reciprocal(out=rden2[:], in_=rden2[:]), reads=[b_rden2], writes=[b_rden2])
                    P.op("dve", lambda e, br=br: e.tensor_tensor(
                        out=coef2[:], in0=rden2[:],
                        in1=gates[:, qt, k * 12:(k + 1) * 12].rearrange("p (g b) -> p g b", b=3)[:, :, br], op=ALU.mult),
                        reads=[b_rden2, b_gates], writes=[b_coef2])
                    P.op("dve", lambda e, O=O: e.tensor_tensor(out=otmp[:], in0=O[:, :, 0:64],
                                                               in1=coef2[:].unsqueeze(2).to_broadcast([128, 4, 64]), op=ALU.mult),
                         reads=[b_O, b_coef2], writes=[b_otmp])
                    P.op("pool", lambda e: e.tensor_tensor(out=oat[:, k * 256:(k + 1) * 256], in0=oat[:, k * 256:(k + 1) * 256],
                                                           in1=otmp[:].rearrange("p g d -> p (g d)"), op=ALU.add),
                         reads=[b_oat, b_otmp], writes=[b_oat])

            prev = None
            for t in tiles:
                (pS, b_pS) = next_S()
                emit_qk(t, pS, b_pS)
                if prev is not None:
                    emit_pv(*prev)
                prev = (t, pS, b_pS)
            emit_pv(*prev)
            if stage == 2:
                P.dma("pool", lambda e: e.dma_start(out=dbg_attn[qsl, :], in_=oat[:]), reads=[b_oat], final=True)
            P.op("act", lambda e: e.activation(out=oj[:], in_=oat[:], func=AF.Square, accum_out=ss3[:]),
                 reads=[b_oat], writes=[b_oj, b_ss3])
            P.op("act", lambda e: e.activation(out=ss3[:], in_=ss3[:], func=AF.Sqrt, scale=1.0 / 512, bias=epsT[:]),
                 reads=[b_ss3, b_eps], writes=[b_ss3])
            P.op("dve", lambda e: e.reciprocal(out=ss3[:], in_=ss3[:]), reads=[b_ss3], writes=[b_ss3])
            P.op("dve", lambda e: e.scalar_tensor_tensor(out=ob[:], in0=oat[:], scalar=ss3[:, 0:1], in1=gao[:],
                                                         op0=ALU.mult, op1=ALU.mult),
                 reads=[b_oat, b_ss3, b_gao], writes=[b_ob])
            for j in range(4):
                P.op("pe", lambda e, j=j: e.transpose(out=ps_tr[:, 4 + j, :], in_=ob[:, j * 128:(j + 1) * 128], identity=idb[:]),
                     reads=[b_ob, b_idb], writes=[b_ps_tr])
            P.op("act", lambda e: e.copy(out=catT[:, 0:4, qsl], in_=ps_tr[:, 4:8, :]), reads=[b_ps_tr], writes=[b_catT])
        if n_qt:
            attn_pre(0)
        for qt in range(n_qt):
            if qt + 1 < n_qt:
                attn_pre(qt + 1)
            attn_main(qt)
        P.barrier()
        C.reset(m_persist2)


        if HAVE_SAMPLE:
            C.reset(m_samp)
            w2b2, b_w2b2 = C.al("w2b2", [128, 2, 128], BF16)
            posb2, b_posb2 = C.al("posb2", [128, 2], F32)
            XTs, b_XTs = C.al("XTs", [128, 2, 16, 512], BF16)
            Eg, b_Eg = C.al("Eg", [128, 4, 128], BF16)
            ovS, b_ovS = C.al("ovS", [128, 4, 128], F32)
            idx, b_idx = C.al("idx", [128, NS * 4], I32)
            pm8, b_pm8 = C.al("pm8", [128, NS * 4], F32)
            idxf, b_idxf = C.al("idxf", [128, NS * 4], F32)
            forc, b_forc = C.al("forc", [128, 1], F32)
            fmaskA, b_fmaskA = C.al("fmaskA", [2, 128], F32)
            ones_b, b_ones_b = C.al("ones_b", [128, 128], F32)
            ones2, b_ones2 = C.al("ones2", [2, 128], F32)
            assert C.off <= m_persist, (C.off, m_persist)
            C.reset(m_persist2)
            w1b2, b_w1b2 = C.al("w1b2", [128, 2, 32, 128], BF16)
            ct = [C.al("ct%d" % i, [128, 4096], F32) for i in range(2)]
            hidS, b_hidS = C.al("hidS", [128, 2, 512], BF16)
            kcS, b_kcS = C.al("kcS", [128, 512], BF16)
            vcS, b_vcS = C.al("vcS", [128, 4, 132], BF16)
            pcS, b_pcS = C.al("pcS", [128, 4, 8], BF16)
            pc32, b_pc32 = C.al("pc32", [128, 4, 8], F32)
            denB, b_denB = C.al("denB", [128, 8], F32)
            pnk32, b_pnk32 = C.al("pnk32", [128, 4, 8], F32)
            pnk, b_pnk = C.al("pnk", [128, 4, 2], BF16)
            pnkf, b_pnkf = C.al("pnkf", [128, 4, 2], F32)
            impAs, b_impAs = C.al("impAs", [2, 128], F32)
            wkA, b_wkA = C.al("wkA", [2, 128], F32)
            m8A, b_m8A = C.al("m8A", [2, 16], F32)
            dthr, b_dthr = C.al("dthr", [2, 2], F32)
            selB, b_selB = C.al("selB", [128, 2], F32)
            BselS, b_BselS = C.al("BselS", [128, 8], BF16)
            KsT = [C.al("KsT%d" % i, [128, 4, 128], BF16) for i in range(2)]
            Vsb, b_Vsb = C.al("Vsb", [128, 64, 132], BF16)
            psS, b_psS = C.al("psS", [128, 512], BF16)
            wt, b_wt = C.al("wt", [128, 4, 256], F32)
            KwS, b_KwS = C.al("KwS", [128, 512], BF16)
            Vwb, b_Vwb = C.al("Vwb", [128, 4, 132], BF16)
            pwS, b_pwS = C.al("pwS", [128, 4, 8], BF16)
            resb, b_resb = C.al("resb", [8, 3, 129], F32)
            for c in range(2):
                for hh in range(2):
                    (stg, b_stg) = ct[hh]
                    sv = stg[:, 0:2048].rearrange("p (a b) -> p a b", a=16)
                    P.dma("sp", lambda e, c=c, hh=hh, sv=sv: e.dma_start(out=sv, in_=w1bd[:, c, hh * 16:(hh + 1) * 16, :]), writes=[b_stg])
                    P.op("pool", lambda e, c=c, hh=hh, sv=sv: e.tensor_copy(out=w1b2[:, c, hh * 16:(hh + 1) * 16, :], in_=sv),
                         reads=[b_stg], writes=[b_w1b2])
            (stg, b_stg) = ct[0]
            P.dma("sp", lambda e: e.dma_start(out=stg[:, 0:256].rearrange("p (a b) -> p a b", a=2), in_=w2bd), writes=[b_stg])
            P.op("dve", lambda e: e.tensor_copy(out=w2b2[:], in_=stg[:, 0:256].rearrange("p (a b) -> p a b", a=2)), reads=[b_stg], writes=[b_w2b2])
            P.dma("sp", lambda e: e.dma_start(out=stg[:, 256:320].rearrange("p (a b) -> p a b", a=2), in_=posT2), writes=[b_stg])
            P.op("dve", lambda e: e.tensor_copy(out=hidS[:, 0, 0:64].rearrange("p (a b) -> p a b", a=2),
                                                in_=stg[:, 256:320].rearrange("p (a b) -> p a b", a=2)), reads=[b_stg], writes=[b_hidS])
            for c in range(2):
                for s in range(32):
                    P.op("pe", lambda e, c=c, s=s: e.matmul(ps_g[:, c:c + 1], lhsT=w1b2[:, c, s, :], rhs=hidS[:, 0, c * 32 + s:c * 32 + s + 1],
                                                            start=(s == 0), stop=(s == 31)),
                         reads=[b_w1b2, b_hidS], writes=[b_ps_g])
            P.op("dve", lambda e: e.tensor_copy(out=posb2[:], in_=ps_g[:, 0:2]), reads=[b_ps_g], writes=[b_posb2])
            (stg1, b_stg1) = ct[1]
            P.dma("sp", lambda e: e.dma_start(out=stg1[:, 0:512].rearrange("p (a b) -> p a b", a=4), in_=egtab), writes=[b_stg1])
            P.op("dve", lambda e: e.tensor_copy(out=Eg[:], in_=stg1[:, 0:512].rearrange("p (a b) -> p a b", a=4)), reads=[b_stg1], writes=[b_Eg])
            P.dma("sp", lambda e: e.dma_start(out=stg1[:, 512:1024].rearrange("p (a b) -> p a b", a=4), in_=ovstab), writes=[b_stg1])
            P.op("dve", lambda e: e.tensor_copy(out=ovS[:], in_=stg1[:, 512:1024].rearrange("p (a b) -> p a b", a=4)), reads=[b_stg1], writes=[b_ovS])
            P.dma("sp", lambda e: e.dma_start(out=idx[:], in_=ptrep), writes=[b_idx])
            P.dma("sp", lambda e: e.dma_start(out=pm8[:], in_=pm8rep), writes=[b_pm8])
            P.dma("sp", lambda e: e.dma_start(out=forc[:], in_=forctab), writes=[b_forc])
            P.dma("sp", lambda e: e.dma_start(out=fmaskA[:], in_=fmaskAtab), writes=[b_fmaskA])
            P.op("dve", lambda e: e.tensor_copy(out=idxf[:], in_=idx[:]), reads=[b_idx], writes=[b_idxf])
            P.op("dve", lambda e: e.scalar_tensor_tensor(out=idxf[:], in0=idxf[:], scalar=8.0, in1=pm8[:], op0=ALU.mult, op1=ALU.add),
                 reads=[b_idxf, b_pm8], writes=[b_idxf])
            P.op("dve", lambda e: e.tensor_copy(out=idx[:], in_=idxf[:]), reads=[b_idxf], writes=[b_idx])
            P.op("dve", lambda e: e.memset(ones_b[:], 1.0), writes=[b_ones_b])
            P.op("dve", lambda e: e.memset(ones2[:], 1.0), writes=[b_ones2])
            P.op("dve", lambda e: e.memset(pcS[:], 0.0), writes=[b_pcS])
            P.op("dve", lambda e: e.memset(pc32[:], 0.0), writes=[b_pc32])
            P.op("dve", lambda e: e.memset(vcS[:], 0.0), writes=[b_vcS])
            P.op("dve", lambda e: e.memset(vcS[:, :, 128:129], 1.0), writes=[b_vcS])
            P.op("dve", lambda e: e.memset(Vsb[:, :, 128:129], 1.0), writes=[b_Vsb])
            P.op("dve", lambda e: e.memset(Vwb[:, :, 128:129], 1.0), writes=[b_Vwb])
            P.op("dve", lambda e: e.memset(hidS[:], 0.0), reads=[b_ps_g], writes=[b_hidS])
            cache_c = cache_cmp
            cache_s = cache_slc
            tcnt = [0]

            if upto == 'S0':
                P.build(st)
                return nc
            def tr_bank():
                r = [(ps_a, b_ps_a), (ps_b, b_ps_b)][tcnt[0] % 2]
                tcnt[0] += 1
                return r

            def sample_seq(b):
                Qb = QbdAll[:, b, :]
                for pg in range(4):
                    (ctile, b_ct) = ct[pg % 2]
                    P.dma("pool", lambda e, pg=pg, ctile=ctile: e.indirect_dma_start(
                        out=ctile[:], out_offset=None, in_=cache_c,
                        in_offset=bass.IndirectOffsetOnAxis(ap=idx[:, b * 4 + pg:b * 4 + pg + 1], axis=0)),
                        reads=[b_idx], writes=[b_ct])
                    cv = ctile.rearrange("p (s c x) -> p s c x", s=16, c=2)
                    for c in range(2):
                        for s4 in range(4):
                            (pt_, b_pt_) = tr_bank()
                            for si in range(4):
                                s = s4 * 4 + si
                                P.op("pe", lambda e, s=s, si=si, c=c, cv=cv, pt_=pt_: e.transpose(
                                    out=pt_[:, si * 128:(si + 1) * 128], in_=cv[:, s, c, :], identity=idf[:]),
                                    reads=[b_ct, b_idf], writes=[b_pt_])
                            eng = "act" if (s4 % 2 == 0) else "dve"
                            if eng == "act":
                                P.op("act", lambda e, c=c, s4=s4, pg=pg, pt_=pt_: e.activation(
                                    out=XTs[:, c, s4 * 4:(s4 + 1) * 4, pg * 128:(pg + 1) * 128],
                                    in_=pt_[:].rearrange("p (a b) -> p a b", a=4), func=AF.Identity), reads=[b_pt_], writes=[b_XTs])
                            else:
                                P.op("dve", lambda e, c=c, s4=s4, pg=pg, pt_=pt_: e.tensor_copy(
                                    out=XTs[:, c, s4 * 4:(s4 + 1) * 4, pg * 128:(pg + 1) * 128],
                                    in_=pt_[:].rearrange("p (a b) -> p a b", a=4)), reads=[b_pt_], writes=[b_XTs])
                for c in range(2):
                    (pp, b_pp) = [(ps_c, b_ps_c), (ps_d, b_ps_d)][c]
                    for s in range(32):
                        P.op("pe", lambda e, c=c, s=s, pp=pp: e.matmul(pp[:, 0:511], lhsT=w1b2[:, c, s, :],
                                                                       rhs=XTs[:, c, s % 16, (s // 16):(s // 16) + 511],
                                                                       start=(s == 0), stop=(s == 31)),
                             reads=[b_w1b2, b_XTs], writes=[b_pp])
                    P.op("act", lambda e, c=c, pp=pp: e.activation(out=hidS[:, c, 0:511], in_=pp[:, 0:511], func=AF.Silu,
                                                                   bias=posb2[:, c:c + 1]),
                         reads=[b_pp, b_posb2], writes=[b_hidS])
                P.op("pe", lambda e: e.matmul(ps_e[:, 0:511], lhsT=w2b2[:, 0, :], rhs=hidS[:, 0, 0:511], start=True, stop=True),
                     reads=[b_w2b2, b_hidS], writes=[b_ps_e])
                P.op("dve", lambda e: e.tensor_copy(out=kcS[:, 0:511], in_=ps_e[:, 0:511]), reads=[b_ps_e], writes=[b_kcS])
                for ch in range(4):
                    nn = 128 if ch < 3 else 127
                    P.op("pe", lambda e, ch=ch, nn=nn: e.matmul(ps_f[0:nn, ch * 128:(ch + 1) * 128], lhsT=hidS[:, 1, ch * 128:ch * 128 + nn],
                                                                rhs=w2b2[:, 1, :], start=True, stop=True),
                         reads=[b_w2b2, b_hidS], writes=[b_ps_f])
                P.op("dve", lambda e: e.tensor_copy(out=vcS[:, 0:3, 0:128], in_=ps_f[:, 0:384].rearrange("p (a b) -> p a b", a=3)),
                     reads=[b_ps_f], writes=[b_vcS])
                P.op("dve", lambda e: e.tensor_copy(out=vcS[0:127, 3, 0:128], in_=ps_f[0:127, 384:512]), reads=[b_ps_f], writes=[b_vcS])
                for ch in range(4):
                    nn = 128 if ch < 3 else 127
                    P.op("pe", lambda e, ch=ch, nn=nn: e.matmul(ps_g[0:nn, ch * 8:(ch + 1) * 8], lhsT=kcS[:, ch * 128:ch * 128 + nn], rhs=Qb,
                                                                start=True, stop=True),
                         reads=[b_kcS, b_QbdAll], writes=[b_ps_g])
                P.op("act", lambda e: e.activation(out=pc32[:, 0:3, :], in_=ps_g[:, 0:24].rearrange("p (a b) -> p a b", a=3), func=AF.Exp, scale=SCALE),
                     reads=[b_ps_g], writes=[b_pc32])
                P.op("act", lambda e: e.activation(out=pc32[0:127, 3, :], in_=ps_g[0:127, 24:32], func=AF.Exp, scale=SCALE),
                     reads=[b_ps_g], writes=[b_pc32])
                P.op("dve", lambda e: e.tensor_copy(out=pcS[:], in_=pc32[:]), reads=[b_pc32], writes=[b_pcS])
                for ch in range(4):
                    P.op("pe", lambda e, ch=ch: e.matmul(ps_g[0:8, 200:329], lhsT=pcS[:, ch, :], rhs=vcS[:, ch, 0:129],
                                                         start=(ch == 0), stop=(ch == 3)),
                         reads=[b_pcS, b_vcS], writes=[b_ps_g])
                P.op("dve", lambda e: e.tensor_copy(out=resb[:, 0, :], in_=ps_g[0:8, 200:329]), reads=[b_ps_g], writes=[b_resb])
                for ch in range(4):
                    P.op("pe", lambda e, ch=ch: e.matmul(ps_g[:, 32:40], lhsT=ones_b[:], rhs=pc32[:, ch, :], start=(ch == 0), stop=(ch == 3)),
                         reads=[b_pc32, b_ones_b], writes=[b_ps_g])
                P.op("dve", lambda e: e.reciprocal(out=denB[:], in_=ps_g[:, 32:40]), reads=[b_ps_g], writes=[b_denB])
                P.op("dve", lambda e: e.tensor_tensor(out=pnk32[:], in0=pc32[:], in1=denB[:].unsqueeze(1).to_broadcast([128, 4, 8]), op=ALU.mult),
                     reads=[b_pc32, b_denB], writes=[b_pnk32])
                P.op("dve", lambda e: e.tensor_reduce(out=pnkf[:], in_=pnk32[:].rearrange("p c (k g) -> p c k g", k=2), axis=AX.X, op=ALU.add),
                     reads=[b_pnk32], writes=[b_pnkf])

                for ch in range(4):
                    P.op("pe", lambda e, ch=ch: e.matmul(ps_g[:, 40:42], lhsT=ovS[:, ch, :], rhs=pnkf[:, ch, :], start=(ch == 0), stop=(ch == 3)),
                         reads=[b_ovS, b_pnkf], writes=[b_ps_g])
                P.op("dve", lambda e: e.tensor_copy(out=selB[:], in_=ps_g[:, 40:42]), reads=[b_ps_g], writes=[b_selB])
                for ch in range(4):
                    P.op("pe", lambda e, ch=ch: e.matmul(ps_g[0:2, 64:192], lhsT=pnkf[:, ch, :], rhs=ovS[:, ch, :], start=(ch == 0), stop=(ch == 3)),
                         reads=[b_ovS, b_pnkf], writes=[b_ps_g])
                P.op("dve", lambda e: e.tensor_tensor(out=impAs[:], in0=ps_g[0:2, 64:192], in1=fmaskA[:], op=ALU.add),
                     reads=[b_ps_g, b_fmaskA], writes=[b_impAs])
                P.op("dve", lambda e: e.max(out=m8A[:, 0:8], in_=impAs[:]), reads=[b_impAs], writes=[b_m8A])
                P.op("dve", lambda e: e.match_replace(out=wkA[:], in_to_replace=m8A[:, 0:8], in_values=impAs[:], imm_value=-5.0),
                     reads=[b_impAs, b_m8A], writes=[b_wkA])
                P.op("dve", lambda e: e.max(out=m8A[:, 8:16], in_=wkA[:]), reads=[b_wkA], writes=[b_m8A])
                P.op("dve", lambda e: e.tensor_scalar(out=dthr[:], in0=idf[0:2, 0:2], scalar1=m8A[:, 12:13], scalar2=0.999999,
                                                      op0=ALU.mult, op1=ALU.mult),
                     reads=[b_m8A, b_idf], writes=[b_dthr])
                P.op("pe", lambda e: e.matmul(ps_g[:, 192:194], lhsT=ones2[:], rhs=dthr[:], start=True, stop=True),
                     reads=[b_ones2, b_dthr], writes=[b_ps_g])
                P.op("dve", lambda e: e.tensor_tensor(out=selB[:], in0=selB[:], in1=ps_g[:, 192:194], op=ALU.is_ge),
                     reads=[b_selB, b_ps_g], writes=[b_selB])
                P.op("dve", lambda e: e.tensor_scalar(out=selB[:], in0=selB[:], scalar1=forc[:, 0:1], scalar2=None, op0=ALU.max),
                     reads=[b_selB, b_forc], writes=[b_selB])
                P.op("dve", lambda e: e.tensor_scalar(out=selB[:], in0=selB[:], scalar1=-NEG, scalar2=NEG, op0=ALU.mult, op1=ALU.add),
                     reads=[b_selB], writes=[b_selB])
                P.op("dve", lambda e: e.tensor_copy(out=BselS[:].rearrange("p (k g) -> p k g", k=2),
                                                    in_=selB[:].unsqueeze(2).to_broadcast([128, 2, 4])),
                     reads=[b_selB], writes=[b_BselS])
                for pg in range(4):
                    (ctile, b_ct) = ct[pg % 2]
                    P.dma("pool", lambda e, pg=pg, ctile=ctile: e.indirect_dma_start(
                        out=ctile[:], out_offset=None, in_=cache_s,
                        in_offset=bass.IndirectOffsetOnAxis(ap=idx[:, b * 4 + pg:b * 4 + pg + 1], axis=0)),
                        reads=[b_idx], writes=[b_ct])
                    cv = ctile.rearrange("p (s c x) -> p s c x", s=16, c=2)
                    P.op("pool", lambda e, pg=pg, cv=cv: e.tensor_copy(out=Vsb[:, pg * 16:(pg + 1) * 16, 0:128], in_=cv[:, :, 1, :]),
                         reads=[b_ct], writes=[b_Vsb])
                    for s4 in range(4):
                        (pt_, b_pt_) = tr_bank()
                        (kst, b_kst) = KsT[s4 % 2]
                        for si in range(4):
                            s = s4 * 4 + si
                            P.op("pe", lambda e, s=s, si=si, cv=cv, pt_=pt_: e.transpose(
                                out=pt_[:, si * 128:(si + 1) * 128], in_=cv[:, s, 0, :], identity=idf[:]),
                                reads=[b_ct, b_idf], writes=[b_pt_])
                        if s4 % 2 == 0:
                            P.op("act", lambda e, pt_=pt_, kst=kst: e.activation(out=kst[:], in_=pt_[:].rearrange("p (a b) -> p a b", a=4), func=AF.Identity),
                                 reads=[b_pt_], writes=[b_kst])
                        else:
                            P.op("dve", lambda e, pt_=pt_, kst=kst: e.tensor_copy(out=kst[:], in_=pt_[:].rearrange("p (a b) -> p a b", a=4)),
                                 reads=[b_pt_], writes=[b_kst])
                        for si in range(4):
                            col = (pg * 16 + s4 * 4 + si) * 8
                            P.op("pe", lambda e, si=si, col=col, kst=kst: e.matmul(ps_e[:, col:col + 8], lhsT=kst[:, si, :], rhs=Qb,
                                                                                   start=True, stop=False),
                                 reads=[b_kst, b_QbdAll], writes=[b_ps_e])
                            P.op("pe", lambda e, col=col, pg=pg: e.matmul(ps_e[:, col:col + 8], lhsT=Eg[:, pg, :], rhs=BselS[:],
                                                                          start=False, stop=True),
                                 reads=[b_Eg, b_BselS], writes=[b_ps_e])
                P.op("act", lambda e: e.activation(out=psS[:], in_=ps_e[:], func=AF.Exp, scale=SCALE), reads=[b_ps_e], writes=[b_psS])
                for j in range(64):
                    P.op("pe", lambda e, j=j: e.matmul(ps_g[0:8, 200:329], lhsT=psS[:, j * 8:(j + 1) * 8], rhs=Vsb[:, j, 0:129],
                                                       start=(j == 0), stop=(j == 63)),
                         reads=[b_psS, b_Vsb], writes=[b_ps_g])
                P.op("dve", lambda e: e.tensor_copy(out=resb[:, 1, :], in_=ps_g[0:8, 200:329]), reads=[b_ps_g], writes=[b_resb])
                P.dma("sp", lambda e: e.dma_start(out=wt[:], in_=state_win[b].rearrange("(t p) x -> p t x", p=128)), writes=[b_wt])
                (pt_, b_pt_) = tr_bank()
                for t in range(4):
                    P.op("pe", lambda e, t=t, pt_=pt_: e.transpose(out=pt_[:, t * 128:(t + 1) * 128], in_=wt[:, t, 0:128], identity=idf[:]),
                         reads=[b_wt, b_idf], writes=[b_pt_])
                P.op("dve", lambda e, pt_=pt_: e.tensor_copy(out=KwS[:], in_=pt_[:]), reads=[b_pt_], writes=[b_KwS])
                P.op("pool", lambda e: e.tensor_copy(out=Vwb[:, :, 0:128], in_=wt[:, :, 128:256]), reads=[b_wt], writes=[b_Vwb])
                for t in range(4):
                    P.op("pe", lambda e, t=t: e.matmul(ps_f[:, t * 8:(t + 1) * 8], lhsT=KwS[:, t * 128:(t + 1) * 128], rhs=Qb, start=True, stop=True),
                         reads=[b_KwS, b_QbdAll], writes=[b_ps_f])
                P.op("act", lambda e: e.activation(out=pwS[:], in_=ps_f[:, 0:32].rearrange("p (a b) -> p a b", a=4), func=AF.Exp, scale=SCALE),
                     reads=[b_ps_f], writes=[b_pwS])
                for t in range(4):
                    P.op("pe", lambda e, t=t: e.matmul(ps_g[0:8, 200:329], lhsT=pwS[:, t, :], rhs=Vwb[:, t, 0:129], start=(t == 0), stop=(t == 3)),
                         reads=[b_pwS, b_Vwb], writes=[b_ps_g])
                P.op("dve", lambda e: e.tensor_copy(out=resb[:, 2, :], in_=ps_g[0:8, 200:329]), reads=[b_ps_g], writes=[b_resb])
                P.dma("sp", lambda e: e.dma_start(out=R_d[b], in_=resb[:]), reads=[b_resb], writes=[b_R_d])

            for b in range(int(os.environ.get('KNSEQ', NS))):
                sample_seq(b)
            if upto == 'S1':
                P.build(st)
                return nc
            P.barrier()
            C.reset(m_persist2)
            R, b_R = C.al("R", [NS, 8, 3, 129], F32)
            P.dma("sp", lambda e: e.dma_start(out=R[:], in_=R_d), reads=[b_R_d], writes=[b_R])
            qk, b_qk = C.al("qk", [NS, 8, 64], F32)
            es, b_es = C.al("es", [NS, 2, 8], F32)
            dn, b_dn = C.al("dn", [NS, 8], F32)
            cf, b_cf = C.al("cf", [NS, 8], F32)
            oS, b_oS = C.al("oS", [NS, 8, 64], F32)
            tS, b_tS = C.al("tS", [NS, 8, 64], F32)
            gaoS, b_gaoS = C.al("gaoS", [NS, 512], F32)
            P.dma("sp", lambda e: e.dma_start(out=gaoS[:], in_=g_attn_out.partition_broadcast(NS)), writes=[b_gaoS])
            qv = qtmS[:].rearrange("p (g k d) -> p k g d", g=4, k=2)
            kvv = kvS[:].rearrange("p (r c k d) -> p r c k d", r=3, c=2, k=2)
            gS3 = gatesS[:].rearrange("p (h r) -> p h r", r=3)
            for bi, br in enumerate((1, 2)):
                for k in range(2):
                    P.op("dve", lambda e, br=br, k=k: e.tensor_tensor(out=qk[:, k * 4:(k + 1) * 4, :], in0=qv[:, k, :, :],
                                                                      in1=kvv[:, br, 0, k, :].unsqueeze(1).to_broadcast([NS, 4, 64]), op=ALU.mult),
                         reads=[b_qtmS, b_kvS], writes=[b_qk])
                P.op("dve", lambda e, bi=bi: e.tensor_reduce(out=es[:, bi, :], in_=qk[:], axis=AX.X, op=ALU.add), reads=[b_qk], writes=[b_es])
            P.op("act", lambda e: e.activation(out=es[:], in_=es[:], func=AF.Exp, scale=SCALE), reads=[b_es], writes=[b_es])
            for br in range(3):
                for k in range(2):
                    P.op("dve", lambda e, br=br, k=k: e.tensor_copy(out=tS[:, k * 4:(k + 1) * 4, :], in_=R[:, k * 4:(k + 1) * 4, br, k * 64:(k + 1) * 64]),
                         reads=[b_R], writes=[b_tS])
                P.op("dve", lambda e, br=br: e.tensor_copy(out=dn[:], in_=R[:, :, br, 128]), reads=[b_R], writes=[b_dn])
                if br > 0:
                    for k in range(2):
                        P.op("dve", lambda e, br=br, k=k: e.tensor_tensor(
                            out=qk[:, k * 4:(k + 1) * 4, :], in0=es[:, br - 1, k * 4:(k + 1) * 4].unsqueeze(2).to_broadcast([NS, 4, 64]),
                            in1=kvv[:, br, 1, k, :].unsqueeze(1).to_broadcast([NS, 4, 64]), op=ALU.mult),
                            reads=[b_es, b_kvS], writes=[b_qk])
                    P.op("dve", lambda e: e.tensor_tensor(out=tS[:], in0=tS[:], in1=qk[:], op=ALU.add), reads=[b_tS, b_qk], writes=[b_tS])
                    P.op("dve", lambda e, br=br: e.tensor_tensor(out=dn[:], in0=dn[:], in1=es[:, br - 1, :], op=ALU.add),
                         reads=[b_dn, b_es], writes=[b_dn])
                P.op("dve", lambda e: e.reciprocal(out=dn[:], in_=dn[:]), reads=[b_dn], writes=[b_dn])
                P.op("dve", lambda e, br=br: e.tensor_tensor(out=cf[:], in0=dn[:], in1=gS3[:, :, br], op=ALU.mult),
                     reads=[b_dn, b_gatesS], writes=[b_cf])
                P.op("dve", lambda e: e.tensor_tensor(out=tS[:], in0=tS[:], in1=cf[:].unsqueeze(2).to_broadcast([NS, 8, 64]), op=ALU.mult),
                     reads=[b_tS, b_cf], writes=[b_tS])
                if br == 0:
                    P.op("dve", lambda e: e.tensor_copy(out=oS[:], in_=tS[:]), reads=[b_tS], writes=[b_oS])
                else:
                    P.op("dve", lambda e: e.tensor_tensor(out=oS[:], in0=oS[:], in1=tS[:], op=ALU.add), reads=[b_tS, b_oS], writes=[b_oS])
            oSf = oS[:].rearrange("p h d -> p (h d)")
            tSf = tS[:].rearrange("p h d -> p (h d)")
            obS, b_obS = C.al("obS", [NS, 512], BF16)
            s1, b_s1 = C.al("s1", [NS, 4], F32)

            def norm_to_cat(src, b_src, gam, b_gam, chunk0):
                P.op("act", lambda e: e.activation(out=tSf, in_=src, func=AF.Square, accum_out=s1[:, 0:1]), reads=[b_src], writes=[b_tS, b_s1])
                P.op("act", lambda e: e.activation(out=s1[:, 0:1], in_=s1[:, 0:1], func=AF.Sqrt, scale=1.0 / 512, bias=epsT[0:NS, :]),
                     reads=[b_s1, b_eps], writes=[b_s1])
                P.op("dve", lambda e: e.reciprocal(out=s1[:, 0:1], in_=s1[:, 0:1]), reads=[b_s1], writes=[b_s1])
                P.op("dve", lambda e: e.scalar_tensor_tensor(out=obS[:], in0=src, scalar=s1[:, 0:1], in1=gam, op0=ALU.mult, op1=ALU.mult),
                     reads=[b_src, b_s1, b_gam], writes=[b_obS])
                for j in range(4):
                    P.op("pe", lambda e, j=j: e.transpose(out=ps_tr[:, j, 0:NS], in_=obS[:, j * 128:(j + 1) * 128], identity=idb[0:NS, 0:NS]),
                         reads=[b_obS, b_idb], writes=[b_ps_tr])
                P.op("dve", lambda e: e.tensor_copy(out=catS[:, chunk0:chunk0 + 4, :], in_=ps_tr[:, 0:4, 0:NS]), reads=[b_ps_tr], writes=[b_catS])

            if DBG:
                P.dma("pool", lambda e: e.dma_start(out=dbg_os, in_=oSf), reads=[b_oS], final=True)
            norm_to_cat(oSf, b_oS, gaoS[:], b_gaoS, 0)
            if upto == 'S2':
                P.build(st)
                return nc
            scv, b_scv = C.al("scv", [NS, 15, 512], F32)
            wdv, b_wdv = C.al("wdv", [NS, 15, 512], F32)
            ycv, b_ycv = C.al("ycv", [NS, 512], F32)
            ytmp, b_ytmp = C.al("ytmp", [NS, 512], F32)
            prm, b_prm = C.al("prm", [NS, 4, 512], F32)
            for pi, src in enumerate((b_dw_row, conv_ln_g, conv_ln_b, g_conv_out)):
                P.dma("sp", lambda e, pi=pi, src=src: e.dma_start(out=prm[:, pi, :], in_=src.partition_broadcast(NS)), writes=[b_prm])
            for hh in range(2):
                P.dma("sp", lambda e, hh=hh: e.dma_start(out=scv[:], in_=state_conv[:, hh * 15:(hh + 1) * 15, :]), writes=[b_scv])
                P.dma("sp", lambda e, hh=hh: e.dma_start(out=wdv[:].rearrange("p k c -> p (k c)"),
                                                         in_=w_dw_rows[:, hh * 15 * 512:(hh + 1) * 15 * 512].partition_broadcast(NS)), writes=[b_wdv])
                P.op("dve", lambda e: e.tensor_tensor(out=scv[:], in0=scv[:], in1=wdv[:], op=ALU.mult), reads=[b_scv, b_wdv], writes=[b_scv])
                if hh == 0:
                    P.op("dve", lambda e: e.tensor_reduce(out=ycv[:], in_=scv[:].rearrange("p k c -> p c k"), axis=AX.X, op=ALU.add),
                         reads=[b_scv], writes=[b_ycv])
                else:
                    P.op("dve", lambda e: e.tensor_reduce(out=ytmp[:], in_=scv[:].rearrange("p k c -> p c k"), axis=AX.X, op=ALU.add),
                         reads=[b_scv], writes=[b_ytmp])
                    P.op("dve", lambda e: e.tensor_tensor(out=ycv[:], in0=ycv[:], in1=ytmp[:], op=ALU.add), reads=[b_ycv, b_ytmp], writes=[b_ycv])
            P.dma("sp", lambda e: e.dma_start(out=wdv[:, 0, :], in_=w_dw_rows[:, 30 * 512:31 * 512].partition_broadcast(NS)), writes=[b_wdv])
            P.op("dve", lambda e: e.tensor_tensor(out=ytmp[:], in0=usS[:], in1=wdv[:, 0, :], op=ALU.mult), reads=[b_usS, b_wdv], writes=[b_ytmp])
            P.op("dve", lambda e: e.tensor_tensor(out=ycv[:], in0=ycv[:], in1=ytmp[:], op=ALU.add), reads=[b_ycv, b_ytmp], writes=[b_ycv])
            P.op("dve", lambda e: e.tensor_tensor(out=ycv[:], in0=ycv[:], in1=prm[:, 0, :], op=ALU.add), reads=[b_ycv, b_prm], writes=[b_ycv])
            st6s, b_st6s = C.al("st6s", [NS, 6], F32)
            mvs, b_mvs = C.al("mvs", [NS, 2], F32)
            P.op("dve", lambda e: e.bn_stats(out=st6s[:], in_=ycv[:]), reads=[b_ycv], writes=[b_st6s])
            P.op("dve", lambda e: e.bn_aggr(out=mvs[:], in_=st6s[:]), reads=[b_st6s], writes=[b_mvs])
            P.op("act", lambda e: e.activation(out=s1[:, 1:2], in_=mvs[:, 1:2], func=AF.Sqrt, bias=epsT[0:NS, :]), reads=[b_mvs, b_eps], writes=[b_s1])
            P.op("dve", lambda e: e.reciprocal(out=s1[:, 1:2], in_=s1[:, 1:2]), reads=[b_s1], writes=[b_s1])
            P.op("dve", lambda e: e.tensor_scalar(out=ycv[:], in0=ycv[:], scalar1=mvs[:, 0:1], scalar2=s1[:, 1:2], op0=ALU.subtract, op1=ALU.mult),
                 reads=[b_ycv, b_mvs, b_s1], writes=[b_ycv])
            P.op("dve", lambda e: e.tensor_tensor(out=ycv[:], in0=ycv[:], in1=prm[:, 1, :], op=ALU.mult), reads=[b_ycv, b_prm], writes=[b_ycv])
            P.op("dve", lambda e: e.tensor_tensor(out=ycv[:], in0=ycv[:], in1=prm[:, 2, :], op=ALU.add), reads=[b_ycv, b_prm], writes=[b_ycv])
            P.op("act", lambda e: e.activation(out=ycv[:], in_=ycv[:], func=AF.Silu), reads=[b_ycv], writes=[b_ycv])
            if DBG:
                P.dma("pool", lambda e: e.dma_start(out=dbg_ycv, in_=ycv[:]), reads=[b_ycv], final=True)
            norm_to_cat(ycv[:], b_ycv, prm[:, 3, :], b_prm, 4)
            P.barrier()
            C.reset(m_persist2)
        if stage < 3:
            P.build(st)
            return nc
        NT = NOWN + NS
        C.reset(m_samp)
        h2T, b_h2T = C.al("h2T", [128, 8, NT], BF16)
        cwT, b_cwT = C.al("cwT", [32, NT], F32)
        m_persist3 = C.mark()
        assert C.off <= m_persist, (C.off, m_persist)
        C.reset(m_persist2)
        wob, b_wob = C.al("wob", [128, 8, D], BF16)
        wstF, b_wstF = C.al("wstF", [128, 8, 256], F32)
        g1Pm, b_g1Pm = C.al("g1Pm", [128, D], F32)
        sh2P, b_sh2P = C.al("sh2P", [128, D], F32)
        A2P, b_A2P = C.al("A2P", [128, D], F32)
        g1Sm, b_g1Sm = C.al("g1Sm", [NS, D], F32)
        sh2S, b_sh2S = C.al("sh2S", [NS, D], F32)
        A2S, b_A2S = C.al("A2S", [NS, D], F32)
        g2P, b_g2P = C.al("g2P", [128, D], F32)
        wrs, b_wrs = C.al("wrs", [128, 8, 36], F32)
        wrb, b_wrb = C.al("wrb", [128, 8, 36], BF16)
        brP, b_brP = C.al("brP", [128, 36], F32)
        xF = [C.al("xF%d" % i, [128, D], F32) for i in range(2)]
        x2t, b_x2t = C.al("x2t", [128, D], F32)
        hj, b_hj = C.al("hj", [128, D], F32)
        h2b, b_h2b = C.al("h2b", [128, D], BF16)
        lg, b_lg = C.al("lg", [128, 36], F32)
        zz, b_zz = C.al("zz", [128, 32], F32)
        ej, b_ej = C.al("ej", [128, 4], F32)
        oh, b_oh = C.al("oh", [128, 4], F32)
        sm, b_sm = C.al("sm", [128, 8], F32)
        m8f, b_m8f = C.al("m8f", [128, 8], F32)
        cw, b_cw = C.al("cw", [128, 32], F32)
        cw2, b_cw2 = C.al("cw2", [128, 32], F32)
        load_mod(2, g1Pm, b_g1Pm, g1Sm, b_g1Sm)
        load_mod(3, sh2P, b_sh2P, sh2S, b_sh2S)
        load_mod(4, A2P, b_A2P, A2S, b_A2S)
        P.dma("sp", lambda e: e.dma_start(out=g2P[:], in_=norm2_g.partition_broadcast(128)), writes=[b_g2P])
        P.op("dve", lambda e: e.scalar_tensor_tensor(out=A2P[:], in0=A2P[:], scalar=1.0, in1=g2P[:], op0=ALU.add, op1=ALU.mult),
             reads=[b_A2P, b_g2P], writes=[b_A2P])
        P.op("dve", lambda e: e.scalar_tensor_tensor(out=A2S[:], in0=A2S[:], scalar=1.0, in1=g2P[0:NS, :], op0=ALU.add, op1=ALU.mult),
             reads=[b_A2S, b_g2P], writes=[b_A2S])
        P.dma("sp", lambda e: e.dma_start(out=wrs[:], in_=w_gr.rearrange("(c p) n -> p c n", p=128)), writes=[b_wrs])
        P.op("dve", lambda e: e.tensor_copy(out=wrb[:], in_=wrs[:]), reads=[b_wrs], writes=[b_wrb])
        P.dma("sp", lambda e: e.dma_start(out=brP[:], in_=b_gr.partition_broadcast(128)), writes=[b_brP])
        w_out_v = w_out.rearrange("(c p) n -> p c n", p=128)
        for c0 in range(0, D, 256):
            P.dma("sp", lambda e, c0=c0: e.dma_start(out=wstF[:], in_=w_out_v[:, :, c0:c0 + 256]), writes=[b_wstF])
            P.op("pool", lambda e, c0=c0: e.tensor_copy(out=wob[:, :, c0:c0 + 256], in_=wstF[:]), reads=[b_wstF], writes=[b_wob])

        def finish_tile(i, n, cat_ap, b_cat, x_src, tok0, kind):
            (x_t, b_x) = xF[i % 2]
            g1m, b_g1m = (g1Sm, b_g1Sm) if kind == "sample" else (g1Pm, b_g1Pm)
            sh2, b_sh2 = (sh2S, b_sh2S) if kind == "sample" else (sh2P, b_sh2P)
            A2, b_A2 = (A2S, b_A2S) if kind == "sample" else (A2P, b_A2P)
            P.dma("sp", lambda e: e.dma_start(out=x_t[0:n, :], in_=x_src), writes=[b_x])
            for half in range(2):
                psx, b_psx = [(ps_a, b_ps_a), (ps_b, b_ps_b)][half]
                hs = slice(half * 512, (half + 1) * 512)
                for c in range(8):
                    P.op("pe", lambda e, c=c, psx=psx, hs=hs: e.matmul(psx[0:n, :], lhsT=cat_ap[:, c, :], rhs=wob[:, c, hs],
                                                                       start=(c == 0), stop=(c == 7)),
                         reads=[b_cat, b_wob], writes=[b_psx])
                P.op("dve", lambda e, psx=psx, hs=hs: e.tensor_tensor(out=x2t[0:n, hs], in0=psx[0:n, :], in1=g1m[0:n, hs], op=ALU.mult),
                     reads=[b_psx, b_g1m], writes=[b_x2t])
                P.op("pool", lambda e, hs=hs: e.tensor_tensor(out=x2t[0:n, hs], in0=x2t[0:n, hs], in1=x_t[0:n, hs], op=ALU.add),
                     reads=[b_x2t, b_x], writes=[b_x2t])
            P.dma("pool", lambda e: e.dma_start(out=x2_d[tok0:tok0 + n, :], in_=x2t[0:n, :]), reads=[b_x2t], writes=[b_x2_d])
            P.op("act", lambda e: e.activation(out=hj[0:n, :], in_=x2t[0:n, :], func=AF.Square, accum_out=sm[0:n, 0:1]),
                 reads=[b_x2t], writes=[b_hj, b_sm])
            P.op("act", lambda e: e.activation(out=sm[0:n, 0:1], in_=sm[0:n, 0:1], func=AF.Sqrt, scale=1.0 / D, bias=epsT[0:n, :]),
                 reads=[b_sm, b_eps], writes=[b_sm])
            P.op("dve", lambda e: e.reciprocal(out=sm[0:n, 0:1], in_=sm[0:n, 0:1]), reads=[b_sm], writes=[b_sm])
            P.op("dve", lambda e: e.scalar_tensor_tensor(out=hj[0:n, :], in0=x2t[0:n, :], scalar=sm[0:n, 0:1], in1=A2[0:n, :],
                                                         op0=ALU.mult, op1=ALU.mult),
                 reads=[b_x2t, b_sm, b_A2], writes=[b_hj])
            P.op("pool", lambda e: e.tensor_tensor(out=h2b[0:n, :], in0=hj[0:n, :], in1=sh2[0:n, :], op=ALU.add),
                 reads=[b_hj, b_sh2], writes=[b_h2b])
            for c in range(8):
                P.op("pe", lambda e, c=c: e.transpose(out=ps_tr[:, c, 0:n], in_=h2b[0:n, c * 128:(c + 1) * 128], identity=idb[0:n, 0:n]),
                     reads=[b_h2b, b_idb], writes=[b_ps_tr])
            P.op("act", lambda e: e.copy(out=h2T[:, :, tok0:tok0 + n], in_=ps_tr[:, :, 0:n]), reads=[b_ps_tr], writes=[b_h2T])
            for c in range(8):
                P.op("pe", lambda e, c=c: e.matmul(ps_c[0:n, 0:36], lhsT=h2T[:, c, tok0:tok0 + n], rhs=wrb[:, c, :],
                                                   start=(c == 0), stop=(c == 7)),
                     reads=[b_h2T, b_wrb], writes=[b_ps_c])
            P.op("dve", lambda e: e.tensor_tensor(out=lg[0:n, :], in0=ps_c[0:n, 0:36], in1=brP[0:n, :], op=ALU.add),
                 reads=[b_ps_c, b_brP], writes=[b_lg])
            P.op("dve", lambda e: e.tensor_reduce(out=sm[0:n, 1:2], in_=lg[0:n, 0:4], axis=AX.X, op=ALU.max),
                 reads=[b_lg], writes=[b_sm])
            P.op("dve", lambda e: e.tensor_scalar(out=sm[0:n, 2:3], in0=sm[0:n, 1:2], scalar1=-1.0, scalar2=None, op0=ALU.mult),
                 reads=[b_sm], writes=[b_sm])
            P.op("act", lambda e: e.activation(out=ej[0:n, :], in_=lg[0:n, 0:4], func=AF.Exp, bias=sm[0:n, 2:3],
                                               accum_out=sm[0:n, 3:4]),
                 reads=[b_lg, b_sm], writes=[b_ej, b_sm])
            P.op("dve", lambda e: e.reciprocal(out=sm[0:n, 3:4], in_=sm[0:n, 3:4]), reads=[b_sm], writes=[b_sm])
            P.op("dve", lambda e: e.tensor_scalar(out=oh[0:n, :], in0=lg[0:n, 0:4], scalar1=sm[0:n, 1:2], scalar2=None, op0=ALU.is_ge),
                 reads=[b_lg, b_sm], writes=[b_oh])
            P.op("dve", lambda e: e.tensor_scalar(out=oh[0:n, :], in0=oh[0:n, :], scalar1=1e4, scalar2=-1e4, op0=ALU.mult, op1=ALU.add),
                 reads=[b_oh], writes=[b_oh])
            P.op("dve", lambda e: e.tensor_tensor(out=zz[0:n, :].rearrange("p (g j) -> p g j", g=4),
                                                  in0=lg[0:n, 4:36].rearrange("p (g j) -> p g j", g=4),
                                                  in1=oh[0:n, :].unsqueeze(2).to_broadcast([n, 4, 8]), op=ALU.add),
                 reads=[b_lg, b_oh], writes=[b_zz])
            P.op("dve", lambda e: e.max(out=m8f[0:n, :], in_=zz[0:n, :]), reads=[b_zz], writes=[b_m8f])
            P.op("dve", lambda e: e.tensor_tensor(out=sm[0:n, 4:5], in0=m8f[0:n, 0:1], in1=m8f[0:n, 1:2], op=ALU.subtract),
                 reads=[b_m8f], writes=[b_sm])
            P.op("act", lambda e: e.activation(out=sm[0:n, 4:5], in_=sm[0:n, 4:5], func=AF.Exp), reads=[b_sm], writes=[b_sm])
            P.op("dve", lambda e: e.tensor_scalar(out=sm[0:n, 4:5], in0=sm[0:n, 4:5], scalar1=1.0, scalar2=None, op0=ALU.add),
                 reads=[b_sm], writes=[b_sm])
            P.op("dve", lambda e: e.reciprocal(out=sm[0:n, 4:5], in_=sm[0:n, 4:5]), reads=[b_sm], writes=[b_sm])
            P.op("dve", lambda e: e.tensor_tensor(out=sm[0:n, 5:6], in0=sm[0:n, 4:5], in1=sm[0:n, 3:4], op=ALU.mult),
                 reads=[b_sm], writes=[b_sm])
            P.op("dve", lambda e: e.tensor_tensor(out=sm[0:n, 6:7], in0=sm[0:n, 3:4], in1=sm[0:n, 5:6], op=ALU.subtract),
                 reads=[b_sm], writes=[b_sm])
            P.op("dve", lambda e: e.tensor_scalar(out=cw[0:n, :], in0=zz[0:n, :], scalar1=m8f[0:n, 0:1], scalar2=sm[0:n, 6:7],
                                                  op0=ALU.is_equal, op1=ALU.mult),
                 reads=[b_zz, b_m8f, b_sm], writes=[b_cw])
            P.op("dve", lambda e: e.tensor_scalar(out=cw2[0:n, :], in0=zz[0:n, :], scalar1=m8f[0:n, 1:2], scalar2=sm[0:n, 5:6],
                                                  op0=ALU.is_equal, op1=ALU.mult),
                 reads=[b_zz, b_m8f, b_sm], writes=[b_cw2])
            P.op("dve", lambda e: e.tensor_tensor(out=cw[0:n, :], in0=cw[0:n, :], in1=cw2[0:n, :], op=ALU.add),
                 reads=[b_cw, b_cw2], writes=[b_cw])
            P.op("pe", lambda e: e.transpose(out=ps_d[0:32, 0:n], in_=cw[0:n, :], identity=idf[0:n, 0:n]),
                 reads=[b_cw, b_idf], writes=[b_ps_d])
            P.op("dve", lambda e: e.tensor_copy(out=cwT[:, tok0:tok0 + n], in_=ps_d[0:32, 0:n]), reads=[b_ps_d], writes=[b_cwT])

        for i in range(16):
            finish_tile(i, 128, catT[:, :, i * 128:(i + 1) * 128], b_catT, xc[i * 128:(i + 1) * 128, :], i * 128, "own")
        if HAVE_SAMPLE:
            finish_tile(16, NS, catS[:, :, :], b_catS, xs, NOWN, "sample")
        P.barrier()
        C.reset(m_persist3)

        NTOK = NT if HAVE_SAMPLE else NOWN
        yacc, b_yT2 = C.al("yTacc", [128, 8, NT], F32)
        m_G = C.mark()
        selT, b_selT = C.al("selT", [32, 32, 128], F32)
        wgs, b_wgs = C.al("wgs", [128, 8, 256], F32)
        wus, b_wus = C.al("wus", [128, 8, 256], F32)
        wds, b_wds = C.al("wds", [128, 2, D], F32)
        wgb = [C.al("wgb%d" % i, [128, 8, 256], BF16) for i in range(2)]
        wub = [C.al("wub%d" % i, [128, 8, 256], BF16) for i in range(2)]
        wdb = [C.al("wdb%d" % i, [128, 2, D], BF16) for i in range(2)]
        sgt = [C.al("sgt%d" % i, [128, 512], F32) for i in range(2)]
        hidb = [C.al("hidb%d" % i, [128, 2, 512], BF16) for i in range(2)]
        P.op("dve", lambda e: e.tensor_copy(out=selT[:], in_=idf[0:32, 0:32].unsqueeze(2).to_broadcast([32, 32, 128])),
             reads=[b_idf], writes=[b_selT])
        groups = [(g * 512, 512) for g in range(4)] + ([(NOWN, NS)] if HAVE_SAMPLE else [])
        ps_gu = [(ps_c, b_ps_c), (ps_d, b_ps_d), (ps_e, b_ps_e), (ps_f, b_ps_f)]
        ps_dn = [(ps_a, b_ps_a), (ps_b, b_ps_b)]
        dn_cnt = [0]

        def expert(ei):
            s = ei % 2
            (wg, b_wg), (wu, b_wu), (wd, b_wd) = wgb[s], wub[s], wdb[s]
            P.dma("sp", lambda e: e.dma_start(out=wgs[:], in_=w_gate[ei].rearrange("(c p) f -> p c f", p=128)), writes=[b_wgs])
            P.dma("sp", lambda e: e.dma_start(out=wus[:], in_=w_up[ei].rearrange("(c p) f -> p c f", p=128)), writes=[b_wus])
            P.dma("sp", lambda e: e.dma_start(out=wds[:], in_=w_down[ei].rearrange("(h p) n -> p h n", p=128)), writes=[b_wds])
            P.op("pool", lambda e: e.tensor_copy(out=wg[:], in_=wgs[:]), reads=[b_wgs], writes=[b_wg])
            P.op("pool", lambda e: e.tensor_copy(out=wu[:], in_=wus[:]), reads=[b_wus], writes=[b_wu])
            P.op("pool", lambda e: e.tensor_copy(out=wd[:], in_=wds[:]), reads=[b_wds], writes=[b_wd])
            def do_group(gi, t0, T):
                (hb_, b_hb_) = hidb[gi % 2]
                P.op("pe", lambda e: e.matmul(ps_g[:, 0:T], lhsT=selT[:, ei, :], rhs=cwT[:, t0:t0 + T], start=True, stop=True),
                     reads=[b_selT, b_cwT], writes=[b_ps_g])
                for which, (wsrc, b_wsrc) in enumerate(((wg, b_wg), (wu, b_wu))):
                    for half in range(2):
                        (psx, b_psx) = ps_gu[which * 2 + half]
                        for c in range(8):
                            P.op("pe", lambda e, c=c, psx=psx, wsrc=wsrc, half=half: e.matmul(
                                psx[:, 0:T], lhsT=wsrc[:, c, half * 128:(half + 1) * 128], rhs=h2T[:, c, t0:t0 + T],
                                start=(c == 0), stop=(c == 7)),
                                reads=[b_wsrc, b_h2T], writes=[b_psx])
                for half in range(2):
                    (pg_, b_pg_) = ps_gu[half]
                    (pu_, b_pu_) = ps_gu[2 + half]
                    (sg_, b_sg_) = sgt[half]
                    P.op("act", lambda e, pg_=pg_, sg_=sg_: e.activation(out=sg_[:, 0:T], in_=pg_[:, 0:T], func=AF.Silu),
                         reads=[b_pg_], writes=[b_sg_])
                    P.op("dve", lambda e, pu_=pu_, sg_=sg_: e.tensor_tensor(out=sg_[:, 0:T], in0=sg_[:, 0:T], in1=pu_[:, 0:T], op=ALU.mult),
                         reads=[b_pu_, b_sg_], writes=[b_sg_])
                    P.op("dve", lambda e, sg_=sg_, hb_=hb_, half=half: e.tensor_tensor(out=hb_[:, half, 0:T], in0=sg_[:, 0:T],
                                                                                       in1=ps_g[:, 0:T], op=ALU.mult),
                         reads=[b_sg_, b_ps_g], writes=[b_hb_])
                for dc in range(8):
                    (pd_, b_pd_) = ps_dn[dn_cnt[0] % 2]
                    dn_cnt[0] += 1
                    for half in range(2):
                        P.op("pe", lambda e, dc=dc, half=half, pd_=pd_, hb_=hb_: e.matmul(
                            pd_[:, 0:T], lhsT=wd[:, half, dc * 128:(dc + 1) * 128], rhs=hb_[:, half, 0:T],
                            start=(half == 0), stop=(half == 1)),
                            reads=[b_wd, b_hb_], writes=[b_pd_])
                    if ei == 0:
                        P.op("dve", lambda e, dc=dc, pd_=pd_: e.tensor_copy(out=yacc[:, dc, t0:t0 + T], in_=pd_[:, 0:T]),
                             reads=[b_pd_], writes=[b_yT2])
                    else:
                        P.op("dve", lambda e, dc=dc, pd_=pd_: e.tensor_tensor(out=yacc[:, dc, t0:t0 + T], in0=yacc[:, dc, t0:t0 + T],
                                                                              in1=pd_[:, 0:T], op=ALU.add),
                             reads=[b_pd_, b_yT2], writes=[b_yT2])

            for gi, (t0, T) in enumerate(groups):
                do_group(gi, t0, T)

        for ei in range(32):
            expert(ei)
        P.barrier()
        C.reset(m_G)

        g2Pm, b_g2Pm = C.al("g2Pm", [128, D], F32)
        g2Sm, b_g2Sm = C.al("g2Sm", [NS, D], F32)
        fgP, b_fgP = C.al("fgP", [128, D], F32)
        x2r = [C.al("x2r%d" % i, [128, D], F32) for i in range(2)]
        xo, b_xo = C.al("xo", [128, D], F32)
        xj, b_xj = C.al("xj", [128, D], F32)
        fs, b_fs = C.al("fs", [128, 1], F32)
        load_mod(5, g2Pm, b_g2Pm, g2Sm, b_g2Sm)
        P.dma("sp", lambda e: e.dma_start(out=fgP[:], in_=final_g.partition_broadcast(128)), writes=[b_fgP])

        def final_tile(i, n, tok0, dst, kind):
            (x_t, b_x) = x2r[i % 2]
            g2m, b_g2m = (g2Sm, b_g2Sm) if kind == "sample" else (g2Pm, b_g2Pm)
            P.dma("sp", lambda e: e.dma_start(out=x_t[0:n, :], in_=x2_d[tok0:tok0 + n, :]), reads=[b_x2_d], writes=[b_x])
            for half in range(2):
                psx, b_psx = [(ps_c, b_ps_c), (ps_d, b_ps_d)][half]
                for j in range(4):
                    dc = half * 4 + j
                    P.op("pe", lambda e, dc=dc, j=j, psx=psx: e.transpose(out=psx[0:n, j * 128:(j + 1) * 128],
                                                                         in_=yacc[:, dc, tok0:tok0 + n], identity=idf[:]),
                         reads=[b_yT2, b_idf], writes=[b_psx])
                hs = slice(half * 512, (half + 1) * 512)
                P.op("dve", lambda e, psx=psx, hs=hs: e.tensor_tensor(out=xo[0:n, hs], in0=psx[0:n, :], in1=g2m[0:n, hs], op=ALU.mult),
                     reads=[b_psx, b_g2m], writes=[b_xo])
                P.op("pool", lambda e, hs=hs: e.tensor_tensor(out=xo[0:n, hs], in0=xo[0:n, hs], in1=x_t[0:n, hs], op=ALU.add),
                     reads=[b_xo, b_x], writes=[b_xo])
            P.op("act", lambda e: e.activation(out=xj[0:n, :], in_=xo[0:n, :], func=AF.Square, accum_out=fs[0:n, :]),
                 reads=[b_xo], writes=[b_xj, b_fs])
            P.op("act", lambda e: e.activation(out=fs[0:n, :], in_=fs[0:n, :], func=AF.Sqrt, scale=1.0 / D, bias=epsT[0:n, :]),
                 reads=[b_fs, b_eps], writes=[b_fs])
            P.op("dve", lambda e: e.reciprocal(out=fs[0:n, :], in_=fs[0:n, :]), reads=[b_fs], writes=[b_fs])
            P.op("dve", lambda e: e.scalar_tensor_tensor(out=xj[0:n, :], in0=xo[0:n, :], scalar=fs[0:n, 0:1], in1=fgP[0:n, :],
                                                         op0=ALU.mult, op1=ALU.mult),
                 reads=[b_xo, b_fs, b_fgP], writes=[b_xj])
            P.dma("pool", lambda e: e.dma_start(out=dst, in_=xj[0:n, :]), reads=[b_xj], final=True)

        for i in range(16):
            final_tile(i, 128, i * 128, y_p[i * 128:(i + 1) * 128, :], "own")
        if HAVE_SAMPLE:
            final_tile(16, NS, NOWN, y_s, "sample")

        P.build(st)
    return nc


_NC_CACHE = {}


def _nat(tp, hf):
    tp = np.asarray(tp)
    return np.where(tp < NOWN, tp + NOWN * hf, tp - NOWN + NOWN * (1 - hf))


def _tables(hf):
    f32 = np.float32
    t = {}
    tp = np.arange(SEQ)
    t["etab"] = (tp[None, :] // 64 == (np.arange(128)[:, None] % 64)).astype(f32)
    kk = np.arange(128)
    t["tridiag"] = np.where(kk[:, None] <= kk[None, :], 0.0, NEG).astype(f32)
    t["trilo"] = np.where(kk[:, None] >= kk[None, :], 0.0, NEG).astype(f32)
    blknat = _nat(64 * np.arange(64), hf) // 64
    t["blknat"] = blknat.astype(f32).reshape(1, 64)
    t["isz"] = (blknat == 0).astype(f32).reshape(1, 64)
    ncn = _nat(16 * np.arange(256), hf) // 16
    valid = ncn[(np.arange(256) + 1) % 256] == ncn + 1
    pe = np.where(valid, 16.0 * ncn + 31.0, 1e9)
    t["pe_slot"] = np.ascontiguousarray(pe.reshape(2, 128).T).astype(f32)
    c0 = 16 * ncn
    ov = valid[:, None] & (c0[:, None] <= 64 * blknat[None, :] + 63) & (c0[:, None] + 31 >= 64 * blknat[None, :])
    t["ovt"] = np.ascontiguousarray(ov.astype(f32).reshape(2, 128, 64).transpose(1, 0, 2))
    qpos = NOWN * hf + np.arange(NOWN)
    t["qrow"] = qpos.astype(f32).reshape(1, NOWN)
    t["curt"] = np.ascontiguousarray((qpos // 64).reshape(16, 128).T).astype(f32)
    t["nvrow"] = np.full((1, 512), NEG * (1 - hf), f32)
    t["hfv"] = np.full((128, 1), float(hf), f32)
    t["identf"] = np.eye(128, dtype=f32)
    return t


def _prep_inputs(inp):
    f32 = np.float32
    g = lambda k: np.asarray(inp[k], f32)
    x_prompt = g("x_prompt")
    x_sample = g("x_sample")
    w1 = g("w_cmp1")[0]
    w1bd = np.zeros((2, 64, 2, 32, 2, 64), f32)
    for k in range(2):
        w1bd[k, :, :, :, k, :] = w1.transpose(2, 0, 1, 3)
    w1bd = w1bd.reshape(128, 2, 32, 128)
    pos = g("pos_cmp")[0]
    posT2 = np.ascontiguousarray(np.tile(pos.transpose(2, 0, 1), (2, 1, 1)))
    w2 = g("w_cmp2")[0]
    w2bd = np.zeros((2, 64, 2, 2, 64), f32)
    for k in range(2):
        w2bd[k, :, :, k, :] = w2.transpose(1, 0, 2)
    w2bd = w2bd.reshape(128, 2, 128)
    wdw = g("w_dw")[0]
    wdwT = np.ascontiguousarray(wdw.reshape(31, 4, 128).transpose(2, 1, 0))
    bdwT = np.ascontiguousarray(g("b_dw")[0].reshape(4, 128).T)
    shared = {
        "norm1_g": g("norm1_g").reshape(1, D),
        "w_ada": g("w_ada")[0],
        "b_ada": g("b_ada").reshape(1, 6 * D),
        "w_in": g("w_in")[0],
        "w1bd": w1bd, "posT2": posT2, "w2bd": w2bd, "wdwT": wdwT, "bdwT": bdwT,
        "b_dw_row": g("b_dw").reshape(1, 512), "w_dw_rows": g("w_dw").reshape(1, 31 * 512),
        "w_out": g("w_out")[0], "norm2_g": g("norm2_g").reshape(1, D),
        "w_gr": np.ascontiguousarray(np.concatenate([g("w_group")[0], g("w_router")[0]], 1)),
        "b_gr": np.concatenate([g("b_group").reshape(1, 4), g("b_router").reshape(1, 32)], 1),
        "w_gate": g("w_gate")[0], "w_up": g("w_up")[0], "w_down": g("w_down")[0],
        "final_g": g("final_g").reshape(1, D),
        "conv_ln_g": g("conv_ln_g").reshape(1, 512), "conv_ln_b": g("conv_ln_b").reshape(1, 512),
        "g_conv_out": g("g_conv_out").reshape(1, 512), "g_attn_out": g("g_attn_out").reshape(1, 512),
    }
    tabs = [_tables(0), _tables(1)]
    if STAGE >= 4 and "cache_cmp_kv" in inp:
        shared["cache_cmp"] = g("cache_cmp_kv").reshape(81920, 4096)
        shared["cache_slc"] = g("cache_slc_kv").reshape(81920, 4096)
    pt = np.asarray(inp["page_table"]).astype(np.int32)
    pp = np.arange(128)
    nn = np.arange(128)
    egt = np.zeros((128, 4, 128), f32)
    ovs = np.zeros((128, 4, 128), f32)
    for pg in range(4):
        egt[:, pg, :] = (pp[:, None] == (128 * pg + nn[None, :]) // 4)
        cn = pg * 128 + nn
        jj = np.arange(128)
        ovs[:, pg, :] = ((cn[:, None] < 511) & (16 * cn[:, None] <= 64 * jj[None, :] + 63) & (16 * cn[:, None] + 31 >= 64 * jj[None, :]))
    forct = np.zeros((128, 1), f32); forct[0] = 1.0; forct[127] = 1.0
    fmA = np.zeros((2, 128), f32); fmA[:, 0] = -10.0; fmA[:, 127] = -10.0
    shared.update({"egtab": egt, "ovstab": ovs, "forctab": forct, "fmaskAtab": fmA,
                   "pm8rep": np.ascontiguousarray(np.tile((pp % 8)[:, None], (1, NS * 4)).astype(f32))})
    maps = []
    for c in range(8):
        b, hf = c // 2, c % 2
        own = x_prompt[b, hf * NOWN:(hf + 1) * NOWN]
        oth = x_prompt[b, (1 - hf) * NOWN:(2 - hf) * NOWN]
        m = dict(shared)
        m.update(tabs[hf])
        m.update({
            "xc": np.ascontiguousarray(np.concatenate([own, oth], 0)),
            "xs": np.ascontiguousarray(x_sample[16 * c:16 * c + 16, 0]),
            "cin": np.ascontiguousarray(np.concatenate([g("c_prompt")[b:b + 1], g("c_sample")[16 * c:16 * c + 16]], 0)),
            "state_win": np.ascontiguousarray(g("state_win_kv")[0, 16 * c:16 * c + 16].reshape(NS, 512, 256)),
            "state_conv": np.ascontiguousarray(g("state_conv")[0, 16 * c:16 * c + 16]),
            "ptrep": np.ascontiguousarray(pt[16 * c:16 * c + 16].reshape(16, 4, 16)[:, :, pp // 8].transpose(2, 0, 1).reshape(128, 64)),
        })
        maps.append(m)
    return maps


def _run(inp, stage=STAGE):
    if stage not in _NC_CACHE:
        _NC_CACHE[stage] = build_program(stage)
    nc = _NC_CACHE[stage]
    maps = _prep_inputs(inp)
    res = run_bass_kernel_spmd(nc, maps, core_ids=list(range(8)))
    return res.results


def kernel(**inp):
    r = _run(inp)
    f32 = np.float32
    B = 4
    y_prompt = np.stack([np.concatenate([r[2 * b]["y_p"], r[2 * b + 1]["y_p"]], 0) for b in range(B)], 0).astype(f32)
    y_sample = np.concatenate([r[c]["y_s"] for c in range(8)], 0).reshape(128, 1, D).astype(f32)
    kvp = np.stack([r[2 * b]["o_kv_p"] for b in range(B)], 0)
    kvp = kvp.reshape(B, SEQ, 3, 2, 2, 64)
    new_cmp_p = np.ascontiguousarray(kvp[None, :, :, 0])
    new_slc_p = np.ascontiguousarray(kvp[None, :, :, 1])
    new_win_p = np.ascontiguousarray(kvp[None, :, SEQ - 512:, 2])
    new_conv_p = np.stack([r[2 * b + 1]["o_conv_p"] for b in range(B)], 0)[None]
    kvs = np.concatenate([r[c]["o_kv_s"] for c in range(8)], 0).reshape(128, 1, 3, 2, 2, 64)
    new_cmp_s = np.ascontiguousarray(kvs[None, :, :, 0])
    new_slc_s = np.ascontiguousarray(kvs[None, :, :, 1])
    new_win_s = np.concatenate([r[c]["o_win_s"] for c in range(8)], 0).reshape(1, 128, 512, 2, 2, 64)
    new_conv_s = np.concatenate([r[c]["o_conv_s"] for c in range(8)], 0)[None]
    return (y_prompt, y_sample, new_cmp_p, new_slc_p, new_win_p, new_conv_p.astype(f32),
            new_cmp_s, new_slc_s, new_win_s.astype(f32), new_conv_s.astype(f32))
```

```python
from contextlib import ExitStack
import numpy as np
import concourse.bass as bass
import concourse.mybir as mybir
from concourse.bass_utils import run_bass_kernel_spmd

F32 = mybir.dt.float32
BF16 = mybir.dt.bfloat16
I32 = mybir.dt.int32
AF = mybir.ActivationFunctionType
ALU = mybir.AluOpType
AX = mybir.AxisListType

ENGS = ("pe", "act", "dve", "pool", "sp")
NDMA_SEMS = {"sp": 24, "pool": 12, "act": 8}

D = 1024
SEQ = 4096
NOWN = 2048
NS = 16
D_IN = 2328
EPS = 1e-6
STAGE = 4


class Buf:
    __slots__ = ("name", "last_w", "readers")

    def __init__(self, name=""):
        self.name = name
        self.last_w = None
        self.readers = []


class Prog:
    def __init__(self, nc):
        self.nc = nc
        self.q = {e: [] for e in ENGS}
        self.cnt = {e: 0 for e in ENGS}
        self.waited = {e: {} for e in ENGS}
        self.dma_i = {e: 0 for e in NDMA_SEMS}
        self.sems = {}
        self.final_tokens = []
        self.barrier_toks = []
        self.dma_last = {}

    def barrier(self):
        toks = [(("eng", e), self.cnt[e]) for e in ENGS if self.cnt[e] > 0]
        toks += list(self.dma_last.items())
        self.barrier_toks = toks

    def _deps(self, eng, reads, writes, strict=False):
        deps = {}

        def add(tok):
            if tok is None:
                return
            k, v = tok
            if k == ("eng", eng) and not strict and eng == "pe":
                return
            if deps.get(k, 0) < v:
                deps[k] = v
        for b in reads:
            add(b.last_w)
        for b in writes:
            add(b.last_w)
            for r in b.readers:
                add(r)
        for t in self.barrier_toks:
            if t[0] != ("eng", eng):
                add(t)
        out = []
        for k, v in deps.items():
            if self.waited[eng].get(k, 0) < v:
                self.waited[eng][k] = v
                out.append((k, v))
        return out

    def _commit(self, tok, reads, writes):
        for b in reads:
            b.readers.append(tok)
            if len(b.readers) > 48:
                mx = {}
                for k, v in b.readers:
                    if mx.get(k, 0) < v:
                        mx[k] = v
                b.readers = list(mx.items())
        for b in writes:
            b.last_w = tok
            b.readers = []

    def op(self, eng, emit, reads=(), writes=()):
        waits = self._deps(eng, reads, writes)
        self.cnt[eng] += 1
        tok = (("eng", eng), self.cnt[eng])
        self.q[eng].append((waits, emit, (("eng", eng), 1)))
        self._commit(tok, reads, writes)
        return tok

    def dma(self, eng, emit, reads=(), writes=(), final=False):
        n = NDMA_SEMS[eng]
        i = self.dma_i[eng]
        self.dma_i[eng] += 1
        k = ("dma", eng, i % n)
        val = 16 * (i // n + 1)
        waits = self._deps(eng, reads, writes, strict=True)
        if i >= n and self.waited[eng].get(k, 0) < val - 16:
            self.waited[eng][k] = val - 16
            waits.append((k, val - 16))
        self.q[eng].append((waits, emit, (k, 16)))
        tok = (k, val)
        self.dma_last[k] = val
        self._commit(tok, reads, writes)
        if final:
            self.final_tokens.append(tok)
        return tok

    def build(self, stack):
        nc = self.nc
        keys = [("eng", e) for e in ENGS]
        for e, n in NDMA_SEMS.items():
            keys += [("dma", e, i) for i in range(n)]
        for k in keys:
            self.sems[k] = stack.enter_context(nc.semaphore("s_" + "_".join(map(str, k))))
        fin = {}
        for k, v in self.final_tokens:
            fin[k] = max(fin.get(k, 0), v)
        block = stack.enter_context(nc.Block())
        sems, q, cnt = self.sems, self.q, self.cnt

        def replay(e, name):
            for waits, emit, inc in q[name]:
                for k, v in waits:
                    e.wait_ge(sems[k], v)
                emit(e).then_inc(sems[inc[0]], inc[1])
            if name == "sp":
                for k, v in fin.items():
                    e.wait_ge(sems[k], v)
                for en in ENGS:
                    if en != "sp" and cnt[en] > 0:
                        e.wait_ge(sems[("eng", en)], cnt[en])

        @block.sync
        def _(e):
            replay(e, "sp")

        @block.tensor
        def _(e):
            replay(e, "pe")

        @block.scalar
        def _(e):
            replay(e, "act")

        @block.vector
        def _(e):
            replay(e, "dve")

        @block.gpsimd
        def _(e):
            replay(e, "pool")


OFFS = {}


class Ctx:
    def __init__(self, nc, st, arena_bytes=0):
        self.nc, self.st = nc, st
        self.bufs = {}
        self.off = 0
        self.peak = 0
        self.arena_bytes = arena_bytes
        if arena_bytes:
            self.arena = st.enter_context(nc.sbuf_tensor("arena", [128, arena_bytes // 4], F32))

    def al(self, name, shape, dt=F32):
        esz = 4 if dt in (F32, I32) else 2
        n = 1
        for d in shape[1:]:
            n *= d
        nb = (n * esz + 31) // 32 * 32
        assert self.off + nb <= self.arena_bytes, (name, self.off, nb)
        v = self.arena[0:shape[0], self.off // 4:(self.off + nb) // 4]
        if dt != F32:
            v = v.bitcast(dt)
        v = v[:, 0:n]
        if len(shape) > 2:
            names = " ".join("a%d" % i for i in range(len(shape) - 1))
            kw = {"a%d" % i: shape[i + 1] for i in range(len(shape) - 1)}
            v = v.rearrange("p (%s) -> p %s" % (names, names), **kw)
        OFFS[name] = (self.off, tuple(shape), "f32" if dt == F32 else ("i32" if dt == I32 else "bf16"))
        self.off += nb
        self.peak = max(self.peak, self.off)
        b = Buf(name)
        self.bufs[name] = b
        return v, b

    def mark(self):
        return self.off

    def reset(self, m):
        self.off = m

    def sb(self, name, shape, dt=F32):
        t = self.st.enter_context(self.nc.sbuf_tensor(name, list(shape), dt))
        self.bufs[name] = Buf(name)
        return t, self.bufs[name]

    def ps(self, name, shape, dt=F32):
        t = self.st.enter_context(self.nc.psum_tensor(name, list(shape), dt))
        self.bufs[name] = Buf(name)
        return t, self.bufs[name]

    def din(self, name, shape, dt=F32):
        return self.nc.dram_tensor(name, list(shape), dt, kind="ExternalInput").ap()

    def dout(self, name, shape, dt=F32):
        return self.nc.dram_tensor(name, list(shape), dt, kind="ExternalOutput").ap()


import os
SKIP = set(os.environ.get('KSKIP', '').split(','))
NEG = -30000.0
SCALE = 0.125
ARENA = 196 * 1024


def build_program(stage=STAGE, upto='Z'):
    nc = bass.Bass("TRN2", target_bir_lowering=False)
    st = ExitStack()
    with st:
        C = Ctx(nc, st, arena_bytes=ARENA)
        P = Prog(nc)
        xc = C.din("xc", [SEQ, D])
        xs = C.din("xs", [NS, D])
        cin = C.din("cin", [1 + NS, D])
        norm1_g = C.din("norm1_g", [1, D])
        w_ada = C.din("w_ada", [D, 6 * D])
        b_ada = C.din("b_ada", [1, 6 * D])
        w_in = C.din("w_in", [D, D_IN])
        identf = C.din("identf", [128, 128])
        hfv = C.din("hfv", [128, 1])
        state_win = C.din("state_win", [NS, 512, 256])
        state_conv = C.din("state_conv", [NS, 30, 512])
        etab = C.din("etab", [128, SEQ])
        tridiag = C.din("tridiag", [128, 128])
        trilo = C.din("trilo", [128, 128])
        pe_slot = C.din("pe_slot", [128, 2])
        qrow = C.din("qrow", [1, NOWN])
        ovt = C.din("ovt", [128, 2, 64])
        blknat = C.din("blknat", [1, 64])
        isz = C.din("isz", [1, 64])
        curt = C.din("curt", [128, 16])
        nvrow = C.din("nvrow", [1, 512])
        w1bd = C.din("w1bd", [128, 2, 32, 128])
        posT2 = C.din("posT2", [128, 2, 32])
        w2bd = C.din("w2bd", [128, 2, 128])
        wdwT = C.din("wdwT", [128, 4, 31])
        bdwT = C.din("bdwT", [128, 4])
        conv_ln_g = C.din("conv_ln_g", [1, 512])
        conv_ln_b = C.din("conv_ln_b", [1, 512])
        g_conv_out = C.din("g_conv_out", [1, 512])
        g_attn_out = C.din("g_attn_out", [1, 512])

        if stage >= 4:
            cache_cmp = C.din("cache_cmp", [81920, 4096])
            cache_slc = C.din("cache_slc", [81920, 4096])
        ptrep = C.din("ptrep", [128, NS * 4], I32)
        pm8rep = C.din("pm8rep", [128, NS * 4])
        egtab = C.din("egtab", [128, 4, 128])
        ovstab = C.din("ovstab", [128, 4, 128])
        forctab = C.din("forctab", [128, 1])
        fmaskAtab = C.din("fmaskAtab", [2, 128])
        b_dw_row = C.din("b_dw_row", [1, 512])
        w_dw_rows = C.din("w_dw_rows", [1, 31 * 512])
        w_out = C.din("w_out", [D, D])
        norm2_g = C.din("norm2_g", [1, D])
        w_gr = C.din("w_gr", [D, 36])
        b_gr = C.din("b_gr", [1, 36])
        w_gate = C.din("w_gate", [32, D, 256])
        w_up = C.din("w_up", [32, D, 256])
        w_down = C.din("w_down", [32, 256, D])
        final_g = C.din("final_g", [1, D])
        y_p = C.dout("y_p", [NOWN, D])
        y_s = C.dout("y_s", [NS, D])
        o_kv_p = C.dout("o_kv_p", [SEQ, 768])
        o_kv_s = C.dout("o_kv_s", [NS, 768])
        o_conv_p = C.dout("o_conv_p", [30, 512])
        o_win_s = C.dout("o_win_s", [NS, 512, 256])
        o_conv_s = C.dout("o_conv_s", [NS, 30, 512])
        DBG = "D" in SKIP
        if stage == 2:
            dbg_attn = C.dout("dbg_attn", [NOWN, 512])
            dbg_conv = C.dout("dbg_conv", [NOWN, 512])
            dbg_br = C.dout("dbg_br", [3, NOWN, 512])
        if DBG:
            dbg_os = C.dout("dbg_os", [NS, 512])
            dbg_ycv = C.dout("dbg_ycv", [NS, 512])

        def scratch(name, shape, dt=F32):
            return nc.dram_tensor(name, list(shape), dt, kind="Internal").ap(), Buf(name)
        mods_d, b_mods_d = scratch("mods_d", [1 + NS, 6 * D])
        qT_d, b_qT_d = scratch("qT_d", [2, 16, 64, 512], BF16)
        u_d, b_u_d = scratch("u_d", [128, 4, 30 + NOWN], BF16)
        x2_d, b_x2_d = scratch("x2_d", [NOWN + NS, D])
        HAVE_SAMPLE = stage >= 4
        R_d, b_R_d = scratch("R_d", [NS, 8, 3, 129])

        ps_tr, b_ps_tr = C.ps("ps_tr", [128, 8, 128], BF16)
        ps_a, b_ps_a = C.ps("ps_a", [128, 512], F32)
        ps_b, b_ps_b = C.ps("ps_b", [128, 512], F32)
        ps_c, b_ps_c = C.ps("ps_c", [128, 512], F32)
        ps_d, b_ps_d = C.ps("ps_d", [128, 512], F32)
        ps_e, b_ps_e = C.ps("ps_e", [128, 512], F32)
        ps_f, b_ps_f = C.ps("ps_f", [128, 512], F32)
        ps_g, b_ps_g = C.ps("ps_g", [128, 512], F32)

        idf, b_idf = C.al("idf", [128, 128], F32)
        idb, b_idb = C.al("idb", [128, 128], BF16)
        epsT, b_eps = C.al("epsT", [128, 1], F32)
        hfvT, b_hfv = C.al("hfvT", [128, 1], F32)
        P.dma("sp", lambda e: e.dma_start(out=idf[:], in_=identf), writes=[b_idf])
        P.dma("sp", lambda e: e.dma_start(out=hfvT[:], in_=hfv), writes=[b_hfv])
        P.op("dve", lambda e: e.tensor_copy(out=idb[:], in_=idf[:]), reads=[b_idf], writes=[b_idb])
        P.op("dve", lambda e: e.memset(epsT[:], EPS), writes=[b_eps])

        m_const = C.mark()
        kvS, b_kvS = C.al("kvS", [NS, 768], F32)
        usS, b_usS = C.al("usS", [NS, 512], F32)
        qtmS, b_qtmS = C.al("qtmS", [NS, 512], F32)
        gatesS, b_gatesS = C.al("gatesS", [NS, 24], F32)
        QbdAll, b_QbdAll = C.al("QbdAll", [128, NS, 8], BF16)
        catS, b_catS = C.al("catS", [128, 8, NS], BF16)
        m_samp = C.mark()
        KE = [C.al("KE%d" % k, [128, SEQ], BF16) for k in range(2)]
        KwT, b_KwT = C.al("KwT", [128, NOWN + 512], BF16)
        XTc, b_XTc = C.al("XTc", [128, 2, SEQ + 32], BF16)
        Vs, b_Vs = C.al("Vs", [128, 32, 2, 68], BF16)
        Vw, b_Vw = C.al("Vw", [128, 20, 2, 68], BF16)
        gates, b_gates = C.al("gates", [128, 16, 24], F32)
        kcT, b_kcT = C.al("kcT", [128, 256], BF16)
        vcx, b_vcx = C.al("vcx", [128, 2, 2, 132], BF16)
        m_persist = C.mark()

        c_t, b_ct = C.al("c_t", [1 + NS, D], F32)
        c_b, b_cb = C.al("c_b", [1 + NS, D], F32)
        cT, b_cT = C.al("cT", [128, 8, 1 + NS], F32)
        badaT, b_badaT = C.al("badaT", [1 + NS, 6 * D], F32)
        wst = [C.al("wst%d" % i, [128, 8, 512], F32) for i in range(4)]
        wst_h = [[Buf("wsth%d_%d" % (i, j)) for j in range(2)] for i in range(4)]
        mo = [C.al("mo%d" % i, [1 + NS, 512], F32) for i in range(2)]
        P.dma("sp", lambda e: e.dma_start(out=c_t[:], in_=cin), writes=[b_ct])
        P.dma("sp", lambda e: e.dma_start(out=badaT[:], in_=b_ada.partition_broadcast(1 + NS)), writes=[b_badaT])
        P.op("act", lambda e: e.activation(out=c_b[:], in_=c_t[:], func=AF.Silu), reads=[b_ct], writes=[b_cb])
        for c in range(8):
            P.op("pe", lambda e, c=c: e.transpose(out=ps_c[:, c * (1 + NS):(c + 1) * (1 + NS)], in_=c_b[:, c * 128:(c + 1) * 128],
                                                  identity=idf[0:1 + NS, 0:1 + NS]),
                 reads=[b_cb, b_idf], writes=[b_ps_c])
        P.op("dve", lambda e: e.tensor_copy(out=cT[:], in_=ps_c[:, 0:8 * (1 + NS)].rearrange("p (a b) -> p a b", a=8)),
             reads=[b_ps_c], writes=[b_cT])
        w_ada_v = w_ada.rearrange("(c p) n -> p c n", p=128)
        for blk in range(12):
            (wf, b_wf), (m_o, b_mo) = wst[blk % 4], mo[blk % 2]
            psx, b_psx = (ps_a, b_ps_a) if blk % 2 == 0 else (ps_b, b_ps_b)
            sl = slice(blk * 512, (blk + 1) * 512)
            q_ = "sp" if blk % 2 == 0 else "act"
            hb2 = wst_h[blk % 4]
            for hh in range(2):
                P.dma(q_, lambda e, sl=sl, wf=wf, hh=hh: e.dma_start(out=wf[:, hh * 4:(hh + 1) * 4, :], in_=w_ada_v[:, hh * 4:(hh + 1) * 4, sl]),
                      writes=[hb2[hh]])
            for c in range(8):
                P.op("pe", lambda e, c=c, wf=wf, psx=psx: e.matmul(psx[0:1 + NS, :], lhsT=cT[:, c, :], rhs=wf[:, c, :],
                                                                    start=(c == 0), stop=(c == 7)),
                     reads=[b_cT, hb2[c // 4]], writes=[b_psx])
            P.op("dve", lambda e, sl=sl, psx=psx, m_o=m_o: e.tensor_tensor(out=m_o[:], in0=psx[0:1 + NS, :],
                                                                            in1=badaT[:, sl], op=ALU.add),
                 reads=[b_psx, b_badaT], writes=[b_mo])
            P.dma("sp", lambda e, sl=sl, m_o=m_o: e.dma_start(out=mods_d[:, sl], in_=m_o[:]), reads=[b_mo], writes=[b_mods_d])

        def load_mod(i, dstP, b_dstP, dstS, b_dstS):
            P.dma("sp", lambda e: e.dma_start(out=dstP[:], in_=mods_d[0:1, i * D:(i + 1) * D].partition_broadcast(128)),
                  reads=[b_mods_d], writes=[b_dstP])
            P.dma("sp", lambda e: e.dma_start(out=dstS[:], in_=mods_d[1:1 + NS, i * D:(i + 1) * D]),
                  reads=[b_mods_d], writes=[b_dstS])

        if upto == 'A':
            P.build(st)
            return nc
        P.barrier()
        C.reset(m_persist)

        winb, b_winb = C.al("winb", [128, 8, D_IN], BF16)
        wstB, b_wstB = C.al("wstB", [128, 8, 256], F32)
        sh1P, b_sh1P = C.al("sh1P", [128, D], F32)
        A1P, b_A1P = C.al("A1P", [128, D], F32)
        sh1S, b_sh1S = C.al("sh1S", [NS, D], F32)
        A1S, b_A1S = C.al("A1S", [NS, D], F32)
        g1P, b_g1P = C.al("g1P", [128, D], F32)
        load_mod(0, sh1P, b_sh1P, sh1S, b_sh1S)
        load_mod(1, A1P, b_A1P, A1S, b_A1S)
        P.dma("sp", lambda e: e.dma_start(out=g1P[:], in_=norm1_g.partition_broadcast(128)), writes=[b_g1P])
        P.op("dve", lambda e: e.scalar_tensor_tensor(out=A1P[:], in0=A1P[:], scalar=1.0, in1=g1P[:],
                                                     op0=ALU.add, op1=ALU.mult),
             reads=[b_A1P, b_g1P], writes=[b_A1P])
        P.op("dve", lambda e: e.scalar_tensor_tensor(out=A1S[:], in0=A1S[:], scalar=1.0, in1=g1P[0:NS, :],
                                                     op0=ALU.add, op1=ALU.mult),
             reads=[b_A1S, b_g1P], writes=[b_A1S])
        w_in_v = w_in.rearrange("(c p) n -> p c n", p=128)
        for c0 in range(0, D_IN, 256):
            c1 = min(D_IN, c0 + 256)
            P.dma("sp", lambda e, c0=c0, c1=c1: e.dma_start(out=wstB[:, :, 0:c1 - c0], in_=w_in_v[:, :, c0:c1]),
                  writes=[b_wstB])
            if c0 < 512:
                for hh in range(4):
                    h = c0 // 64 + hh
                    k, g = h // 4, h % 4
                    dc = g * 128 + k * 64
                    P.op("pool", lambda e, hh=hh, dc=dc: e.tensor_copy(out=winb[:, :, dc:dc + 64],
                                                                       in_=wstB[:, :, hh * 64:(hh + 1) * 64]),
                         reads=[b_wstB], writes=[b_winb])
            else:
                P.op("pool", lambda e, c0=c0, c1=c1: e.tensor_copy(out=winb[:, :, c0:c1], in_=wstB[:, :, 0:c1 - c0]),
                     reads=[b_wstB], writes=[b_winb])

        P.op("pool", lambda e: e.memset(Vs[:], 0.0), writes=[b_Vs])
        P.op("pool", lambda e: e.memset(Vw[:], 0.0), writes=[b_Vw])
        P.op("pool", lambda e: e.memset(Vs[:, :, :, 64:65], 1.0), writes=[b_Vs])
        P.op("pool", lambda e: e.memset(Vw[:, :, :, 64:65], 1.0), writes=[b_Vw])
        for k in range(2):
            r0 = 64 if k == 0 else 0
            for c0 in range(0, SEQ, 2048):
                P.dma("sp", lambda e, r0=r0, c0=c0: e.dma_start(out=wstB[r0:r0 + 64, :, :].rearrange("p a b -> p (a b)"),
                                                                 in_=etab[r0:r0 + 64, c0:c0 + 2048]), writes=[b_wstB])
                P.op("pool", lambda e, r0=r0, c0=c0, k=k: e.tensor_copy(
                    out=KE[k][0][r0:r0 + 64, c0:c0 + 2048], in_=wstB[r0:r0 + 64, :, :].rearrange("p a b -> p (a b)")),
                    reads=[b_wstB], writes=[KE[k][1]])

        if upto == 'B1':
            P.build(st)
            return nc
        xt = [C.al("xt%d" % i, [128, D], F32) for i in range(2)]
        sss = [C.al("ss%d" % i, [128, 1], F32) for i in range(2)]
        rstds = [C.al("rstd%d" % i, [128, 1], F32) for i in range(2)]
        h32s = [C.al("h32%d" % i, [128, D], F32) for i in range(2)]
        hbs = [C.al("hb%d" % i, [128, D], BF16) for i in range(2)]
        hTs = [C.al("hT%d" % i, [128, 8, 128], BF16) for i in range(2)]
        kvt = [C.al("kvt%d" % i, [128, 792], F32) for i in range(2)]
        sig, b_sig = C.al("sig", [128, 4, 128], F32)
        qtmp, b_qtmp = C.al("qtmp", [128, 4, 128], BF16)
        utmp, b_utmp = C.al("utmp", [128, 4, 128], BF16)
        u32, b_u32 = C.al("u32", [128, 4, 32], F32)
        ps_q, b_ps_q = ps_c[:].rearrange("p (a b) -> p a b", a=4), b_ps_c
        ps_kv, b_ps_kv = ps_d[:].rearrange("p (a b) -> p a b", a=4), b_ps_d
        ps_ga, b_ps_ga = ps_e[:].rearrange("p (a b) -> p a b", a=4), b_ps_e
        ps_gg, b_ps_gg = ps_f[:].rearrange("p (a b) -> p a b", a=4), b_ps_f

        def glu_part(i, kind):
            hT, b_hT = hTs[i % 2]
            for j in range(4):
                for c in range(8):
                    P.op("pe", lambda e, c=c, j=j: e.matmul(ps_ga[:, j, :], lhsT=winb[:, c, 1304 + j * 128:1304 + (j + 1) * 128],
                                                            rhs=hT[:, c, :], start=(c == 0), stop=(c == 7)),
                         reads=[b_hT, b_winb], writes=[b_ps_ga])
            for j in range(4):
                for c in range(8):
                    P.op("pe", lambda e, c=c, j=j: e.matmul(ps_gg[:, j, :], lhsT=winb[:, c, 1816 + j * 128:1816 + (j + 1) * 128],
                                                            rhs=hT[:, c, :], start=(c == 0), stop=(c == 7)),
                         reads=[b_hT, b_winb], writes=[b_ps_gg])
            P.op("act", lambda e: e.activation(out=sig[:], in_=ps_gg[:], func=AF.Sigmoid), reads=[b_ps_gg], writes=[b_sig])
            if kind == "own":
                P.op("dve", lambda e: e.tensor_tensor(out=utmp[:], in0=ps_ga[:], in1=sig[:], op=ALU.mult),
                     reads=[b_ps_ga, b_sig], writes=[b_utmp])
                P.dma("pool", lambda e: e.dma_start(out=u_d[:, :, 30 + i * 128:30 + (i + 1) * 128], in_=utmp[:]),
                      reads=[b_utmp], writes=[b_u_d])
                if i == 15:
                    P.op("dve", lambda e: e.tensor_tensor(out=u32[:, :, 0:30], in0=ps_ga[:, :, 98:128], in1=sig[:, :, 98:128],
                                                          op=ALU.mult),
                         reads=[b_ps_ga, b_sig], writes=[b_u32])
            else:
                P.op("dve", lambda e: e.scalar_tensor_tensor(out=utmp[:, :, 0:30], in0=ps_ga[:, :, 98:128],
                                                             scalar=hfvT[:, 0:1], in1=sig[:, :, 98:128],
                                                             op0=ALU.mult, op1=ALU.mult),
                     reads=[b_ps_ga, b_sig, b_hfv], writes=[b_utmp])
                P.dma("pool", lambda e: e.dma_start(out=u_d[:, :, 0:30], in_=utmp[:, :, 0:30]),
                      reads=[b_utmp], writes=[b_u_d])

        def token_tile(i, kind):
            n = NS if kind == "sample" else 128
            hT, b_hT = hTs[i % 2]
            hb, b_hb = hbs[i % 2]
            h32, b_h32 = h32s[i % 2]
            ss, b_ss = sss[i % 2]
            rstd, b_rstd = rstds[i % 2]
            (x_t, b_x) = xt[i % 2]
            (kv_t, b_kv) = kvt[i % 2]
            src = xs if kind == "sample" else xc[i * 128:(i + 1) * 128, :]
            A1, b_A1 = (A1S, b_A1S) if kind == "sample" else (A1P, b_A1P)
            sh1, b_sh1 = (sh1S, b_sh1S) if kind == "sample" else (sh1P, b_sh1P)
            P.dma("sp", lambda e: e.dma_start(out=x_t[0:n, :], in_=src), writes=[b_x])
            P.op("act", lambda e: e.activation(out=h32[0:n, :], in_=x_t[0:n, :], func=AF.Square, accum_out=ss[0:n, :]),
                 reads=[b_x], writes=[b_h32, b_ss])
            P.op("act", lambda e: e.activation(out=rstd[0:n, :], in_=ss[0:n, :], func=AF.Sqrt, scale=1.0 / D,
                                               bias=epsT[0:n, :]),
                 reads=[b_ss, b_eps], writes=[b_rstd])
            P.op("dve", lambda e: e.reciprocal(out=rstd[0:n, :], in_=rstd[0:n, :]), reads=[b_rstd], writes=[b_rstd])
            P.op("dve", lambda e: e.scalar_tensor_tensor(out=h32[0:n, :], in0=x_t[0:n, :], scalar=rstd[0:n, :],
                                                         in1=A1[0:n, :], op0=ALU.mult, op1=ALU.mult),
                 reads=[b_x, b_rstd, b_A1], writes=[b_h32])
            P.op("pool", lambda e: e.tensor_tensor(out=hb[0:n, :], in0=h32[0:n, :], in1=sh1[0:n, :], op=ALU.add),
                 reads=[b_h32, b_sh1], writes=[b_hb])
            for c in range(8):
                P.op("pe", lambda e, c=c: e.transpose(out=ps_tr[:, c, 0:n], in_=hb[0:n, c * 128:(c + 1) * 128],
                                                      identity=idb[0:n, 0:n]),
                     reads=[b_hb, b_idb], writes=[b_ps_tr])
            P.op("act", lambda e: e.copy(out=hT[:, :, 0:n], in_=ps_tr[:, :, 0:n]), reads=[b_ps_tr], writes=[b_hT])
            ncol_b = 256 if kind == "other" else 280
            for c in range(8):
                P.op("pe", lambda e, c=c: e.matmul(ps_a[0:n, :], lhsT=hT[:, c, 0:n], rhs=winb[:, c, 512:1024],
                                                   start=(c == 0), stop=(c == 7)),
                     reads=[b_hT, b_winb], writes=[b_ps_a])
            for c in range(8):
                P.op("pe", lambda e, c=c: e.matmul(ps_b[0:n, 0:ncol_b], lhsT=hT[:, c, 0:n],
                                                   rhs=winb[:, c, 1024:1024 + ncol_b],
                                                   start=(c == 0), stop=(c == 7)),
                     reads=[b_hT, b_winb], writes=[b_ps_b])
            P.op("dve", lambda e: e.tensor_copy(out=kv_t[0:n, 0:512], in_=ps_a[0:n, :]), reads=[b_ps_a], writes=[b_kv])
            P.op("act", lambda e: e.copy(out=kv_t[0:n, 512:512 + ncol_b], in_=ps_b[0:n, 0:ncol_b]),
                 reads=[b_ps_b], writes=[b_kv])
            dst = o_kv_s if kind == "sample" else o_kv_p[i * 128:(i + 1) * 128, :]
            P.dma("pool", lambda e: e.dma_start(out=dst, in_=kv_t[0:n, 0:768]), reads=[b_kv], final=True)
            if kind == "sample":
                P.op("dve", lambda e: e.tensor_copy(out=kvS[:], in_=kv_t[0:NS, 0:768]), reads=[b_kv], writes=[b_kvS])
                P.op("act", lambda e: e.activation(out=gatesS[:], in_=kv_t[0:NS, 768:792], func=AF.Sigmoid), reads=[b_kv], writes=[b_gatesS])
                for c in range(8):
                    P.op("pe", lambda e, c=c: e.matmul(ps_a[0:NS, :], lhsT=hT[:, c, 0:NS], rhs=winb[:, c, 0:512], start=(c == 0), stop=(c == 7)),
                         reads=[b_hT, b_winb], writes=[b_ps_a])
                P.op("dve", lambda e: e.tensor_copy(out=qtmS[:], in_=ps_a[0:NS, :]), reads=[b_ps_a], writes=[b_qtmS])
                for g in range(4):
                    for c in range(8):
                        P.op("pe", lambda e, c=c, g=g: e.matmul(ps_q[:, g, 0:NS], lhsT=winb[:, c, g * 128:(g + 1) * 128], rhs=hT[:, c, 0:NS],
                                                                start=(c == 0), stop=(c == 7)),
                             reads=[b_hT, b_winb], writes=[b_ps_q])
                P.op("dve", lambda e: e.memset(QbdAll[:], 0.0), writes=[b_QbdAll])
                P.op("dve", lambda e: e.tensor_copy(out=QbdAll[0:64, :, 0:4], in_=ps_q[0:64, :, 0:NS].rearrange("p g b -> p b g")),
                     reads=[b_ps_q], writes=[b_QbdAll])
                P.op("dve", lambda e: e.tensor_copy(out=QbdAll[64:128, :, 4:8], in_=ps_q[64:128, :, 0:NS].rearrange("p g b -> p b g")),
                     reads=[b_ps_q], writes=[b_QbdAll])
                return kv_t, b_kv
            if 'V' not in SKIP:
              P.op("pool", lambda e: e.tensor_copy(out=Vs[:, i, :, 0:64],
                                                 in_=kv_t[:, 384:512].rearrange("p (k d) -> p k d", k=2)),
                 reads=[b_kv], writes=[b_Vs])
            wslot = None
            if kind == "own":
                wslot = 4 + i
            elif i >= 28:
                wslot = i - 28
            if wslot is not None and 'V' not in SKIP:
                P.op("pool", lambda e: e.tensor_copy(out=Vw[:, wslot, :, 0:64],
                                                     in_=kv_t[:, 640:768].rearrange("p (k d) -> p k d", k=2)),
                     reads=[b_kv], writes=[b_Vw])
            if kind == "own":
                P.op("act", lambda e: e.activation(out=gates[:, i, :], in_=kv_t[:, 768:792], func=AF.Sigmoid),
                     reads=[b_kv], writes=[b_gates])
            if 'CH' in SKIP:
                return kv_t, b_kv
            for j, c0 in enumerate((512, 640, 768, 1024)):
                for c in range(8):
                    P.op("pe", lambda e, c=c, j=j, c0=c0: e.matmul(ps_kv[:, j, :], lhsT=winb[:, c, c0:c0 + 128],
                                                                   rhs=hT[:, c, :], start=(c == 0), stop=(c == 7)),
                         reads=[b_hT, b_winb], writes=[b_ps_kv])
            tsl = slice(i * 128, (i + 1) * 128)
            P.op("dve", lambda e: e.tensor_copy(out=XTc[:, :, tsl], in_=ps_kv[:, 0:2, :]), reads=[b_ps_kv], writes=[b_XTc])
            if i == 0:
                P.op("dve", lambda e: e.tensor_copy(out=XTc[:, :, SEQ:SEQ + 32], in_=ps_kv[:, 0:2, 0:32]),
                     reads=[b_ps_kv], writes=[b_XTc])
            if 'KE0' not in SKIP:
                P.op("dve", lambda e: e.tensor_copy(out=KE[0][0][0:64, tsl], in_=ps_kv[0:64, 2, :]), reads=[b_ps_kv], writes=[KE[0][1]])
            if 'KE1' not in SKIP:
                P.op("dve", lambda e: e.tensor_copy(out=KE[1][0][64:128, tsl], in_=ps_kv[64:128, 2, :]), reads=[b_ps_kv], writes=[KE[1][1]])
            if wslot is not None:
                wsl = slice(wslot * 128, (wslot + 1) * 128)
                P.op("dve", lambda e: e.tensor_copy(out=KwT[:, wsl], in_=ps_kv[:, 3, :]), reads=[b_ps_kv], writes=[b_KwT])
            if kind == "own":
                for g in range(4):
                    for c in range(8):
                        P.op("pe", lambda e, c=c, g=g: e.matmul(ps_q[:, g, :], lhsT=winb[:, c, g * 128:(g + 1) * 128],
                                                                rhs=hT[:, c, :], start=(c == 0), stop=(c == 7)),
                             reads=[b_hT, b_winb], writes=[b_ps_q])
                P.op("dve", lambda e: e.tensor_copy(out=qtmp[:], in_=ps_q[:]), reads=[b_ps_q], writes=[b_qtmp])
                for k in range(2):
                    P.dma("pool", lambda e, k=k: e.dma_start(
                        out=qT_d[k, i, :, :], in_=qtmp[k * 64:(k + 1) * 64, :, :].rearrange("p a b -> p (a b)")),
                        reads=[b_qtmp], writes=[b_qT_d])
            if (kind == "own" or i == 31) and 'GLU' not in SKIP:
                glu_part(i, kind)
            return kv_t, b_kv

        for i in range(16, 32):
            token_tile(i, "other")
        if upto == 'B2':
            P.build(st)
            return nc
        for i in range(16):
            token_tile(i, "own")
        if upto == 'B3':
            P.build(st)
            return nc

        uo, b_uo = C.al("uo", [32, 512], F32)
        for j in range(4):
            P.op("pe", lambda e, j=j: e.transpose(out=ps_g[0:30, j * 128:(j + 1) * 128], in_=u32[:, j, 0:30],
                                                  identity=idf[:]),
                 reads=[b_u32, b_idf], writes=[b_ps_g])
        P.op("dve", lambda e: e.tensor_copy(out=uo[0:30, :], in_=ps_g[0:30, :]), reads=[b_ps_g], writes=[b_uo])
        P.dma("pool", lambda e: e.dma_start(out=o_conv_p, in_=uo[0:30, :]), reads=[b_uo], final=True)

        kv_s_t, b_kv_s = token_tile(32, "sample")
        hT, b_hT = hTs[0]
        us, b_us = C.al("us", [NS, 512], F32)
        sgs, b_sgs = C.al("sgs", [NS, 512], F32)
        for c in range(8):
            P.op("pe", lambda e, c=c: e.matmul(ps_a[0:NS, :], lhsT=hT[:, c, 0:NS], rhs=winb[:, c, 1304:1816],
                                               start=(c == 0), stop=(c == 7)),
                 reads=[b_hT, b_winb], writes=[b_ps_a])
        for c in range(8):
            P.op("pe", lambda e, c=c: e.matmul(ps_b[0:NS, :], lhsT=hT[:, c, 0:NS], rhs=winb[:, c, 1816:2328],
                                               start=(c == 0), stop=(c == 7)),
                 reads=[b_hT, b_winb], writes=[b_ps_b])
        P.op("act", lambda e: e.activation(out=sgs[:], in_=ps_b[0:NS, :], func=AF.Sigmoid), reads=[b_ps_b], writes=[b_sgs])
        P.op("dve", lambda e: e.tensor_tensor(out=us[:], in0=ps_a[0:NS, :], in1=sgs[:], op=ALU.mult),
             reads=[b_ps_a, b_sgs], writes=[b_us])
        P.op("dve", lambda e: e.tensor_copy(out=usS[:], in_=us[:]), reads=[b_us], writes=[b_usS])
        P.dma("pool", lambda e: e.dma_start(out=o_win_s[:, 0:511, :], in_=state_win[:, 1:512, :]), final=True)
        P.dma("pool", lambda e: e.dma_start(out=o_win_s[:, 511, :], in_=kv_s_t[0:NS, 512:768]), reads=[b_kv_s], final=True)
        P.dma("pool", lambda e: e.dma_start(out=o_conv_s[:, 0:29, :], in_=state_conv[:, 1:30, :]), final=True)
        P.dma("pool", lambda e: e.dma_start(out=o_conv_s[:, 29, :], in_=us[:]), reads=[b_us], final=True)

        P.barrier()
        C.reset(m_persist)
        catT, b_catT = C.al("catT", [128, 8, NOWN], BF16)
        m_persist2 = C.mark()

        if upto == 'B':
            P.build(st)
            return nc
        uTb, b_uTb = C.al("uTb", [128, 4, 30 + NOWN], BF16)
        wdw, b_wdw = C.al("wdw", [128, 4, 31], F32)
        bdw, b_bdw = C.al("bdw", [128, 4], F32)
        diag, b_diag = C.al("diag", [128, 4, 31, 128], BF16)
        lng, b_lng = C.al("lng", [128, 512], F32)
        lnb, b_lnb = C.al("lnb", [128, 512], F32)
        gco, b_gco = C.al("gco", [128, 512], F32)
        yT, b_yT = C.al("yT", [128, 4, 512], F32)
        zt, b_zt = C.al("zt", [128, 512], F32)
        zj, b_zj = C.al("zj", [128, 512], F32)
        zb, b_zb = C.al("zb", [128, 512], BF16)
        st6, b_st6 = C.al("st6", [128, 6], F32)
        mv, b_mv = C.al("mv", [128, 2], F32)
        rs2, b_rs2 = C.al("rs2", [128, 1], F32)
        ss2, b_ss2 = C.al("ss2", [128, 1], F32)
        for j in range(4):
            P.dma("sp", lambda e, j=j: e.dma_start(out=uTb[:, j, :], in_=u_d[:, j, :]), reads=[b_u_d], writes=[b_uTb])
        P.dma("sp", lambda e: e.dma_start(out=wdw[:], in_=wdwT), writes=[b_wdw])
        P.dma("sp", lambda e: e.dma_start(out=bdw[:], in_=bdwT), writes=[b_bdw])
        P.dma("sp", lambda e: e.dma_start(out=lng[:], in_=conv_ln_g.partition_broadcast(128)), writes=[b_lng])
        P.dma("sp", lambda e: e.dma_start(out=lnb[:], in_=conv_ln_b.partition_broadcast(128)), writes=[b_lnb])
        P.dma("sp", lambda e: e.dma_start(out=gco[:], in_=g_conv_out.partition_broadcast(128)), writes=[b_gco])
        for j in range(4):
            P.op("dve", lambda e, j=j: e.tensor_tensor(out=diag[:, j, :, :],
                                                       in0=idb[:].unsqueeze(1).to_broadcast([128, 31, 128]),
                                                       in1=wdw[:, j, :].unsqueeze(2).to_broadcast([128, 31, 128]),
                                                       op=ALU.mult),
                 reads=[b_idb, b_wdw], writes=[b_diag])
        ps_tm, b_ps_tm = ps_g, b_ps_g
        for G in range(4):
            for j in range(4):
                psx, b_psx = [(ps_a, b_ps_a), (ps_b, b_ps_b)][j % 2]
                for k in range(31):
                    P.op("pe", lambda e, j=j, k=k, psx=psx, G=G: e.matmul(psx[:], lhsT=diag[:, j, k, :],
                                                                          rhs=uTb[:, j, G * 512 + k:G * 512 + k + 512],
                                                                          start=(k == 0), stop=(k == 30)),
                         reads=[b_diag, b_uTb], writes=[b_psx])
                P.op("act", lambda e, j=j, psx=psx: e.activation(out=yT[:, j, :], in_=psx[:], func=AF.Identity,
                                                                 bias=bdw[:, j:j + 1]),
                     reads=[b_psx, b_bdw], writes=[b_yT])
            for t in range(4):
                tile_i = G * 4 + t
                for j in range(4):
                    P.op("pe", lambda e, j=j, t=t: e.transpose(out=ps_tm[:, j * 128:(j + 1) * 128],
                                                               in_=yT[:, j, t * 128:(t + 1) * 128], identity=idf[:]),
                         reads=[b_yT, b_idf], writes=[b_ps_tm])
                P.op("dve", lambda e: e.bn_stats(out=st6[:], in_=ps_tm[:]), reads=[b_ps_tm], writes=[b_st6])
                P.op("dve", lambda e: e.bn_aggr(out=mv[:], in_=st6[:]), reads=[b_st6], writes=[b_mv])
                P.op("act", lambda e: e.activation(out=rs2[:], in_=mv[:, 1:2], func=AF.Sqrt, bias=epsT[:]),
                     reads=[b_mv, b_eps], writes=[b_rs2])
                P.op("dve", lambda e: e.reciprocal(out=rs2[:], in_=rs2[:]), reads=[b_rs2], writes=[b_rs2])
                P.op("dve", lambda e: e.tensor_scalar(out=zt[:], in0=ps_tm[:], scalar1=mv[:, 0:1], scalar2=rs2[:, 0:1],
                                                      op0=ALU.subtract, op1=ALU.mult),
                     reads=[b_ps_tm, b_mv, b_rs2], writes=[b_zt])
                P.op("pool", lambda e: e.tensor_tensor(out=zt[:], in0=zt[:], in1=lng[:], op=ALU.mult),
                     reads=[b_zt, b_lng], writes=[b_zt])
                P.op("pool", lambda e: e.tensor_tensor(out=zt[:], in0=zt[:], in1=lnb[:], op=ALU.add),
                     reads=[b_zt, b_lnb], writes=[b_zt])
                P.op("act", lambda e: e.activation(out=zt[:], in_=zt[:], func=AF.Silu), reads=[b_zt], writes=[b_zt])
                if stage == 2:
                    P.dma("pool", lambda e, tile_i=tile_i: e.dma_start(out=dbg_conv[tile_i * 128:(tile_i + 1) * 128, :], in_=zt[:]),
                          reads=[b_zt], final=True)
                P.op("act", lambda e: e.activation(out=zj[:], in_=zt[:], func=AF.Square, accum_out=ss2[:]),
                     reads=[b_zt], writes=[b_zj, b_ss2])
                P.op("act", lambda e: e.activation(out=ss2[:], in_=ss2[:], func=AF.Sqrt, scale=1.0 / 512, bias=epsT[:]),
                     reads=[b_ss2, b_eps], writes=[b_ss2])
                P.op("dve", lambda e: e.reciprocal(out=ss2[:], in_=ss2[:]), reads=[b_ss2], writes=[b_ss2])
                P.op("dve", lambda e: e.scalar_tensor_tensor(out=zb[:], in0=zt[:], scalar=ss2[:, 0:1], in1=gco[:],
                                                             op0=ALU.mult, op1=ALU.mult),
                     reads=[b_zt, b_ss2, b_gco], writes=[b_zb])
                for j in range(4):
                    P.op("pe", lambda e, j=j: e.transpose(out=ps_tr[:, j, :], in_=zb[:, j * 128:(j + 1) * 128], identity=idb[:]),
                         reads=[b_zb, b_idb], writes=[b_ps_tr])
                P.op("act", lambda e, tile_i=tile_i: e.copy(out=catT[:, 4:8, tile_i * 128:(tile_i + 1) * 128], in_=ps_tr[:, 0:4, :]),
                     reads=[b_ps_tr], writes=[b_catT])
        P.barrier()
        C.reset(m_persist2)

        if upto == 'C':
            P.build(st)
            return nc
        w1b, b_w1b = C.al("w1b", [128, 2, 32, 128], BF16)
        w1s, b_w1s = C.al("w1s", [128, 16, 128], F32)
        pos2, b_pos2 = C.al("pos2", [128, 2, 32], F32)
        pos2b, b_pos2b = C.al("pos2b", [128, 2, 32], BF16)
        posb, b_posb = C.al("posb", [128, 2], F32)
        w2s, b_w2s = C.al("w2s", [128, 2, 128], F32)
        w2b, b_w2b = C.al("w2b", [128, 2, 128], BF16)
        hidT, b_hidT = C.al("hidT", [128, 2, 256], BF16)
        ovs, b_ovs = C.al("ovs", [128, 2, 64], F32)
        for c in range(2):
            for hh in range(2):
                P.dma("sp", lambda e, c=c, hh=hh: e.dma_start(out=w1s[:], in_=w1bd[:, c, hh * 16:(hh + 1) * 16, :]), writes=[b_w1s])
                P.op("pool", lambda e, c=c, hh=hh: e.tensor_copy(out=w1b[:, c, hh * 16:(hh + 1) * 16, :], in_=w1s[:]),
                     reads=[b_w1s], writes=[b_w1b])
        P.dma("sp", lambda e: e.dma_start(out=pos2[:], in_=posT2), writes=[b_pos2])
        P.dma("sp", lambda e: e.dma_start(out=w2s[:], in_=w2bd), writes=[b_w2s])
        P.dma("sp", lambda e: e.dma_start(out=ovs[:], in_=ovt), writes=[b_ovs])
        P.op("dve", lambda e: e.tensor_copy(out=pos2b[:], in_=pos2[:]), reads=[b_pos2], writes=[b_pos2b])
        P.op("dve", lambda e: e.tensor_copy(out=w2b[:], in_=w2s[:]), reads=[b_w2s], writes=[b_w2b])
        for c in range(2):
            for s in range(32):
                P.op("pe", lambda e, c=c, s=s: e.matmul(ps_g[:, c:c + 1], lhsT=w1b[:, c, s, :], rhs=pos2b[:, c, s:s + 1],
                                                        start=(s == 0), stop=(s == 31)),
                     reads=[b_w1b, b_pos2b], writes=[b_ps_g])
        P.op("dve", lambda e: e.tensor_copy(out=posb[:], in_=ps_g[:, 0:2]), reads=[b_ps_g], writes=[b_posb])
        for c in range(2):
            psx, b_psx = [(ps_a, b_ps_a), (ps_b, b_ps_b)][c]
            for s in range(32):
                P.op("pe", lambda e, c=c, s=s, psx=psx: e.matmul(psx[:, 0:256], lhsT=w1b[:, c, s, :],
                                                                 rhs=XTc[:, c, s:s + 4096:16],
                                                                 start=(s == 0), stop=(s == 31)),
                     reads=[b_w1b, b_XTc], writes=[b_psx])
            P.op("act", lambda e, c=c, psx=psx: e.activation(out=hidT[:, c, :], in_=psx[:, 0:256], func=AF.Silu,
                                                             bias=posb[:, c:c + 1]),
                 reads=[b_psx, b_posb], writes=[b_hidT])
        P.op("pe", lambda e: e.matmul(ps_c[:, 0:256], lhsT=w2b[:, 0, :], rhs=hidT[:, 0, :], start=True, stop=True),
             reads=[b_w2b, b_hidT], writes=[b_ps_c])
        P.op("dve", lambda e: e.tensor_copy(out=kcT[:], in_=ps_c[:, 0:256]), reads=[b_ps_c], writes=[b_kcT])
        for ch in range(2):
            P.op("pe", lambda e, ch=ch: e.matmul(ps_d[:, ch * 128:(ch + 1) * 128], lhsT=hidT[:, 1, ch * 128:(ch + 1) * 128],
                                                 rhs=w2b[:, 1, :], start=True, stop=True),
                 reads=[b_w2b, b_hidT], writes=[b_ps_d])
        P.op("dve", lambda e: e.tensor_copy(out=vcx[:, :, :, 0:64],
                                            in_=ps_d[:, 0:256].rearrange("p (c k d) -> p c k d", c=2, k=2)),
             reads=[b_ps_d], writes=[b_vcx])
        P.op("dve", lambda e: e.memset(vcx[:, :, :, 64:65], 1.0), writes=[b_vcx])
        for k in range(2):
            P.op("dve", lambda e, k=k: e.tensor_copy(out=vcx[:, :, k, 65:129], in_=ovs[:]), reads=[b_ovs], writes=[b_vcx])
        P.barrier()
        C.reset(m_persist2)

        if upto == 'D':
            P.build(st)
            return nc
        qrow_bc, b_qrow = C.al("qrow_bc", [128, NOWN], F32)
        pes, b_pes = C.al("pes", [128, 2], F32)
        blkn, b_blkn = C.al("blkn", [128, 64], F32)
        iszb, b_iszb = C.al("iszb", [128, 64], F32)
        cur, b_cur = C.al("cur", [128, 16], F32)
        trs, b_trs = C.al("trs", [128, 2, 128], F32)
        tdb, b_tdb = C.al("tdb", [128, 4, 128], BF16)
        tlb, b_tlb = C.al("tlb", [128, 4, 128], BF16)
        nvs, b_nvs = C.al("nvs", [1, 512], F32)
        nvr, b_nvr = C.al("nvr", [1, 512], BF16)
        ones1, b_ones1 = C.al("ones1", [1, 128], BF16)
        gao, b_gao = C.al("gao", [128, 512], F32)
        P.dma("sp", lambda e: e.dma_start(out=qrow_bc[:], in_=qrow.partition_broadcast(128)), writes=[b_qrow])
        P.dma("sp", lambda e: e.dma_start(out=pes[:], in_=pe_slot), writes=[b_pes])
        P.dma("sp", lambda e: e.dma_start(out=blkn[:], in_=blknat.partition_broadcast(128)), writes=[b_blkn])
        P.dma("sp", lambda e: e.dma_start(out=iszb[:], in_=isz.partition_broadcast(128)), writes=[b_iszb])
        P.dma("sp", lambda e: e.dma_start(out=cur[:], in_=curt), writes=[b_cur])
        P.dma("sp", lambda e: e.dma_start(out=trs[:, 0, :], in_=tridiag), writes=[b_trs])
        P.dma("sp", lambda e: e.dma_start(out=trs[:, 1, :], in_=trilo), writes=[b_trs])
        P.dma("sp", lambda e: e.dma_start(out=nvs[:], in_=nvrow), writes=[b_nvs])
        P.dma("sp", lambda e: e.dma_start(out=gao[:], in_=g_attn_out.partition_broadcast(128)), writes=[b_gao])
        P.op("dve", lambda e: e.tensor_copy(out=tdb[:], in_=trs[:, 0, :].unsqueeze(1).to_broadcast([128, 4, 128])),
             reads=[b_trs], writes=[b_tdb])
        P.op("dve", lambda e: e.tensor_copy(out=tlb[:], in_=trs[:, 1, :].unsqueeze(1).to_broadcast([128, 4, 128])),
             reads=[b_trs], writes=[b_tlb])
        P.op("dve", lambda e: e.tensor_copy(out=nvr[:], in_=nvs[:]), reads=[b_nvs], writes=[b_nvr])
        P.op("dve", lambda e: e.memset(ones1[:], 1.0), writes=[b_ones1])
        tdb_f = tdb.rearrange("p g q -> p (g q)")
        tlb_f = tlb.rearrange("p g q -> p (g q)")

        QBt = [[C.al("QBt%d_%d" % (r, k), [128, 512], BF16) for k in range(2)] for r in range(2)]
        cmask, b_cmask = C.al("cmask", [128, 2, 128], BF16)
        vmask, b_vmask = C.al("vmask", [128, 64], F32)
        fmask, b_fmask = C.al("fmask", [128, 64], F32)
        ndt, b_ndt = C.al("ndt", [128, 64], F32)
        pc, b_pc = C.al("pc", [128, 2, 512], BF16)
        pTs = [C.al("pT%d" % i, [128, 512], BF16) for i in range(3)]
        rden, b_rden = C.al("rden", [128, 4], F32)
        coef, b_coef = C.al("coef", [128, 4], F32)
        imp, b_imp = C.al("imp", [128, 64], F32)
        impF, b_impF = C.al("impF", [128, 64], F32)
        wk, b_wk = C.al("wk", [128, 64], F32)
        m8, b_m8 = C.al("m8", [128, 16], F32)
        selv, b_selv = C.al("selv", [128, 64], F32)
        Bq, b_Bq = C.al("Bq", [128, 128], BF16)
        oats = [C.al("oat%d" % i, [128, 512], F32) for i in range(2)]
        rden2, b_rden2 = C.al("rden2", [128, 4], F32)
        coef2, b_coef2 = C.al("coef2", [128, 4], F32)
        otmp, b_otmp = C.al("otmp", [128, 4, 64], F32)
        oj, b_oj = C.al("oj", [128, 512], F32)
        ob, b_ob = C.al("ob", [128, 512], BF16)
        ss3, b_ss3 = C.al("ss3", [128, 1], F32)
        ps_S = [(ps_a, b_ps_a), (ps_b, b_ps_b)]
        ps_oc = [(ps_c[:, 0:258].rearrange("p (g n) -> p g n", g=2), b_ps_c),
                 (ps_d[:, 0:258].rearrange("p (g n) -> p g n", g=2), b_ps_d)]
        ps_os = (ps_e[:, 0:260].rearrange("p (g n) -> p g n", g=4), b_ps_e)
        ps_ow = (ps_f[:, 0:260].rearrange("p (g n) -> p g n", g=4), b_ps_f)
        cnt = {"s": 0, "p": 0}

        def next_S():
            r = ps_S[cnt["s"] % 2]
            cnt["s"] += 1
            return r

        def next_pT():
            r = pTs[cnt["p"] % 3]
            cnt["p"] += 1
            return r

        def combine(k, br, O, b_O, first):
            P.op("dve", lambda e: e.tensor_scalar(out=rden[:], in0=O[:, :, 64], scalar1=1e-30, scalar2=None, op0=ALU.add),
                 reads=[b_O], writes=[b_rden])
            P.op("dve", lambda e: e.reciprocal(out=rden[:], in_=rden[:]), reads=[b_rden], writes=[b_rden])
            return

        n_qt = 16 if stage >= 2 else 0
        def attn_pre(qt):
            (oat, b_oat) = oats[qt % 2]
            qsl = slice(qt * 128, (qt + 1) * 128)
            QBk = QBt[qt % 2]
            for k in range(2):
                kh = slice(k * 64, (k + 1) * 64)
                P.dma("sp", lambda e, k=k, kh=kh, QBk=QBk: e.dma_start(out=QBk[k][0][kh, :], in_=qT_d[k, qt, :, :]),
                      reads=[b_qT_d], writes=[QBk[k][1]])
            for ch in range(2):
                P.op("dve", lambda e, ch=ch: e.tensor_scalar(out=cmask[:, ch, :], in0=qrow_bc[:, qsl], scalar1=pes[:, ch:ch + 1],
                                                             scalar2=None, op0=ALU.is_ge),
                     reads=[b_qrow, b_pes], writes=[b_cmask])
            P.op("dve", lambda e: e.tensor_scalar(out=ndt[:], in0=blkn[:], scalar1=cur[:, qt:qt + 1], scalar2=None,
                                                  op0=ALU.subtract),
                 reads=[b_blkn, b_cur], writes=[b_ndt])
            P.op("dve", lambda e: e.tensor_scalar(out=vmask[:], in0=ndt[:], scalar1=0.0, scalar2=None, op0=ALU.is_le),
                 reads=[b_ndt], writes=[b_vmask])
            P.op("dve", lambda e: e.tensor_scalar(out=fmask[:], in0=ndt[:], scalar1=-1.0, scalar2=None, op0=ALU.is_ge),
                 reads=[b_ndt], writes=[b_fmask])
            P.op("dve", lambda e: e.tensor_tensor(out=fmask[:], in0=fmask[:], in1=iszb[:], op=ALU.max),
                 reads=[b_fmask, b_iszb], writes=[b_fmask])
            P.op("dve", lambda e: e.tensor_tensor(out=fmask[:], in0=fmask[:], in1=vmask[:], op=ALU.mult),
                 reads=[b_fmask, b_vmask], writes=[b_fmask])
            def cmp_k(k):
                kh = slice(k * 64, (k + 1) * 64)
                (QB, b_QB) = QBk[k]
                for ch in range(2):
                    (pS, b_pS) = next_S()
                    P.op("pe", lambda e, ch=ch, pS=pS, QB=QB, kh=kh: e.matmul(pS[:], lhsT=kcT[kh, ch * 128:(ch + 1) * 128],
                                                                              rhs=QB[kh, :], start=True, stop=True),
                         reads=[b_kcT, b_QB], writes=[b_pS])
                    P.op("act", lambda e, ch=ch, pS=pS: e.activation(out=pc[:, ch, :], in_=pS[:], func=AF.Exp, scale=SCALE),
                         reads=[b_pS], writes=[b_pc])
                    P.op("dve", lambda e, ch=ch: e.tensor_tensor(
                        out=pc[:, ch, :].rearrange("p (g q) -> p g q", g=4),
                        in0=pc[:, ch, :].rearrange("p (g q) -> p g q", g=4),
                        in1=cmask[:, ch, :].unsqueeze(1).to_broadcast([128, 4, 128]), op=ALU.mult),
                        reads=[b_pc, b_cmask], writes=[b_pc])
                for g in range(4):
                    (O2, b_O2) = ps_oc[g // 2]
                    for ch in range(2):
                        P.op("pe", lambda e, g=g, ch=ch, O2=O2: e.matmul(O2[:, g % 2, :], lhsT=pc[:, ch, g * 128:(g + 1) * 128],
                                                                         rhs=vcx[:, ch, k, 0:129], start=(ch == 0), stop=(ch == 1)),
                             reads=[b_pc, b_vcx], writes=[b_O2])
                for hh in range(2):
                    (O2, b_O2) = ps_oc[hh]
                    P.op("dve", lambda e, hh=hh, O2=O2: e.tensor_scalar(out=rden[:, 2 * hh:2 * hh + 2], in0=O2[:, :, 64],
                                                                        scalar1=1e-30, scalar2=None, op0=ALU.add),
                         reads=[b_O2], writes=[b_rden])
                P.op("dve", lambda e: e.reciprocal(out=rden[:], in_=rden[:]), reads=[b_rden], writes=[b_rden])
                for g in range(4):
                    (O2, b_O2) = ps_oc[g // 2]
                    if g == 0:
                        P.op("dve", lambda e, O2=O2: e.tensor_scalar(out=imp[:], in0=O2[:, 0, 65:129], scalar1=rden[:, 0:1],
                                                                     scalar2=None, op0=ALU.mult),
                             reads=[b_O2, b_rden], writes=[b_imp])
                    else:
                        P.op("dve", lambda e, g=g, O2=O2: e.scalar_tensor_tensor(out=imp[:], in0=O2[:, g % 2, 65:129],
                                                                                 scalar=rden[:, g:g + 1], in1=imp[:],
                                                                                 op0=ALU.mult, op1=ALU.add),
                             reads=[b_O2, b_rden, b_imp], writes=[b_imp])
                P.op("dve", lambda e, k=k: e.tensor_tensor(
                    out=coef[:], in0=rden[:],
                    in1=gates[:, qt, k * 12:(k + 1) * 12].rearrange("p (g b) -> p g b", b=3)[:, :, 0], op=ALU.mult),
                    reads=[b_rden, b_gates], writes=[b_coef])
                for hh in range(2):
                    (O2, b_O2) = ps_oc[hh]
                    h0 = k * 4 + hh * 2
                    P.op("dve", lambda e, hh=hh, O2=O2, h0=h0: e.tensor_tensor(
                        out=oat[:, h0 * 64:(h0 + 2) * 64].rearrange("p (g d) -> p g d", g=2), in0=O2[:, :, 0:64],
                        in1=coef[:, 2 * hh:2 * hh + 2].unsqueeze(2).to_broadcast([128, 2, 64]), op=ALU.mult),
                        reads=[b_O2, b_coef], writes=[b_oat])
                P.op("dve", lambda e: e.tensor_tensor(out=impF[:], in0=imp[:], in1=vmask[:], op=ALU.mult),
                     reads=[b_imp, b_vmask], writes=[b_impF])
                P.op("dve", lambda e: e.scalar_tensor_tensor(out=impF[:], in0=impF[:], scalar=-1.0, in1=vmask[:],
                                                             op0=ALU.add, op1=ALU.add),
                     reads=[b_impF, b_vmask], writes=[b_impF])
                P.op("dve", lambda e: e.scalar_tensor_tensor(out=impF[:], in0=fmask[:], scalar=1e4, in1=impF[:],
                                                             op0=ALU.mult, op1=ALU.add),
                     reads=[b_impF, b_fmask], writes=[b_impF])
                P.op("dve", lambda e: e.max(out=m8[:, 0:8], in_=impF[:]), reads=[b_impF], writes=[b_m8])
                P.op("dve", lambda e: e.match_replace(out=wk[:], in_to_replace=m8[:, 0:8], in_values=impF[:], imm_value=-2.0),
                     reads=[b_impF, b_m8], writes=[b_wk])
                P.op("dve", lambda e: e.max(out=m8[:, 8:16], in_=wk[:]), reads=[b_wk], writes=[b_m8])
                P.op("dve", lambda e: e.tensor_scalar(out=selv[:], in0=impF[:], scalar1=m8[:, 15:16], scalar2=None, op0=ALU.is_ge),
                     reads=[b_impF, b_m8], writes=[b_selv])
                P.op("dve", lambda e: e.scalar_tensor_tensor(out=selv[:], in0=impF[:], scalar=0.0, in1=selv[:],
                                                             op0=ALU.is_ge, op1=ALU.mult),
                     reads=[b_impF, b_selv], writes=[b_selv])
                bc = slice(64, 128) if k == 0 else slice(0, 64)
                P.op("dve", lambda e, bc=bc: e.tensor_scalar(out=Bq[:, bc], in0=selv[:], scalar1=-NEG, scalar2=NEG,
                                                             op0=ALU.mult, op1=ALU.add),
                     reads=[b_selv], writes=[b_Bq])
            cmp_k(0)
            cmp_k(1)
            if stage == 2:
                P.dma("pool", lambda e: e.dma_start(out=dbg_br[0, qsl, :], in_=oat[:]), reads=[b_oat], final=True)
            P.op("pe", lambda e: e.transpose(out=ps_tr[:, 0, :], in_=Bq[:], identity=idb[:]), reads=[b_Bq, b_idb], writes=[b_ps_tr])
            P.op("dve", lambda e, QBk=QBk: e.tensor_copy(out=QBk[0][0][64:128, :].rearrange("p (g q) -> p g q", g=4),
                                                 in_=ps_tr[64:128, 0, :].unsqueeze(1).to_broadcast([64, 4, 128])),
                 reads=[b_ps_tr], writes=[QBk[0][1]])
            P.op("dve", lambda e, QBk=QBk: e.tensor_copy(out=QBk[1][0][0:64, :].rearrange("p (g q) -> p g q", g=4),
                                                 in_=ps_tr[0:64, 0, :].unsqueeze(1).to_broadcast([64, 4, 128])),
                 reads=[b_ps_tr], writes=[QBk[1][1]])
        def attn_main(qt):
            qsl = slice(qt * 128, (qt + 1) * 128)
            QBk = QBt[qt % 2]
            (oat, b_oat) = oats[qt % 2]
            tiles = []
            for k in range(2):
                kts = list(range(qt + 1)) + list(range(16, 32))
                for idx, kt in enumerate(kts):
                    tiles.append(("s", k, idx, kt, len(kts)))
                for w in range(5):
                    tiles.append(("w", k, w, None, 5))

            def emit_qk(t, pS, b_pS):
                kind, k, idx, kt, n = t
                kh = slice(k * 64, (k + 1) * 64)
                (QB, b_QB) = QBk[k]
                if kind == "s":
                    (KEk, b_KEk) = KE[k]
                    P.op("pe", lambda e: e.matmul(pS[:], lhsT=KEk[:, kt * 128:(kt + 1) * 128], rhs=QB[:, :], start=True, stop=(kt != qt)),
                         reads=[b_KEk, b_QB], writes=[b_pS])
                    if kt == qt:
                        P.op("pe", lambda e: e.matmul(pS[:], lhsT=idb[:], rhs=tdb_f, start=False, stop=True),
                             reads=[b_idb, b_tdb], writes=[b_pS])
                else:
                    w = idx
                    col0 = (qt + w) * 128
                    extra = []
                    if w == 0:
                        extra.append("lo")
                    if w == 4:
                        extra.append("di")
                    if qt + w < 4:
                        extra.append("nv")
                    P.op("pe", lambda e: e.matmul(pS[:], lhsT=KwT[kh, col0:col0 + 128], rhs=QB[kh, :], start=True, stop=(len(extra) == 0)),
                         reads=[b_KwT, b_QB], writes=[b_pS])
                    for xi, x in enumerate(extra):
                        last = (xi == len(extra) - 1)
                        if x == "lo":
                            P.op("pe", lambda e, last=last: e.matmul(pS[:], lhsT=idb[:], rhs=tlb_f, start=False, stop=last),
                                 reads=[b_idb, b_tlb], writes=[b_pS])
                        elif x == "di":
                            P.op("pe", lambda e, last=last: e.matmul(pS[:], lhsT=idb[:], rhs=tdb_f, start=False, stop=last),
                                 reads=[b_idb, b_tdb], writes=[b_pS])
                        else:
                            P.op("pe", lambda e, last=last: e.matmul(pS[:], lhsT=ones1[0:1, :], rhs=nvr[0:1, :], start=False, stop=last),
                                 reads=[b_ones1, b_nvr], writes=[b_pS])

            def emit_pv(t, pS, b_pS):
                kind, k, idx, kt, n = t
                (pT, b_pT) = next_pT()
                P.op("act", lambda e: e.activation(out=pT[:], in_=pS[:], func=AF.Exp, scale=SCALE), reads=[b_pS], writes=[b_pT])
                if kind == "s":
                    (Os, b_Os) = ps_os
                    for g in range(4):
                        P.op("pe", lambda e, g=g: e.matmul(Os[:, g, :], lhsT=pT[:, g * 128:(g + 1) * 128], rhs=Vs[:, kt, k, 0:65],
                                                           start=(idx == 0 and g == 0), stop=(idx == n - 1 and g == 3)),
                             reads=[b_pT, b_Vs], writes=[b_Os])
                else:
                    (Ow, b_Ow) = ps_ow
                    w = idx
                    for g in range(4):
                        P.op("pe", lambda e, g=g: e.matmul(Ow[:, g, :], lhsT=pT[:, g * 128:(g + 1) * 128], rhs=Vw[:, qt + w, k, 0:65],
                                                           start=(w == 0 and g == 0), stop=(w == 4 and g == 3)),
                             reads=[b_pT, b_Vw], writes=[b_Ow])
                    if w == 4:
                        combine_k(k)

            def combine_k(k):
                for br, (O, b_O) in ((1, ps_os), (2, ps_ow)):
                    P.op("dve", lambda e, O=O: e.tensor_scalar(out=rden2[:], in0=O[:, :, 64], scalar1=1e-30, scalar2=None, op0=ALU.add),
                         reads=[b_O], writes=[b_rden2])
                    P.op("dve", lambda e: e.reciprocal(out=rden2[:], in_=rden2[:]), reads=[b_rden2], writes=[b_rden2])
                    P.op("dve", lambda e, br=br: e.tensor_tensor(
                        out=coef2[:], in0=rden2[:],
                        in1=gates[:, qt, k * 12:(k + 1) * 12].rearrange("p (g b) -> p g b", b=3)[:, :, br], op=ALU.mult),
                        reads=[b_rden2, b_gates], writes=[b_coef2])
                    P.op("dve", lambda e, O=O: e.tensor_tensor(out=otmp[:], in0=O[:, :, 0:64],
                                                               in1=coef2[:].unsqueeze(2).to_broadcast([128, 4, 64]), op=ALU.mult),
                         reads=[b_O, b_coef2], writes=[b_otmp])
                    P.op("pool", lambda e: e.tensor_tensor(out=oat[:, k * 256:(k + 1) * 256], in0=oat[:, k * 256:(k + 1) * 256],
                                                           in1=otmp[:].rearrange("p g d -> p (g d)"), op=ALU.add),
                         reads=[b_oat, b_otmp], writes=[b_oat])

            prev = None
            for t in tiles:
                (pS, b_pS) = next_S()
                emit_qk(t, pS, b_pS)
                if prev is not None:
                    emit_pv(*prev)
                prev = (t, pS, b_pS)
            emit_pv(*prev)
            if stage == 2:
                P.dma("pool", lambda e: e.dma_start(out=dbg_attn[qsl, :], in_=oat[:]), reads=[b_oat], final=True)
            P.op("act", lambda e: e.activation(out=oj[:], in_=oat[:], func=AF.Square, accum_out=ss3[:]),
                 reads=[b_oat], writes=[b_oj, b_ss3])
            P.op("act", lambda e: e.activation(out=ss3[:], in_=ss3[:], func=AF.Sqrt, scale=1.0 / 512, bias=epsT[:]),
                 reads=[b_ss3, b_eps], writes=[b_ss3])
            P.op("dve", lambda e: e.reciprocal(out=ss3[:], in_=ss3[:]), reads=[b_ss3], writes=[b_ss3])
            P.op("dve", lambda e: e.scalar_tensor_tensor(out=ob[:], in0=oat[:], scalar=ss3[:, 0:1], in1=gao[:],
                                                         op0=ALU.mult, op1=ALU.mult),
                 reads=[b_oat, b_ss3, b_gao], writes=[b_ob])
            for j in range(4):
                P.op("pe", lambda e, j=j: e.transpose(out=ps_tr[:, 4 + j, :], in_=ob[:, j * 128:(j + 1) * 128], identity=idb[:]),
                     reads=[b_ob, b_idb], writes=[b_ps_tr])
            P.op("act", lambda e: e.copy(out=catT[:, 0:4, qsl], in_=ps_tr[:, 4:8, :]), reads=[b_ps_tr], writes=[b_catT])
        if n_qt:
            attn_pre(0)
        for qt in range(n_qt):
            if qt + 1 < n_qt:
                attn_pre(qt + 1)
            attn_main(qt)
        P.barrier()
        C.reset(m_persist2)


        if HAVE_SAMPLE:
            C.reset(m_samp)
            w2b2, b_w2b2 = C.al("w2b2", [128, 2, 128], BF16)
            posb2, b_posb2 = C.al("posb2", [128, 2], F32)
            XTs, b_XTs = C.al("XTs", [128, 2, 16, 512], BF16)
            Eg, b_Eg = C.al("Eg", [128, 4, 128], BF16)
            ovS, b_ovS = C.al("ovS", [128, 4, 128], F32)
            idx, b_idx = C.al("idx", [128, NS * 4], I32)
            pm8, b_pm8 = C.al("pm8", [128, NS * 4], F32)
            idxf, b_idxf = C.al("idxf", [128, NS * 4], F32)
            forc, b_forc = C.al("forc", [128, 1], F32)
            fmaskA, b_fmaskA = C.al("fmaskA", [2, 128], F32)
            ones_b, b_ones_b = C.al("ones_b", [128, 128], F32)
            ones2, b_ones2 = C.al("ones2", [2, 128], F32)
            assert C.off <= m_persist, (C.off, m_persist)
            C.reset(m_persist2)
            w1b2, b_w1b2 = C.al("w1b2", [128, 2, 32, 128], BF16)
            ct = [C.al("ct%d" % i, [128, 4096], F32) for i in range(3)]
            ct_cnt = [0]
            hidS, b_hidS = C.al("hidS", [128, 2, 512], BF16)
            kcS, b_kcS = C.al("kcS", [128, 512], BF16)
            vcS, b_vcS = C.al("vcS", [128, 4, 132], BF16)
            pcS, b_pcS = C.al("pcS", [128, 4, 8], BF16)
            pc32, b_pc32 = C.al("pc32", [128, 4, 8], F32)
            denB, b_denB = C.al("denB", [128, 8], F32)
            pnk32, b_pnk32 = C.al("pnk32", [128, 4, 8], F32)
            pnk, b_pnk = C.al("pnk", [128, 4, 2], BF16)
            pnkf, b_pnkf = C.al("pnkf", [128, 4, 2], F32)
            impAs, b_impAs = C.al("impAs", [2, 128], F32)
            wkA, b_wkA = C.al("wkA", [2, 128], F32)
            m8A, b_m8A = C.al("m8A", [2, 16], F32)
            dthr, b_dthr = C.al("dthr", [2, 2], F32)
            selB, b_selB = C.al("selB", [128, 2], F32)
            BselS, b_BselS = C.al("BselS", [128, 8], BF16)
            KsT = [C.al("KsT%d" % i, [128, 4, 128], BF16) for i in range(2)]
            Vsb, b_Vsb = C.al("Vsb", [128, 64, 132], BF16)
            psS, b_psS = C.al("psS", [128, 512], BF16)
            wt, b_wt = C.al("wt", [128, 4, 256], F32)
            KwS, b_KwS = C.al("KwS", [128, 512], BF16)
            Vwb, b_Vwb = C.al("Vwb", [128, 4, 132], BF16)
            pwS, b_pwS = C.al("pwS", [128, 4, 8], BF16)
            resb, b_resb = C.al("resb", [8, 3, 129], F32)
            for c in range(2):
                for hh in range(2):
                    (stg, b_stg) = ct[hh]
                    sv = stg[:, 0:2048].rearrange("p (a b) -> p a b", a=16)
                    P.dma("sp", lambda e, c=c, hh=hh, sv=sv: e.dma_start(out=sv, in_=w1bd[:, c, hh * 16:(hh + 1) * 16, :]), writes=[b_stg])
                    P.op("pool", lambda e, c=c, hh=hh, sv=sv: e.tensor_copy(out=w1b2[:, c, hh * 16:(hh + 1) * 16, :], in_=sv),
                         reads=[b_stg], writes=[b_w1b2])
            (stg, b_stg) = ct[0]
            P.dma("sp", lambda e: e.dma_start(out=stg[:, 0:256].rearrange("p (a b) -> p a b", a=2), in_=w2bd), writes=[b_stg])
            P.op("dve", lambda e: e.tensor_copy(out=w2b2[:], in_=stg[:, 0:256].rearrange("p (a b) -> p a b", a=2)), reads=[b_stg], writes=[b_w2b2])
            P.dma("sp", lambda e: e.dma_start(out=stg[:, 256:320].rearrange("p (a b) -> p a b", a=2), in_=posT2), writes=[b_stg])
            P.op("dve", lambda e: e.tensor_copy(out=hidS[:, 0, 0:64].rearrange("p (a b) -> p a b", a=2),
                                                in_=stg[:, 256:320].rearrange("p (a b) -> p a b", a=2)), reads=[b_stg], writes=[b_hidS])
            for c in range(2):
                for s in range(32):
                    P.op("pe", lambda e, c=c, s=s: e.matmul(ps_g[:, c:c + 1], lhsT=w1b2[:, c, s, :], rhs=hidS[:, 0, c * 32 + s:c * 32 + s + 1],
                                                            start=(s == 0), stop=(s == 31)),
                         reads=[b_w1b2, b_hidS], writes=[b_ps_g])
            P.op("dve", lambda e: e.tensor_copy(out=posb2[:], in_=ps_g[:, 0:2]), reads=[b_ps_g], writes=[b_posb2])
            (stg1, b_stg1) = ct[1]
            P.dma("sp", lambda e: e.dma_start(out=stg1[:, 0:512].rearrange("p (a b) -> p a b", a=4), in_=egtab), writes=[b_stg1])
            P.op("dve", lambda e: e.tensor_copy(out=Eg[:], in_=stg1[:, 0:512].rearrange("p (a b) -> p a b", a=4)), reads=[b_stg1], writes=[b_Eg])
            P.dma("sp", lambda e: e.dma_start(out=stg1[:, 512:1024].rearrange("p (a b) -> p a b", a=4), in_=ovstab), writes=[b_stg1])
            P.op("dve", lambda e: e.tensor_copy(out=ovS[:], in_=stg1[:, 512:1024].rearrange("p (a b) -> p a b", a=4)), reads=[b_stg1], writes=[b_ovS])
            P.dma("sp", lambda e: e.dma_start(out=idx[:], in_=ptrep), writes=[b_idx])
            P.dma("sp", lambda e: e.dma_start(out=pm8[:], in_=pm8rep), writes=[b_pm8])
            P.dma("sp", lambda e: e.dma_start(out=forc[:], in_=forctab), writes=[b_forc])
            P.dma("sp", lambda e: e.dma_start(out=fmaskA[:], in_=fmaskAtab), writes=[b_fmaskA])
            P.op("dve", lambda e: e.tensor_copy(out=idxf[:], in_=idx[:]), reads=[b_idx], writes=[b_idxf])
            P.op("dve", lambda e: e.scalar_tensor_tensor(out=idxf[:], in0=idxf[:], scalar=8.0, in1=pm8[:], op0=ALU.mult, op1=ALU.add),
                 reads=[b_idxf, b_pm8], writes=[b_idxf])
            P.op("dve", lambda e: e.tensor_copy(out=idx[:], in_=idxf[:]), reads=[b_idxf], writes=[b_idx])
            P.op("dve", lambda e: e.memset(ones_b[:], 1.0), writes=[b_ones_b])
            P.op("dve", lambda e: e.memset(ones2[:], 1.0), writes=[b_ones2])
            P.op("dve", lambda e: e.memset(pcS[:], 0.0), writes=[b_pcS])
            P.op("dve", lambda e: e.memset(pc32[:], 0.0), writes=[b_pc32])
            P.op("dve", lambda e: e.memset(vcS[:], 0.0), writes=[b_vcS])
            P.op("dve", lambda e: e.memset(vcS[:, :, 128:129], 1.0), writes=[b_vcS])
            P.op("dve", lambda e: e.memset(Vsb[:, :, 128:129], 1.0), writes=[b_Vsb])
            P.op("dve", lambda e: e.memset(Vwb[:, :, 128:129], 1.0), writes=[b_Vwb])
            P.op("dve", lambda e: e.memset(hidS[:], 0.0), reads=[b_ps_g], writes=[b_hidS])
            cache_c = cache_cmp
            cache_s = cache_slc
            tcnt = [0]

            if upto == 'S0':
                P.build(st)
                return nc
            def tr_bank():
                r = [(ps_a, b_ps_a), (ps_b, b_ps_b)][tcnt[0] % 2]
                tcnt[0] += 1
                return r

            def sample_seq(b):
                Qb = QbdAll[:, b, :]
                for pg in range(4):
                    (ctile, b_ct) = ct[ct_cnt[0] % 3]
                    ct_cnt[0] += 1
                    P.dma("pool", lambda e, pg=pg, ctile=ctile: e.indirect_dma_start(
                        out=ctile[:], out_offset=None, in_=cache_c,
                        in_offset=bass.IndirectOffsetOnAxis(ap=idx[:, b * 4 + pg:b * 4 + pg + 1], axis=0)),
                        reads=[b_idx], writes=[b_ct])
                    cv = ctile.rearrange("p (s c x) -> p s c x", s=16, c=2)
                    for c in range(2):
                        for s4 in range(4):
                            (pt_, b_pt_) = tr_bank()
                            for si in range(4):
                                s = s4 * 4 + si
                                P.op("pe", lambda e, s=s, si=si, c=c, cv=cv, pt_=pt_: e.transpose(
                                    out=pt_[:, si * 128:(si + 1) * 128], in_=cv[:, s, c, :], identity=idf[:]),
                                    reads=[b_ct, b_idf], writes=[b_pt_])
                            eng = "act" if (s4 % 2 == 0) else "dve"
                            if eng == "act":
                                P.op("act", lambda e, c=c, s4=s4, pg=pg, pt_=pt_: e.activation(
                                    out=XTs[:, c, s4 * 4:(s4 + 1) * 4, pg * 128:(pg + 1) * 128],
                                    in_=pt_[:].rearrange("p (a b) -> p a b", a=4), func=AF.Identity), reads=[b_pt_], writes=[b_XTs])
                            else:
                                P.op("dve", lambda e, c=c, s4=s4, pg=pg, pt_=pt_: e.tensor_copy(
                                    out=XTs[:, c, s4 * 4:(s4 + 1) * 4, pg * 128:(pg + 1) * 128],
                                    in_=pt_[:].rearrange("p (a b) -> p a b", a=4)), reads=[b_pt_], writes=[b_XTs])
                for c in range(2):
                    (pp, b_pp) = [(ps_c, b_ps_c), (ps_d, b_ps_d)][c]
                    for s in range(32):
                        P.op("pe", lambda e, c=c, s=s, pp=pp: e.matmul(pp[:, 0:511], lhsT=w1b2[:, c, s, :],
                                                                       rhs=XTs[:, c, s % 16, (s // 16):(s // 16) + 511],
                                                                       start=(s == 0), stop=(s == 31)),
                             reads=[b_w1b2, b_XTs], writes=[b_pp])
                    P.op("act", lambda e, c=c, pp=pp: e.activation(out=hidS[:, c, 0:511], in_=pp[:, 0:511], func=AF.Silu,
                                                                   bias=posb2[:, c:c + 1]),
                         reads=[b_pp, b_posb2], writes=[b_hidS])
                P.op("pe", lambda e: e.matmul(ps_e[:, 0:511], lhsT=w2b2[:, 0, :], rhs=hidS[:, 0, 0:511], start=True, stop=True),
                     reads=[b_w2b2, b_hidS], writes=[b_ps_e])
                P.op("dve", lambda e: e.tensor_copy(out=kcS[:, 0:511], in_=ps_e[:, 0:511]), reads=[b_ps_e], writes=[b_kcS])
                for ch in range(4):
                    nn = 128 if ch < 3 else 127
                    P.op("pe", lambda e, ch=ch, nn=nn: e.matmul(ps_f[0:nn, ch * 128:(ch + 1) * 128], lhsT=hidS[:, 1, ch * 128:ch * 128 + nn],
                                                                rhs=w2b2[:, 1, :], start=True, stop=True),
                         reads=[b_w2b2, b_hidS], writes=[b_ps_f])
                P.op("dve", lambda e: e.tensor_copy(out=vcS[:, 0:3, 0:128], in_=ps_f[:, 0:384].rearrange("p (a b) -> p a b", a=3)),
                     reads=[b_ps_f], writes=[b_vcS])
                P.op("dve", lambda e: e.tensor_copy(out=vcS[0:127, 3, 0:128], in_=ps_f[0:127, 384:512]), reads=[b_ps_f], writes=[b_vcS])
                for ch in range(4):
                    nn = 128 if ch < 3 else 127
                    P.op("pe", lambda e, ch=ch, nn=nn: e.matmul(ps_g[0:nn, ch * 8:(ch + 1) * 8], lhsT=kcS[:, ch * 128:ch * 128 + nn], rhs=Qb,
                                                                start=True, stop=True),
                         reads=[b_kcS, b_QbdAll], writes=[b_ps_g])
                P.op("act", lambda e: e.activation(out=pc32[:, 0:3, :], in_=ps_g[:, 0:24].rearrange("p (a b) -> p a b", a=3), func=AF.Exp, scale=SCALE),
                     reads=[b_ps_g], writes=[b_pc32])
                P.op("act", lambda e: e.activation(out=pc32[0:127, 3, :], in_=ps_g[0:127, 24:32], func=AF.Exp, scale=SCALE),
                     reads=[b_ps_g], writes=[b_pc32])
                P.op("dve", lambda e: e.tensor_copy(out=pcS[:], in_=pc32[:]), reads=[b_pc32], writes=[b_pcS])
                for ch in range(4):
                    P.op("pe", lambda e, ch=ch: e.matmul(ps_g[0:8, 200:329], lhsT=pcS[:, ch, :], rhs=vcS[:, ch, 0:129],
                                                         start=(ch == 0), stop=(ch == 3)),
                         reads=[b_pcS, b_vcS], writes=[b_ps_g])
                P.op("dve", lambda e: e.tensor_copy(out=resb[:, 0, :], in_=ps_g[0:8, 200:329]), reads=[b_ps_g], writes=[b_resb])
                for ch in range(4):
                    P.op("pe", lambda e, ch=ch: e.matmul(ps_g[:, 32:40], lhsT=ones_b[:], rhs=pc32[:, ch, :], start=(ch == 0), stop=(ch == 3)),
                         reads=[b_pc32, b_ones_b], writes=[b_ps_g])
                P.op("dve", lambda e: e.reciprocal(out=denB[:], in_=ps_g[:, 32:40]), reads=[b_ps_g], writes=[b_denB])
                P.op("dve", lambda e: e.tensor_tensor(out=pnk32[:], in0=pc32[:], in1=denB[:].unsqueeze(1).to_broadcast([128, 4, 8]), op=ALU.mult),
                     reads=[b_pc32, b_denB], writes=[b_pnk32])
                P.op("dve", lambda e: e.tensor_reduce(out=pnkf[:], in_=pnk32[:].rearrange("p c (k g) -> p c k g", k=2), axis=AX.X, op=ALU.add),
                     reads=[b_pnk32], writes=[b_pnkf])

                for ch in range(4):
                    P.op("pe", lambda e, ch=ch: e.matmul(ps_g[:, 40:42], lhsT=ovS[:, ch, :], rhs=pnkf[:, ch, :], start=(ch == 0), stop=(ch == 3)),
                         reads=[b_ovS, b_pnkf], writes=[b_ps_g])
                P.op("dve", lambda e: e.tensor_copy(out=selB[:], in_=ps_g[:, 40:42]), reads=[b_ps_g], writes=[b_selB])
                for ch in range(4):
                    P.op("pe", lambda e, ch=ch: e.matmul(ps_g[0:2, 64:192], lhsT=pnkf[:, ch, :], rhs=ovS[:, ch, :], start=(ch == 0), stop=(ch == 3)),
                         reads=[b_ovS, b_pnkf], writes=[b_ps_g])
                P.op("dve", lambda e: e.tensor_tensor(out=impAs[:], in0=ps_g[0:2, 64:192], in1=fmaskA[:], op=ALU.add),
                     reads=[b_ps_g, b_fmaskA], writes=[b_impAs])
                P.op("dve", lambda e: e.max(out=m8A[:, 0:8], in_=impAs[:]), reads=[b_impAs], writes=[b_m8A])
                P.op("dve", lambda e: e.match_replace(out=wkA[:], in_to_replace=m8A[:, 0:8], in_values=impAs[:], imm_value=-5.0),
                     reads=[b_impAs, b_m8A], writes=[b_wkA])
                P.op("dve", lambda e: e.max(out=m8A[:, 8:16], in_=wkA[:]), reads=[b_wkA], writes=[b_m8A])
                P.op("dve", lambda e: e.tensor_scalar(out=dthr[:], in0=idf[0:2, 0:2], scalar1=m8A[:, 12:13], scalar2=0.999999,
                                                      op0=ALU.mult, op1=ALU.mult),
                     reads=[b_m8A, b_idf], writes=[b_dthr])
                P.op("pe", lambda e: e.matmul(ps_g[:, 192:194], lhsT=ones2[:], rhs=dthr[:], start=True, stop=True),
                     reads=[b_ones2, b_dthr], writes=[b_ps_g])
                P.op("dve", lambda e: e.tensor_tensor(out=selB[:], in0=selB[:], in1=ps_g[:, 192:194], op=ALU.is_ge),
                     reads=[b_selB, b_ps_g], writes=[b_selB])
                P.op("dve", lambda e: e.tensor_scalar(out=selB[:], in0=selB[:], scalar1=forc[:, 0:1], scalar2=None, op0=ALU.max),
                     reads=[b_selB, b_forc], writes=[b_selB])
                P.op("dve", lambda e: e.tensor_scalar(out=selB[:], in0=selB[:], scalar1=-NEG, scalar2=NEG, op0=ALU.mult, op1=ALU.add),
                     reads=[b_selB], writes=[b_selB])
                P.op("dve", lambda e: e.tensor_copy(out=BselS[:].rearrange("p (k g) -> p k g", k=2),
                                                    in_=selB[:].unsqueeze(2).to_broadcast([128, 2, 4])),
                     reads=[b_selB], writes=[b_BselS])
                for pg in range(4):
                    (ctile, b_ct) = ct[ct_cnt[0] % 3]
                    ct_cnt[0] += 1
                    P.dma("pool", lambda e, pg=pg, ctile=ctile: e.indirect_dma_start(
                        out=ctile[:], out_offset=None, in_=cache_s,
                        in_offset=bass.IndirectOffsetOnAxis(ap=idx[:, b * 4 + pg:b * 4 + pg + 1], axis=0)),
                        reads=[b_idx], writes=[b_ct])
                    cv = ctile.rearrange("p (s c x) -> p s c x", s=16, c=2)
                    P.op("act", lambda e, pg=pg, cv=cv: e.activation(out=Vsb[:, pg * 16:(pg + 1) * 16, 0:128], in_=cv[:, :, 1, :], func=AF.Identity),
                         reads=[b_ct], writes=[b_Vsb])
                    for s4 in range(4):
                        (pt_, b_pt_) = tr_bank()
                        (kst, b_kst) = KsT[s4 % 2]
                        for si in range(4):
                            s = s4 * 4 + si
                            P.op("pe", lambda e, s=s, si=si, cv=cv, pt_=pt_: e.transpose(
                                out=pt_[:, si * 128:(si + 1) * 128], in_=cv[:, s, 0, :], identity=idf[:]),
                                reads=[b_ct, b_idf], writes=[b_pt_])
                        if s4 % 2 == 0:
                            P.op("act", lambda e, pt_=pt_, kst=kst: e.activation(out=kst[:], in_=pt_[:].rearrange("p (a b) -> p a b", a=4), func=AF.Identity),
                                 reads=[b_pt_], writes=[b_kst])
                        else:
                            P.op("dve", lambda e, pt_=pt_, kst=kst: e.tensor_copy(out=kst[:], in_=pt_[:].rearrange("p (a b) -> p a b", a=4)),
                                 reads=[b_pt_], writes=[b_kst])
                        for si in range(4):
                            col = (pg * 16 + s4 * 4 + si) * 8
                            P.op("pe", lambda e, si=si, col=col, kst=kst: e.matmul(ps_e[:, col:col + 8], lhsT=kst[:, si, :], rhs=Qb,
                                                                                   start=True, stop=False),
                                 reads=[b_kst, b_QbdAll], writes=[b_ps_e])
                            P.op("pe", lambda e, col=col, pg=pg: e.matmul(ps_e[:, col:col + 8], lhsT=Eg[:, pg, :], rhs=BselS[:],
                                                                          start=False, stop=True),
                                 reads=[b_Eg, b_BselS], writes=[b_ps_e])
                P.op("act", lambda e: e.activation(out=psS[:], in_=ps_e[:], func=AF.Exp, scale=SCALE), reads=[b_ps_e], writes=[b_psS])
                for j in range(64):
                    P.op("pe", lambda e, j=j: e.matmul(ps_g[0:8, 200:329], lhsT=psS[:, j * 8:(j + 1) * 8], rhs=Vsb[:, j, 0:129],
                                                       start=(j == 0), stop=(j == 63)),
                         reads=[b_psS, b_Vsb], writes=[b_ps_g])
                P.op("dve", lambda e: e.tensor_copy(out=resb[:, 1, :], in_=ps_g[0:8, 200:329]), reads=[b_ps_g], writes=[b_resb])
                P.dma("sp", lambda e: e.dma_start(out=wt[:], in_=state_win[b].rearrange("(t p) x -> p t x", p=128)), writes=[b_wt])
                (pt_, b_pt_) = tr_bank()
                for t in range(4):
                    P.op("pe", lambda e, t=t, pt_=pt_: e.transpose(out=pt_[:, t * 128:(t + 1) * 128], in_=wt[:, t, 0:128], identity=idf[:]),
                         reads=[b_wt, b_idf], writes=[b_pt_])
                P.op("dve", lambda e, pt_=pt_: e.tensor_copy(out=KwS[:], in_=pt_[:]), reads=[b_pt_], writes=[b_KwS])
                P.op("dve", lambda e: e.tensor_copy(out=Vwb[:, :, 0:128], in_=wt[:, :, 128:256]), reads=[b_wt], writes=[b_Vwb])
                for t in range(4):
                    P.op("pe", lambda e, t=t: e.matmul(ps_f[:, t * 8:(t + 1) * 8], lhsT=KwS[:, t * 128:(t + 1) * 128], rhs=Qb, start=True, stop=True),
                         reads=[b_KwS, b_QbdAll], writes=[b_ps_f])
                P.op("act", lambda e: e.activation(out=pwS[:], in_=ps_f[:, 0:32].rearrange("p (a b) -> p a b", a=4), func=AF.Exp, scale=SCALE),
                     reads=[b_ps_f], writes=[b_pwS])
                for t in range(4):
                    P.op("pe", lambda e, t=t: e.matmul(ps_g[0:8, 200:329], lhsT=pwS[:, t, :], rhs=Vwb[:, t, 0:129], start=(t == 0), stop=(t == 3)),
                         reads=[b_pwS, b_Vwb], writes=[b_ps_g])
                P.op("dve", lambda e: e.tensor_copy(out=resb[:, 2, :], in_=ps_g[0:8, 200:329]), reads=[b_ps_g], writes=[b_resb])
                P.dma("sp", lambda e: e.dma_start(out=R_d[b], in_=resb[:]), reads=[b_resb], writes=[b_R_d])

            for b in range(int(os.environ.get('KNSEQ', NS))):
                sample_seq(b)
            if upto == 'S1':
                P.build(st)
                return nc
            P.barrier()
            C.reset(m_persist2)
            R, b_R = C.al("R", [NS, 8, 3, 129], F32)
            P.dma("sp", lambda e: e.dma_start(out=R[:], in_=R_d), reads=[b_R_d], writes=[b_R])
            qk, b_qk = C.al("qk", [NS, 8, 64], F32)
            es, b_es = C.al("es", [NS, 2, 8], F32)
            dn, b_dn = C.al("dn", [NS, 8], F32)
            cf, b_cf = C.al("cf", [NS, 8], F32)
            oS, b_oS = C.al("oS", [NS, 8, 64], F32)
            tS, b_tS = C.al("tS", [NS, 8, 64], F32)
            gaoS, b_gaoS = C.al("gaoS", [NS, 512], F32)
            P.dma("sp", lambda e: e.dma_start(out=gaoS[:], in_=g_attn_out.partition_broadcast(NS)), writes=[b_gaoS])
            qv = qtmS[:].rearrange("p (g k d) -> p k g d", g=4, k=2)
            kvv = kvS[:].rearrange("p (r c k d) -> p r c k d", r=3, c=2, k=2)
            gS3 = gatesS[:].rearrange("p (h r) -> p h r", r=3)
            for bi, br in enumerate((1, 2)):
                for k in range(2):
                    P.op("dve", lambda e, br=br, k=k: e.tensor_tensor(out=qk[:, k * 4:(k + 1) * 4, :], in0=qv[:, k, :, :],
                                                                      in1=kvv[:, br, 0, k, :].unsqueeze(1).to_broadcast([NS, 4, 64]), op=ALU.mult),
                         reads=[b_qtmS, b_kvS], writes=[b_qk])
                P.op("dve", lambda e, bi=bi: e.tensor_reduce(out=es[:, bi, :], in_=qk[:], axis=AX.X, op=ALU.add), reads=[b_qk], writes=[b_es])
            P.op("act", lambda e: e.activation(out=es[:], in_=es[:], func=AF.Exp, scale=SCALE), reads=[b_es], writes=[b_es])
            for br in range(3):
                for k in range(2):
                    P.op("dve", lambda e, br=br, k=k: e.tensor_copy(out=tS[:, k * 4:(k + 1) * 4, :], in_=R[:, k * 4:(k + 1) * 4, br, k * 64:(k + 1) * 64]),
                         reads=[b_R], writes=[b_tS])
                P.op("dve", lambda e, br=br: e.tensor_copy(out=dn[:], in_=R[:, :, br, 128]), reads=[b_R], writes=[b_dn])
                if br > 0:
                    for k in range(2):
                        P.op("dve", lambda e, br=br, k=k: e.tensor_tensor(
                            out=qk[:, k * 4:(k + 1) * 4, :], in0=es[:, br - 1, k * 4:(k + 1) * 4].unsqueeze(2).to_broadcast([NS, 4, 64]),
                            in1=kvv[:, br, 1, k, :].unsqueeze(1).to_broadcast([NS, 4, 64]), op=ALU.mult),
                            reads=[b_es, b_kvS], writes=[b_qk])
                    P.op("dve", lambda e: e.tensor_tensor(out=tS[:], in0=tS[:], in1=qk[:], op=ALU.add), reads=[b_tS, b_qk], writes=[b_tS])
                    P.op("dve", lambda e, br=br: e.tensor_tensor(out=dn[:], in0=dn[:], in1=es[:, br - 1, :], op=ALU.add),
                         reads=[b_dn, b_es], writes=[b_dn])
                P.op("dve", lambda e: e.reciprocal(out=dn[:], in_=dn[:]), reads=[b_dn], writes=[b_dn])
                P.op("dve", lambda e, br=br: e.tensor_tensor(out=cf[:], in0=dn[:], in1=gS3[:, :, br], op=ALU.mult),
                     reads=[b_dn, b_gatesS], writes=[b_cf])
                P.op("dve", lambda e: e.tensor_tensor(out=tS[:], in0=tS[:], in1=cf[:].unsqueeze(2).to_broadcast([NS, 8, 64]), op=ALU.mult),
                     reads=[b_tS, b_cf], writes=[b_tS])
                if br == 0:
                    P.op("dve", lambda e: e.tensor_copy(out=oS[:], in_=tS[:]), reads=[b_tS], writes=[b_oS])
                else:
                    P.op("dve", lambda e: e.tensor_tensor(out=oS[:], in0=oS[:], in1=tS[:], op=ALU.add), reads=[b_tS, b_oS], writes=[b_oS])
            oSf = oS[:].rearrange("p h d -> p (h d)")
            tSf = tS[:].rearrange("p h d -> p (h d)")
            obS, b_obS = C.al("obS", [NS, 512], BF16)
            s1, b_s1 = C.al("s1", [NS, 4], F32)

            def norm_to_cat(src, b_src, gam, b_gam, chunk0):
                P.op("act", lambda e: e.activation(out=tSf, in_=src, func=AF.Square, accum_out=s1[:, 0:1]), reads=[b_src], writes=[b_tS, b_s1])
                P.op("act", lambda e: e.activation(out=s1[:, 0:1], in_=s1[:, 0:1], func=AF.Sqrt, scale=1.0 / 512, bias=epsT[0:NS, :]),
                     reads=[b_s1, b_eps], writes=[b_s1])
                P.op("dve", lambda e: e.reciprocal(out=s1[:, 0:1], in_=s1[:, 0:1]), reads=[b_s1], writes=[b_s1])
                P.op("dve", lambda e: e.scalar_tensor_tensor(out=obS[:], in0=src, scalar=s1[:, 0:1], in1=gam, op0=ALU.mult, op1=ALU.mult),
                     reads=[b_src, b_s1, b_gam], writes=[b_obS])
                for j in range(4):
                    P.op("pe", lambda e, j=j: e.transpose(out=ps_tr[:, j, 0:NS], in_=obS[:, j * 128:(j + 1) * 128], identity=idb[0:NS, 0:NS]),
                         reads=[b_obS, b_idb], writes=[b_ps_tr])
                P.op("dve", lambda e: e.tensor_copy(out=catS[:, chunk0:chunk0 + 4, :], in_=ps_tr[:, 0:4, 0:NS]), reads=[b_ps_tr], writes=[b_catS])

            if DBG:
                P.dma("pool", lambda e: e.dma_start(out=dbg_os, in_=oSf), reads=[b_oS], final=True)
            norm_to_cat(oSf, b_oS, gaoS[:], b_gaoS, 0)
            if upto == 'S2':
                P.build(st)
                return nc
            scv, b_scv = C.al("scv", [NS, 15, 512], F32)
            wdv, b_wdv = C.al("wdv", [NS, 15, 512], F32)
            ycv, b_ycv = C.al("ycv", [NS, 512], F32)
            ytmp, b_ytmp = C.al("ytmp", [NS, 512], F32)
            prm, b_prm = C.al("prm", [NS, 4, 512], F32)
            for pi, src in enumerate((b_dw_row, conv_ln_g, conv_ln_b, g_conv_out)):
                P.dma("sp", lambda e, pi=pi, src=src: e.dma_start(out=prm[:, pi, :], in_=src.partition_broadcast(NS)), writes=[b_prm])
            for hh in range(2):
                P.dma("sp", lambda e, hh=hh: e.dma_start(out=scv[:], in_=state_conv[:, hh * 15:(hh + 1) * 15, :]), writes=[b_scv])
                P.dma("sp", lambda e, hh=hh: e.dma_start(out=wdv[:].rearrange("p k c -> p (k c)"),
                                                         in_=w_dw_rows[:, hh * 15 * 512:(hh + 1) * 15 * 512].partition_broadcast(NS)), writes=[b_wdv])
                P.op("dve", lambda e: e.tensor_tensor(out=scv[:], in0=scv[:], in1=wdv[:], op=ALU.mult), reads=[b_scv, b_wdv], writes=[b_scv])
                if hh == 0:
                    P.op("dve", lambda e: e.tensor_reduce(out=ycv[:], in_=scv[:].rearrange("p k c -> p c k"), axis=AX.X, op=ALU.add),
                         reads=[b_scv], writes=[b_ycv])
                else:
                    P.op("dve", lambda e: e.tensor_reduce(out=ytmp[:], in_=scv[:].rearrange("p k c -> p c k"), axis=AX.X, op=ALU.add),
                         reads=[b_scv], writes=[b_ytmp])
                    P.op("dve", lambda e: e.tensor_tensor(out=ycv[:], in0=ycv[:], in1=ytmp[:], op=ALU.add), reads=[b_ycv, b_ytmp], writes=[b_ycv])
            P.dma("sp", lambda e: e.dma_start(out=wdv[:, 0, :], in_=w_dw_rows[:, 30 * 512:31 * 512].partition_broadcast(NS)), writes=[b_wdv])
            P.op("dve", lambda e: e.tensor_tensor(out=ytmp[:], in0=usS[:], in1=wdv[:, 0, :], op=ALU.mult), reads=[b_usS, b_wdv], writes=[b_ytmp])
            P.op("dve", lambda e: e.tensor_tensor(out=ycv[:], in0=ycv[:], in1=ytmp[:], op=ALU.add), reads=[b_ycv, b_ytmp], writes=[b_ycv])
            P.op("dve", lambda e: e.tensor_tensor(out=ycv[:], in0=ycv[:], in1=prm[:, 0, :], op=ALU.add), reads=[b_ycv, b_prm], writes=[b_ycv])
            st6s, b_st6s = C.al("st6s", [NS, 6], F32)
            mvs, b_mvs = C.al("mvs", [NS, 2], F32)
            P.op("dve", lambda e: e.bn_stats(out=st6s[:], in_=ycv[:]), reads=[b_ycv], writes=[b_st6s])
            P.op("dve", lambda e: e.bn_aggr(out=mvs[:], in_=st6s[:]), reads=[b_st6s], writes=[b_mvs])
            P.op("act", lambda e: e.activation(out=s1[:, 1:2], in_=mvs[:, 1:2], func=AF.Sqrt, bias=epsT[0:NS, :]), reads=[b_mvs, b_eps], writes=[b_s1])
            P.op("dve", lambda e: e.reciprocal(out=s1[:, 1:2], in_=s1[:, 1:2]), reads=[b_s1], writes=[b_s1])
            P.op("dve", lambda e: e.tensor_scalar(out=ycv[:], in0=ycv[:], scalar1=mvs[:, 0:1], scalar2=s1[:, 1:2], op0=ALU.subtract, op1=ALU.mult),
                 reads=[b_ycv, b_mvs, b_s1], writes=[b_ycv])
            P.op("dve", lambda e: e.tensor_tensor(out=ycv[:], in0=ycv[:], in1=prm[:, 1, :], op=ALU.mult), reads=[b_ycv, b_prm], writes=[b_ycv])
            P.op("dve", lambda e: e.tensor_tensor(out=ycv[:], in0=ycv[:], in1=prm[:, 2, :], op=ALU.add), reads=[b_ycv, b_prm], writes=[b_ycv])
            P.op("act", lambda e: e.activation(out=ycv[:], in_=ycv[:], func=AF.Silu), reads=[b_ycv], writes=[b_ycv])
            if DBG:
                P.dma("pool", lambda e: e.dma_start(out=dbg_ycv, in_=ycv[:]), reads=[b_ycv], final=True)
            norm_to_cat(ycv[:], b_ycv, prm[:, 3, :], b_prm, 4)
            P.barrier()
            C.reset(m_persist2)
        if stage < 3:
            P.build(st)
            return nc
        NT = NOWN + NS
        C.reset(m_samp)
        h2T, b_h2T = C.al("h2T", [128, 8, NT], BF16)
        cwT, b_cwT = C.al("cwT", [32, NT], F32)
        m_persist3 = C.mark()
        assert C.off <= m_persist, (C.off, m_persist)
        C.reset(m_persist2)
        wob, b_wob = C.al("wob", [128, 8, D], BF16)
        wstF, b_wstF = C.al("wstF", [128, 8, 256], F32)
        g1Pm, b_g1Pm = C.al("g1Pm", [128, D], F32)
        sh2P, b_sh2P = C.al("sh2P", [128, D], F32)
        A2P, b_A2P = C.al("A2P", [128, D], F32)
        g1Sm, b_g1Sm = C.al("g1Sm", [NS, D], F32)
        sh2S, b_sh2S = C.al("sh2S", [NS, D], F32)
        A2S, b_A2S = C.al("A2S", [NS, D], F32)
        g2P, b_g2P = C.al("g2P", [128, D], F32)
        wrs, b_wrs = C.al("wrs", [128, 8, 36], F32)
        wrb, b_wrb = C.al("wrb", [128, 8, 36], BF16)
        brP, b_brP = C.al("brP", [128, 36], F32)
        xF = [C.al("xF%d" % i, [128, D], F32) for i in range(2)]
        x2t, b_x2t = C.al("x2t", [128, D], F32)
        hj, b_hj = C.al("hj", [128, D], F32)
        h2b, b_h2b = C.al("h2b", [128, D], BF16)
        lg, b_lg = C.al("lg", [128, 36], F32)
        zz, b_zz = C.al("zz", [128, 32], F32)
        ej, b_ej = C.al("ej", [128, 4], F32)
        oh, b_oh = C.al("oh", [128, 4], F32)
        sm, b_sm = C.al("sm", [128, 8], F32)
        m8f, b_m8f = C.al("m8f", [128, 8], F32)
        cw, b_cw = C.al("cw", [128, 32], F32)
        cw2, b_cw2 = C.al("cw2", [128, 32], F32)
        load_mod(2, g1Pm, b_g1Pm, g1Sm, b_g1Sm)
        load_mod(3, sh2P, b_sh2P, sh2S, b_sh2S)
        load_mod(4, A2P, b_A2P, A2S, b_A2S)
        P.dma("sp", lambda e: e.dma_start(out=g2P[:], in_=norm2_g.partition_broadcast(128)), writes=[b_g2P])
        P.op("dve", lambda e: e.scalar_tensor_tensor(out=A2P[:], in0=A2P[:], scalar=1.0, in1=g2P[:], op0=ALU.add, op1=ALU.mult),
             reads=[b_A2P, b_g2P], writes=[b_A2P])
        P.op("dve", lambda e: e.scalar_tensor_tensor(out=A2S[:], in0=A2S[:], scalar=1.0, in1=g2P[0:NS, :], op0=ALU.add, op1=ALU.mult),
             reads=[b_A2S, b_g2P], writes=[b_A2S])
        P.dma("sp", lambda e: e.dma_start(out=wrs[:], in_=w_gr.rearrange("(c p) n -> p c n", p=128)), writes=[b_wrs])
        P.op("dve", lambda e: e.tensor_copy(out=wrb[:], in_=wrs[:]), reads=[b_wrs], writes=[b_wrb])
        P.dma("sp", lambda e: e.dma_start(out=brP[:], in_=b_gr.partition_broadcast(128)), writes=[b_brP])
        w_out_v = w_out.rearrange("(c p) n -> p c n", p=128)
        for c0 in range(0, D, 256):
            P.dma("sp", lambda e, c0=c0: e.dma_start(out=wstF[:], in_=w_out_v[:, :, c0:c0 + 256]), writes=[b_wstF])
            P.op("pool", lambda e, c0=c0: e.tensor_copy(out=wob[:, :, c0:c0 + 256], in_=wstF[:]), reads=[b_wstF], writes=[b_wob])

        def finish_tile(i, n, cat_ap, b_cat, x_src, tok0, kind):
            (x_t, b_x) = xF[i % 2]
            g1m, b_g1m = (g1Sm, b_g1Sm) if kind == "sample" else (g1Pm, b_g1Pm)
            sh2, b_sh2 = (sh2S, b_sh2S) if kind == "sample" else (sh2P, b_sh2P)
            A2, b_A2 = (A2S, b_A2S) if kind == "sample" else (A2P, b_A2P)
            P.dma("sp", lambda e: e.dma_start(out=x_t[0:n, :], in_=x_src), writes=[b_x])
            for half in range(2):
                psx, b_psx = [(ps_a, b_ps_a), (ps_b, b_ps_b)][half]
                hs = slice(half * 512, (half + 1) * 512)
                for c in range(8):
                    P.op("pe", lambda e, c=c, psx=psx, hs=hs: e.matmul(psx[0:n, :], lhsT=cat_ap[:, c, :], rhs=wob[:, c, hs],
                                                                       start=(c == 0), stop=(c == 7)),
                         reads=[b_cat, b_wob], writes=[b_psx])
                P.op("dve", lambda e, psx=psx, hs=hs: e.tensor_tensor(out=x2t[0:n, hs], in0=psx[0:n, :], in1=g1m[0:n, hs], op=ALU.mult),
                     reads=[b_psx, b_g1m], writes=[b_x2t])
                P.op("pool", lambda e, hs=hs: e.tensor_tensor(out=x2t[0:n, hs], in0=x2t[0:n, hs], in1=x_t[0:n, hs], op=ALU.add),
                     reads=[b_x2t, b_x], writes=[b_x2t])
            P.dma("pool", lambda e: e.dma_start(out=x2_d[tok0:tok0 + n, :], in_=x2t[0:n, :]), reads=[b_x2t], writes=[b_x2_d])
            P.op("act", lambda e: e.activation(out=hj[0:n, :], in_=x2t[0:n, :], func=AF.Square, accum_out=sm[0:n, 0:1]),
                 reads=[b_x2t], writes=[b_hj, b_sm])
            P.op("act", lambda e: e.activation(out=sm[0:n, 0:1], in_=sm[0:n, 0:1], func=AF.Sqrt, scale=1.0 / D, bias=epsT[0:n, :]),
                 reads=[b_sm, b_eps], writes=[b_sm])
            P.op("dve", lambda e: e.reciprocal(out=sm[0:n, 0:1], in_=sm[0:n, 0:1]), reads=[b_sm], writes=[b_sm])
            P.op("dve", lambda e: e.scalar_tensor_tensor(out=hj[0:n, :], in0=x2t[0:n, :], scalar=sm[0:n, 0:1], in1=A2[0:n, :],
                                                         op0=ALU.mult, op1=ALU.mult),
                 reads=[b_x2t, b_sm, b_A2], writes=[b_hj])
            P.op("pool", lambda e: e.tensor_tensor(out=h2b[0:n, :], in0=hj[0:n, :], in1=sh2[0:n, :], op=ALU.add),
                 reads=[b_hj, b_sh2], writes=[b_h2b])
            for c in range(8):
                P.op("pe", lambda e, c=c: e.transpose(out=ps_tr[:, c, 0:n], in_=h2b[0:n, c * 128:(c + 1) * 128], identity=idb[0:n, 0:n]),
                     reads=[b_h2b, b_idb], writes=[b_ps_tr])
            P.op("act", lambda e: e.copy(out=h2T[:, :, tok0:tok0 + n], in_=ps_tr[:, :, 0:n]), reads=[b_ps_tr], writes=[b_h2T])
            for c in range(8):
                P.op("pe", lambda e, c=c: e.matmul(ps_c[0:n, 0:36], lhsT=h2T[:, c, tok0:tok0 + n], rhs=wrb[:, c, :],
                                                   start=(c == 0), stop=(c == 7)),
                     reads=[b_h2T, b_wrb], writes=[b_ps_c])
            P.op("dve", lambda e: e.tensor_tensor(out=lg[0:n, :], in0=ps_c[0:n, 0:36], in1=brP[0:n, :], op=ALU.add),
                 reads=[b_ps_c, b_brP], writes=[b_lg])
            P.op("dve", lambda e: e.tensor_reduce(out=sm[0:n, 1:2], in_=lg[0:n, 0:4], axis=AX.X, op=ALU.max),
                 reads=[b_lg], writes=[b_sm])
            P.op("dve", lambda e: e.tensor_scalar(out=sm[0:n, 2:3], in0=sm[0:n, 1:2], scalar1=-1.0, scalar2=None, op0=ALU.mult),
                 reads=[b_sm], writes=[b_sm])
            P.op("act", lambda e: e.activation(out=ej[0:n, :], in_=lg[0:n, 0:4], func=AF.Exp, bias=sm[0:n, 2:3],
                                               accum_out=sm[0:n, 3:4]),
                 reads=[b_lg, b_sm], writes=[b_ej, b_sm])
            P.op("dve", lambda e: e.reciprocal(out=sm[0:n, 3:4], in_=sm[0:n, 3:4]), reads=[b_sm], writes=[b_sm])
            P.op("dve", lambda e: e.tensor_scalar(out=oh[0:n, :], in0=lg[0:n, 0:4], scalar1=sm[0:n, 1:2], scalar2=None, op0=ALU.is_ge),
                 reads=[b_lg, b_sm], writes=[b_oh])
            P.op("dve", lambda e: e.tensor_scalar(out=oh[0:n, :], in0=oh[0:n, :], scalar1=1e4, scalar2=-1e4, op0=ALU.mult, op1=ALU.add),
                 reads=[b_oh], writes=[b_oh])
            P.op("dve", lambda e: e.tensor_tensor(out=zz[0:n, :].rearrange("p (g j) -> p g j", g=4),
                                                  in0=lg[0:n, 4:36].rearrange("p (g j) -> p g j", g=4),
                                                  in1=oh[0:n, :].unsqueeze(2).to_broadcast([n, 4, 8]), op=ALU.add),
                 reads=[b_lg, b_oh], writes=[b_zz])
            P.op("dve", lambda e: e.max(out=m8f[0:n, :], in_=zz[0:n, :]), reads=[b_zz], writes=[b_m8f])
            P.op("dve", lambda e: e.tensor_tensor(out=sm[0:n, 4:5], in0=m8f[0:n, 0:1], in1=m8f[0:n, 1:2], op=ALU.subtract),
                 reads=[b_m8f], writes=[b_sm])
            P.op("act", lambda e: e.activation(out=sm[0:n, 4:5], in_=sm[0:n, 4:5], func=AF.Exp), reads=[b_sm], writes=[b_sm])
            P.op("dve", lambda e: e.tensor_scalar(out=sm[0:n, 4:5], in0=sm[0:n, 4:5], scalar1=1.0, scalar2=None, op0=ALU.add),
                 reads=[b_sm], writes=[b_sm])
            P.op("dve", lambda e: e.reciprocal(out=sm[0:n, 4:5], in_=sm[0:n, 4:5]), reads=[b_sm], writes=[b_sm])
            P.op("dve", lambda e: e.tensor_tensor(out=sm[0:n, 5:6], in0=sm[0:n, 4:5], in1=sm[0:n, 3:4], op=ALU.mult),
                 reads=[b_sm], writes=[b_sm])
            P.op("dve", lambda e: e.tensor_tensor(out=sm[0:n, 6:7], in0=sm[0:n, 3:4], in1=sm[0:n, 5:6], op=ALU.subtract),
                 reads=[b_sm], writes=[b_sm])
            P.op("dve", lambda e: e.tensor_scalar(out=cw[0:n, :], in0=zz[0:n, :], scalar1=m8f[0:n, 0:1], scalar2=sm[0:n, 6:7],
                                                  op0=ALU.is_equal, op1=ALU.mult),
                 reads=[b_zz, b_m8f, b_sm], writes=[b_cw])
            P.op("dve", lambda e: e.tensor_scalar(out=cw2[0:n, :], in0=zz[0:n, :], scalar1=m8f[0:n, 1:2], scalar2=sm[0:n, 5:6],
                                                  op0=ALU.is_equal, op1=ALU.mult),
                 reads=[b_zz, b_m8f, b_sm], writes=[b_cw2])
            P.op("dve", lambda e: e.tensor_tensor(out=cw[0:n, :], in0=cw[0:n, :], in1=cw2[0:n, :], op=ALU.add),
                 reads=[b_cw, b_cw2], writes=[b_cw])
            P.op("pe", lambda e: e.transpose(out=ps_d[0:32, 0:n], in_=cw[0:n, :], identity=idf[0:n, 0:n]),
                 reads=[b_cw, b_idf], writes=[b_ps_d])
            P.op("dve", lambda e: e.tensor_copy(out=cwT[:, tok0:tok0 + n], in_=ps_d[0:32, 0:n]), reads=[b_ps_d], writes=[b_cwT])

        for i in range(16):
            finish_tile(i, 128, catT[:, :, i * 128:(i + 1) * 128], b_catT, xc[i * 128:(i + 1) * 128, :], i * 128, "own")
        if HAVE_SAMPLE:
            finish_tile(16, NS, catS[:, :, :], b_catS, xs, NOWN, "sample")
        P.barrier()
        C.reset(m_persist3)

        NTOK = NT if HAVE_SAMPLE else NOWN
        yacc, b_yT2 = C.al("yTacc", [128, 8, NT], F32)
        m_G = C.mark()
        selT, b_selT = C.al("selT", [32, 32, 128], F32)
        wgs, b_wgs = C.al("wgs", [128, 8, 256], F32)
        wus, b_wus = C.al("wus", [128, 8, 256], F32)
        wds, b_wds = C.al("wds", [128, 2, D], F32)
        wgb = [C.al("wgb%d" % i, [128, 8, 256], BF16) for i in range(2)]
        wub = [C.al("wub%d" % i, [128, 8, 256], BF16) for i in range(2)]
        wdb = [C.al("wdb%d" % i, [128, 2, D], BF16) for i in range(2)]
        sgt = [C.al("sgt%d" % i, [128, 512], F32) for i in range(2)]
        hidb = [C.al("hidb%d" % i, [128, 2, 512], BF16) for i in range(2)]
        P.op("dve", lambda e: e.tensor_copy(out=selT[:], in_=idf[0:32, 0:32].unsqueeze(2).to_broadcast([32, 32, 128])),
             reads=[b_idf], writes=[b_selT])
        groups = [(g * 512, 512) for g in range(4)] + ([(NOWN, NS)] if HAVE_SAMPLE else [])
        ps_gu = [(ps_c, b_ps_c), (ps_d, b_ps_d), (ps_e, b_ps_e), (ps_f, b_ps_f)]
        ps_dn = [(ps_a, b_ps_a), (ps_b, b_ps_b)]
        dn_cnt = [0]

        def expert(ei):
            s = ei % 2
            (wg, b_wg), (wu, b_wu), (wd, b_wd) = wgb[s], wub[s], wdb[s]
            P.dma("sp", lambda e: e.dma_start(out=wgs[:], in_=w_gate[ei].rearrange("(c p) f -> p c f", p=128)), writes=[b_wgs])
            P.dma("sp", lambda e: e.dma_start(out=wus[:], in_=w_up[ei].rearrange("(c p) f -> p c f", p=128)), writes=[b_wus])
            P.dma("sp", lambda e: e.dma_start(out=wds[:], in_=w_down[ei].rearrange("(h p) n -> p h n", p=128)), writes=[b_wds])
            P.op("pool", lambda e: e.tensor_copy(out=wg[:], in_=wgs[:]), reads=[b_wgs], writes=[b_wg])
            P.op("pool", lambda e: e.tensor_copy(out=wu[:], in_=wus[:]), reads=[b_wus], writes=[b_wu])
            P.op("pool", lambda e: e.tensor_copy(out=wd[:], in_=wds[:]), reads=[b_wds], writes=[b_wd])
            def front(fi, t0, T):
                (hb_, b_hb_) = hidb[fi % 2]
                P.op("pe", lambda e: e.matmul(ps_g[:, 0:T], lhsT=selT[:, ei, :], rhs=cwT[:, t0:t0 + T], start=True, stop=True),
                     reads=[b_selT, b_cwT], writes=[b_ps_g])
                for which, (wsrc, b_wsrc) in enumerate(((wg, b_wg), (wu, b_wu))):
                    for half in range(2):
                        (psx, b_psx) = ps_gu[which * 2 + half]
                        for c in range(8):
                            P.op("pe", lambda e, c=c, psx=psx, wsrc=wsrc, half=half: e.matmul(
                                psx[:, 0:T], lhsT=wsrc[:, c, half * 128:(half + 1) * 128], rhs=h2T[:, c, t0:t0 + T],
                                start=(c == 0), stop=(c == 7)),
                                reads=[b_wsrc, b_h2T], writes=[b_psx])
                for half in range(2):
                    (pg_, b_pg_) = ps_gu[half]
                    (pu_, b_pu_) = ps_gu[2 + half]
                    (sg_, b_sg_) = sgt[half]
                    P.op("act", lambda e, pg_=pg_, sg_=sg_: e.activation(out=sg_[:, 0:T], in_=pg_[:, 0:T], func=AF.Silu),
                         reads=[b_pg_], writes=[b_sg_])
                    P.op("dve", lambda e, pu_=pu_, sg_=sg_: e.tensor_tensor(out=sg_[:, 0:T], in0=sg_[:, 0:T], in1=pu_[:, 0:T], op=ALU.mult),
                         reads=[b_pu_, b_sg_], writes=[b_sg_])
                    P.op("dve", lambda e, sg_=sg_, hb_=hb_, half=half: e.tensor_tensor(out=hb_[:, half, 0:T], in0=sg_[:, 0:T],
                                                                                       in1=ps_g[:, 0:T], op=ALU.mult),
                         reads=[b_sg_, b_ps_g], writes=[b_hb_])
            def down(fi, t0, T):
                (hb_, b_hb_) = hidb[fi % 2]
                for dc in range(8):
                    (pd_, b_pd_) = ps_dn[dn_cnt[0] % 2]
                    dn_cnt[0] += 1
                    for half in range(2):
                        P.op("pe", lambda e, dc=dc, half=half, pd_=pd_, hb_=hb_: e.matmul(
                            pd_[:, 0:T], lhsT=wd[:, half, dc * 128:(dc + 1) * 128], rhs=hb_[:, half, 0:T],
                            start=(half == 0), stop=(half == 1)),
                            reads=[b_wd, b_hb_], writes=[b_pd_])
                    if ei == 0:
                        P.op("dve", lambda e, dc=dc, pd_=pd_: e.tensor_copy(out=yacc[:, dc, t0:t0 + T], in_=pd_[:, 0:T]),
                             reads=[b_pd_], writes=[b_yT2])
                    else:
                        P.op("dve", lambda e, dc=dc, pd_=pd_: e.tensor_tensor(out=yacc[:, dc, t0:t0 + T], in0=yacc[:, dc, t0:t0 + T],
                                                                              in1=pd_[:, 0:T], op=ALU.add),
                             reads=[b_pd_, b_yT2], writes=[b_yT2])

            evs = []
            for gi, (t0, T) in enumerate(groups):
                fi = flat_cnt[0]
                flat_cnt[0] += 1
                evs.append((lambda fi=fi, t0=t0, T=T: front(fi, t0, T), lambda fi=fi, t0=t0, T=T: down(fi, t0, T)))
            return evs

        flat_cnt = [0]
        pending = None
        for ei in range(32):
            for (f_front, f_down) in expert(ei):
                f_front()
                if pending is not None:
                    pending()
                pending = f_down
        pending()
        P.barrier()
        C.reset(m_G)

        g2Pm, b_g2Pm = C.al("g2Pm", [128, D], F32)
        g2Sm, b_g2Sm = C.al("g2Sm", [NS, D], F32)
        fgP, b_fgP = C.al("fgP", [128, D], F32)
        x2r = [C.al("x2r%d" % i, [128, D], F32) for i in range(2)]
        xo, b_xo = C.al("xo", [128, D], F32)
        xj, b_xj = C.al("xj", [128, D], F32)
        fs, b_fs = C.al("fs", [128, 1], F32)
        load_mod(5, g2Pm, b_g2Pm, g2Sm, b_g2Sm)
        P.dma("sp", lambda e: e.dma_start(out=fgP[:], in_=final_g.partition_broadcast(128)), writes=[b_fgP])

        def final_tile(i, n, tok0, dst, kind):
            (x_t, b_x) = x2r[i % 2]
            g2m, b_g2m = (g2Sm, b_g2Sm) if kind == "sample" else (g2Pm, b_g2Pm)
            P.dma("sp", lambda e: e.dma_start(out=x_t[0:n, :], in_=x2_d[tok0:tok0 + n, :]), reads=[b_x2_d], writes=[b_x])
            for half in range(2):
                psx, b_psx = [(ps_c, b_ps_c), (ps_d, b_ps_d)][half]
                for j in range(4):
                    dc = half * 4 + j
                    P.op("pe", lambda e, dc=dc, j=j, psx=psx: e.transpose(out=psx[0:n, j * 128:(j + 1) * 128],
                                                                         in_=yacc[:, dc, tok0:tok0 + n], identity=idf[:]),
                         reads=[b_yT2, b_idf], writes=[b_psx])
                hs = slice(half * 512, (half + 1) * 512)
                P.op("dve", lambda e, psx=psx, hs=hs: e.tensor_tensor(out=xo[0:n, hs], in0=psx[0:n, :], in1=g2m[0:n, hs], op=ALU.mult),
                     reads=[b_psx, b_g2m], writes=[b_xo])
                P.op("pool", lambda e, hs=hs: e.tensor_tensor(out=xo[0:n, hs], in0=xo[0:n, hs], in1=x_t[0:n, hs], op=ALU.add),
                     reads=[b_xo, b_x], writes=[b_xo])
            P.op("act", lambda e: e.activation(out=xj[0:n, :], in_=xo[0:n, :], func=AF.Square, accum_out=fs[0:n, :]),
                 reads=[b_xo], writes=[b_xj, b_fs])
            P.op("act", lambda e: e.activation(out=fs[0:n, :], in_=fs[0:n, :], func=AF.Sqrt, scale=1.0 / D, bias=epsT[0:n, :]),
                 reads=[b_fs, b_eps], writes=[b_fs])
            P.op("dve", lambda e: e.reciprocal(out=fs[0:n, :], in_=fs[0:n, :]), reads=[b_fs], writes=[b_fs])
            P.op("dve", lambda e: e.scalar_tensor_tensor(out=xj[0:n, :], in0=xo[0:n, :], scalar=fs[0:n, 0:1], in1=fgP[0:n, :],
                                                         op0=ALU.mult, op1=ALU.mult),
                 reads=[b_xo, b_fs, b_fgP], writes=[b_xj])
            P.dma("pool", lambda e: e.dma_start(out=dst, in_=xj[0:n, :]), reads=[b_xj], final=True)

        for i in range(16):
            final_tile(i, 128, i * 128, y_p[i * 128:(i + 1) * 128, :], "own")
        if HAVE_SAMPLE:
            final_tile(16, NS, NOWN, y_s, "sample")

        P.build(st)
    return nc


_NC_CACHE = {}


def _nat(tp, hf):
    tp = np.asarray(tp)
    return np.where(tp < NOWN, tp + NOWN * hf, tp - NOWN + NOWN * (1 - hf))


def _tables(hf):
    f32 = np.float32
    t = {}
    tp = np.arange(SEQ)
    t["etab"] = (tp[None, :] // 64 == (np.arange(128)[:, None] % 64)).astype(f32)
    kk = np.arange(128)
    t["tridiag"] = np.where(kk[:, None] <= kk[None, :], 0.0, NEG).astype(f32)
    t["trilo"] = np.where(kk[:, None] >= kk[None, :], 0.0, NEG).astype(f32)
    blknat = _nat(64 * np.arange(64), hf) // 64
    t["blknat"] = blknat.astype(f32).reshape(1, 64)
    t["isz"] = (blknat == 0).astype(f32).reshape(1, 64)
    ncn = _nat(16 * np.arange(256), hf) // 16
    valid = ncn[(np.arange(256) + 1) % 256] == ncn + 1
    pe = np.where(valid, 16.0 * ncn + 31.0, 1e9)
    t["pe_slot"] = np.ascontiguousarray(pe.reshape(2, 128).T).astype(f32)
    c0 = 16 * ncn
    ov = valid[:, None] & (c0[:, None] <= 64 * blknat[None, :] + 63) & (c0[:, None] + 31 >= 64 * blknat[None, :])
    t["ovt"] = np.ascontiguousarray(ov.astype(f32).reshape(2, 128, 64).transpose(1, 0, 2))
    qpos = NOWN * hf + np.arange(NOWN)
    t["qrow"] = qpos.astype(f32).reshape(1, NOWN)
    t["curt"] = np.ascontiguousarray((qpos // 64).reshape(16, 128).T).astype(f32)
    t["nvrow"] = np.full((1, 512), NEG * (1 - hf), f32)
    t["hfv"] = np.full((128, 1), float(hf), f32)
    t["identf"] = np.eye(128, dtype=f32)
    return t


def _prep_inputs(inp):
    f32 = np.float32
    g = lambda k: np.asarray(inp[k], f32)
    x_prompt = g("x_prompt")
    x_sample = g("x_sample")
    w1 = g("w_cmp1")[0]
    w1bd = np.zeros((2, 64, 2, 32, 2, 64), f32)
    for k in range(2):
        w1bd[k, :, :, :, k, :] = w1.transpose(2, 0, 1, 3)
    w1bd = w1bd.reshape(128, 2, 32, 128)
    pos = g("pos_cmp")[0]
    posT2 = np.ascontiguousarray(np.tile(pos.transpose(2, 0, 1), (2, 1, 1)))
    w2 = g("w_cmp2")[0]
    w2bd = np.zeros((2, 64, 2, 2, 64), f32)
    for k in range(2):
        w2bd[k, :, :, k, :] = w2.transpose(1, 0, 2)
    w2bd = w2bd.reshape(128, 2, 128)
    wdw = g("w_dw")[0]
    wdwT = np.ascontiguousarray(wdw.reshape(31, 4, 128).transpose(2, 1, 0))
    bdwT = np.ascontiguousarray(g("b_dw")[0].reshape(4, 128).T)
    shared = {
        "norm1_g": g("norm1_g").reshape(1, D),
        "w_ada": g("w_ada")[0],
        "b_ada": g("b_ada").reshape(1, 6 * D),
        "w_in": g("w_in")[0],
        "w1bd": w1bd, "posT2": posT2, "w2bd": w2bd, "wdwT": wdwT, "bdwT": bdwT,
        "b_dw_row": g("b_dw").reshape(1, 512), "w_dw_rows": g("w_dw").reshape(1, 31 * 512),
        "w_out": g("w_out")[0], "norm2_g": g("norm2_g").reshape(1, D),
        "w_gr": np.ascontiguousarray(np.concatenate([g("w_group")[0], g("w_router")[0]], 1)),
        "b_gr": np.concatenate([g("b_group").reshape(1, 4), g("b_router").reshape(1, 32)], 1),
        "w_gate": g("w_gate")[0], "w_up": g("w_up")[0], "w_down": g("w_down")[0],
        "final_g": g("final_g").reshape(1, D),
        "conv_ln_g": g("conv_ln_g").reshape(1, 512), "conv_ln_b": g("conv_ln_b").reshape(1, 512),
        "g_conv_out": g("g_conv_out").reshape(1, 512), "g_attn_out": g("g_attn_out").reshape(1, 512),
    }
    tabs = [_tables(0), _tables(1)]
    if STAGE >= 4 and "cache_cmp_kv" in inp:
        shared["cache_cmp"] = g("cache_cmp_kv").reshape(81920, 4096)
        shared["cache_slc"] = g("cache_slc_kv").reshape(81920, 4096)
    pt = np.asarray(inp["page_table"]).astype(np.int32)
    pp = np.arange(128)
    nn = np.arange(128)
    egt = np.zeros((128, 4, 128), f32)
    ovs = np.zeros((128, 4, 128), f32)
    for pg in range(4):
        egt[:, pg, :] = (pp[:, None] == (128 * pg + nn[None, :]) // 4)
        cn = pg * 128 + nn
        jj = np.arange(128)
        ovs[:, pg, :] = ((cn[:, None] < 511) & (16 * cn[:, None] <= 64 * jj[None, :] + 63) & (16 * cn[:, None] + 31 >= 64 * jj[None, :]))
    forct = np.zeros((128, 1), f32); forct[0] = 1.0; forct[127] = 1.0
    fmA = np.zeros((2, 128), f32); fmA[:, 0] = -10.0; fmA[:, 127] = -10.0
    shared.update({"egtab": egt, "ovstab": ovs, "forctab": forct, "fmaskAtab": fmA,
                   "pm8rep": np.ascontiguousarray(np.tile((pp % 8)[:, None], (1, NS * 4)).astype(f32))})
    maps = []
    for c in range(8):
        b, hf = c // 2, c % 2
        own = x_prompt[b, hf * NOWN:(hf + 1) * NOWN]
        oth = x_prompt[b, (1 - hf) * NOWN:(2 - hf) * NOWN]
        m = dict(shared)
        m.update(tabs[hf])
        m.update({
            "xc": np.ascontiguousarray(np.concatenate([own, oth], 0)),
            "xs": np.ascontiguousarray(x_sample[16 * c:16 * c + 16, 0]),
            "cin": np.ascontiguousarray(np.concatenate([g("c_prompt")[b:b + 1], g("c_sample")[16 * c:16 * c + 16]], 0)),
            "state_win": np.ascontiguousarray(g("state_win_kv")[0, 16 * c:16 * c + 16].reshape(NS, 512, 256)),
            "state_conv": np.ascontiguousarray(g("state_conv")[0, 16 * c:16 * c + 16]),
            "ptrep": np.ascontiguousarray(pt[16 * c:16 * c + 16].reshape(16, 4, 16)[:, :, pp // 8].transpose(2, 0, 1).reshape(128, 64)),
        })
        maps.append(m)
    return maps


def _run(inp, stage=STAGE):
    if stage not in _NC_CACHE:
        _NC_CACHE[stage] = build_program(stage)
    nc = _NC_CACHE[stage]
    maps = _prep_inputs(inp)
    res = run_bass_kernel_spmd(nc, maps, core_ids=list(range(8)))
    return res.results


def kernel(**inp):
    r = _run(inp)
    f32 = np.float32
    B = 4
    y_prompt = np.stack([np.concatenate([r[2 * b]["y_p"], r[2 * b + 1]["y_p"]], 0) for b in range(B)], 0).astype(f32)
    y_sample = np.concatenate([r[c]["y_s"] for c in range(8)], 0).reshape(128, 1, D).astype(f32)
    kvp = np.stack([r[2 * b]["o_kv_p"] for b in range(B)], 0)
    kvp = kvp.reshape(B, SEQ, 3, 2, 2, 64)
    new_cmp_p = np.ascontiguousarray(kvp[None, :, :, 0])
    new_slc_p = np.ascontiguousarray(kvp[None, :, :, 1])
    new_win_p = np.ascontiguousarray(kvp[None, :, SEQ - 512:, 2])
    new_conv_p = np.stack([r[2 * b + 1]["o_conv_p"] for b in range(B)], 0)[None]
    kvs = np.concatenate([r[c]["o_kv_s"] for c in range(8)], 0).reshape(128, 1, 3, 2, 2, 64)
    new_cmp_s = np.ascontiguousarray(kvs[None, :, :, 0])
    new_slc_s = np.ascontiguousarray(kvs[None, :, :, 1])
    new_win_s = np.concatenate([r[c]["o_win_s"] for c in range(8)], 0).reshape(1, 128, 512, 2, 2, 64)
    new_conv_s = np.concatenate([r[c]["o_conv_s"] for c in range(8)], 0)[None]
    return (y_prompt, y_sample, new_cmp_p, new_slc_p, new_win_p, new_conv_p.astype(f32),
            new_cmp_s, new_slc_s, new_win_s.astype(f32), new_conv_s.astype(f32))
```
